# Optimizing a Trainium2 kernel written in Bass

```python
import jax, jax.numpy as jnp
from jax import lax
import numpy as np

D_MODEL = 1024
BATCH = 2
SEQ = 16384
DEPTH = 2

N_MIXERS = 2
N_META = 16
POOL_WINDOWS = (2, 4, 8, 16)
POOL_GROUPS = len(POOL_WINDOWS)
POOL_GROUP_DIM = D_MODEL // POOL_GROUPS
MAX_WIN = max(POOL_WINDOWS)
HEAD_DIM = 64
N_HEADS = D_MODEL // HEAD_DIM
ATTN_BLOCK = 128
NEG_INF = -1e30
N_EXPERTS = 32
TOP_K = 4
D_EXPERT = D_MODEL
SWIGLU_LIMIT = 7.0
SWIGLU_ALPHA = 1.702
EXPERT_ROW_BLOCK = 256
DEEPNORM_ALPHA = (2 * DEPTH) ** 0.25
DEEPNORM_BETA = (8 * DEPTH) ** -0.25
LN_EPS = 1e-5
N_POOL_LAYERS = (DEPTH + N_MIXERS - 1) // N_MIXERS
N_FOX_LAYERS = DEPTH // N_MIXERS

kernel_name = 'hybrid_pool_fox_moe_deepnorm'


def layer_norm(x, g, b):
    xf = x.astype(jnp.float32)
    mu = jnp.mean(xf, axis=-1, keepdims=True)
    var = jnp.mean(jnp.square(xf - mu), axis=-1, keepdims=True)
    y = (xf - mu) * lax.rsqrt(var + LN_EPS) * g.astype(jnp.float32) + b.astype(jnp.float32)
    return y.astype(x.dtype)


def pool_mixer(x, pool_w, pool_scale):
    bsz, length, _ = x.shape
    xf = x.astype(jnp.float32)
    cs = jnp.cumsum(jnp.pad(xf, ((0, 0), (MAX_WIN + 1, 0), (0, 0))), axis=1)
    hi = cs[:, MAX_WIN + 1:MAX_WIN + 1 + length]
    pos = jnp.arange(length)
    means = []
    for g, w in enumerate(POOL_WINDOWS):
        sl = slice(g * POOL_GROUP_DIM, (g + 1) * POOL_GROUP_DIM)
        lo = cs[:, MAX_WIN + 1 - w:MAX_WIN + 1 - w + length, sl]
        cnt = jnp.minimum(pos + 1, w).astype(jnp.float32)[None, :, None]
        means.append((hi[..., sl] - lo) / cnt)
    u = jnp.concatenate(means, axis=-1) - xf
    u = u.reshape(bsz, length, POOL_GROUPS, POOL_GROUP_DIM)
    y = jnp.einsum('blgc,gcd->blgd', u, pool_w.astype(jnp.float32))
    y = y.reshape(bsz, length, D_MODEL) * pool_scale.astype(jnp.float32)
    return y.astype(x.dtype)


def fox_mixer(x, w_in, b_f, w_out):
    bsz, length, _ = x.shape
    proj = x @ w_in
    q = proj[..., :D_MODEL]
    k = proj[..., D_MODEL:2 * D_MODEL]
    v = proj[..., 2 * D_MODEL:3 * D_MODEL]
    log_f = jax.nn.log_sigmoid((proj[..., 3 * D_MODEL:] + b_f).astype(jnp.float32))
    pad = ATTN_BLOCK - N_META
    padded_len = pad + length
    n_blocks = padded_len // ATTN_BLOCK
    def to_heads(t):
        t = jnp.pad(t, ((0, 0), (pad, 0), (0, 0)))
        return t.reshape(bsz, padded_len, N_HEADS, HEAD_DIM).transpose(0, 2, 1, 3)
    qh, kh, vh = to_heads(q), to_heads(k), to_heads(v)
    c = jnp.cumsum(jnp.pad(log_f, ((0, 0), (pad, 0), (0, 0))), axis=1).transpose(0, 2, 1)
    key_pos = jnp.arange(padded_len)
    scale = HEAD_DIM ** -0.5

    def attend_block(i):
        start = i * ATTN_BLOCK
        qb = lax.dynamic_slice_in_dim(qh, start, ATTN_BLOCK, axis=2)
        cb = lax.dynamic_slice_in_dim(c, start, ATTN_BLOCK, axis=2)
        q_pos = start + jnp.arange(ATTN_BLOCK)
        logits = jnp.einsum('bhqd,bhkd->bhqk', qb, kh).astype(jnp.float32) * scale
        logits = logits + (cb[..., :, None] - c[..., None, :])
        mask = (key_pos[None, :] <= q_pos[:, None]) & (key_pos[None, :] >= pad)
        logits = jnp.where(mask, logits, NEG_INF)
        p = jax.nn.softmax(logits, axis=-1).astype(vh.dtype)
        return jnp.einsum('bhqk,bhkd->bhqd', p, vh)

    o = lax.map(attend_block, jnp.arange(n_blocks))
    o = o.transpose(1, 0, 3, 2, 4).reshape(bsz, padded_len, D_MODEL)[:, pad:]
    return o @ w_out


def clamped_swiglu(h):
    gate = jnp.minimum(h[..., :D_EXPERT], SWIGLU_LIMIT)
    up = jnp.clip(h[..., D_EXPERT:], -SWIGLU_LIMIT, SWIGLU_LIMIT)
    return gate * jax.nn.sigmoid(SWIGLU_ALPHA * gate) * (up + 1.0)


def moe(x, router_w, router_b, w1, b1, w2, b2):
    bsz, length, d = x.shape
    xt = x.reshape(-1, d)
    n_tok = xt.shape[0]
    logits = (xt @ router_w + router_b).astype(jnp.float32)
    top_v, top_i = lax.top_k(logits, TOP_K)
    gates = jax.nn.softmax(top_v, axis=-1).astype(x.dtype)
    n_copies = n_tok * TOP_K
    flat_e = top_i.reshape(-1)
    flat_tok = jnp.arange(n_copies, dtype=jnp.int32) // TOP_K
    flat_g = gates.reshape(-1)
    order = jnp.argsort(flat_e)
    sorted_e = flat_e[order]
    counts = jnp.bincount(flat_e, length=N_EXPERTS)
    starts = jnp.cumsum(counts) - counts
    padded = (counts + EXPERT_ROW_BLOCK - 1) // EXPERT_ROW_BLOCK * EXPERT_ROW_BLOCK
    pad_ends = jnp.cumsum(padded)
    pad_starts = pad_ends - padded
    dest = pad_starts[sorted_e] + (jnp.arange(n_copies) - starts[sorted_e])
    n_blocks = -(-n_copies // EXPERT_ROW_BLOCK) + N_EXPERTS
    n_rows = n_blocks * EXPERT_ROW_BLOCK
    row_tok = jnp.full((n_rows,), n_tok, jnp.int32).at[dest].set(flat_tok[order])
    row_g = jnp.zeros((n_rows,), x.dtype).at[dest].set(flat_g[order])
    block_e = jnp.minimum(
        jnp.searchsorted(pad_ends, jnp.arange(n_blocks) * EXPERT_ROW_BLOCK, side='right'),
        N_EXPERTS - 1)
    x_ext = jnp.concatenate([xt, jnp.zeros((1, d), xt.dtype)], axis=0)

    def expert_rows(args):
        tok, g, e = args
        h = x_ext[tok] @ w1[e] + b1[e]
        return (clamped_swiglu(h) @ w2[e] + b2[e]) * g[:, None]

    y = lax.map(expert_rows, (row_tok.reshape(n_blocks, EXPERT_ROW_BLOCK),
                              row_g.reshape(n_blocks, EXPERT_ROW_BLOCK), block_e))
    out = jax.ops.segment_sum(y.reshape(n_rows, d), row_tok, num_segments=n_tok + 1)[:n_tok]
    return out.reshape(bsz, length, d)


def setup_inputs(seed: int = 0) -> dict:
    key = jax.random.key(seed)
    ks = jax.random.split(key, 15)
    d, e, f, h = D_MODEL, N_EXPERTS, D_EXPERT, N_HEADS
    nrm = jax.random.normal
    x = nrm(ks[0], (BATCH, SEQ, d), jnp.float32)
    meta_tokens = nrm(ks[1], (N_META, d), jnp.float32)
    pool_w = nrm(ks[2], (N_POOL_LAYERS, POOL_GROUPS, POOL_GROUP_DIM, POOL_GROUP_DIM), jnp.float32) * (POOL_GROUP_DIM ** -0.5 * DEEPNORM_BETA)
    pool_scale = 1.0 + 0.02 * nrm(ks[3], (N_POOL_LAYERS, d), jnp.float32)
    col_scale = jnp.concatenate([jnp.ones((2 * d,), jnp.float32),
                                 jnp.full((d,), DEEPNORM_BETA, jnp.float32),
                                 jnp.ones((h,), jnp.float32)])
    attn_w_in = nrm(ks[4], (N_FOX_LAYERS, d, 3 * d + h), jnp.float32) * (d ** -0.5) * col_scale
    attn_b_f = jax.random.uniform(ks[5], (N_FOX_LAYERS, h), jnp.float32, minval=1.0, maxval=4.0)
    attn_w_out = nrm(ks[6], (N_FOX_LAYERS, d, d), jnp.float32) * (d ** -0.5 * DEEPNORM_BETA)
    ln_g = 1.0 + 0.02 * nrm(ks[7], (DEPTH, 2, d), jnp.float32)
    ln_b = 0.01 * nrm(ks[8], (DEPTH, 2, d), jnp.float32)
    router_w = nrm(ks[9], (DEPTH, d, e), jnp.float32) * (d ** -0.5)
    router_b = 0.01 * nrm(ks[10], (DEPTH, e), jnp.float32)
    w1 = nrm(ks[11], (DEPTH, e, d, 2 * f), jnp.float32) * (d ** -0.5)
    b1 = 0.01 * nrm(ks[12], (DEPTH, e, 2 * f), jnp.float32)
    w2 = nrm(ks[13], (DEPTH, e, f, d), jnp.float32) * (f ** -0.5 * DEEPNORM_BETA)
    b2 = 0.01 * nrm(ks[14], (DEPTH, e, d), jnp.float32)
    return {'x': x, 'meta_tokens': meta_tokens, 'pool_w': pool_w, 'pool_scale': pool_scale,
            'attn_w_in': attn_w_in, 'attn_b_f': attn_b_f, 'attn_w_out': attn_w_out,
            'ln_g': ln_g, 'ln_b': ln_b, 'router_w': router_w, 'router_b': router_b,
            'w1': w1, 'b1': b1, 'w2': w2, 'b2': b2}


def reference(x, meta_tokens, pool_w, pool_scale, attn_w_in, attn_b_f, attn_w_out,
              ln_g, ln_b, router_w, router_b, w1, b1, w2, b2):
    bsz = x.shape[0]
    meta = jnp.broadcast_to(meta_tokens.astype(x.dtype)[None], (bsz, N_META, D_MODEL))
    h = jnp.concatenate([meta, x], axis=1)
    for i in range(DEPTH):
        j = i // N_MIXERS
        if i % N_MIXERS == 0:
            mix = pool_mixer(h, pool_w[j], pool_scale[j])
        else:
            mix = fox_mixer(h, attn_w_in[j], attn_b_f[j], attn_w_out[j])
        h = layer_norm(DEEPNORM_ALPHA * h + mix, ln_g[i, 0], ln_b[i, 0])
        ffn = moe(h, router_w[i], router_b[i], w1[i], b1[i], w2[i], b2[i])
        h = layer_norm(DEEPNORM_ALPHA * h + ffn, ln_g[i, 1], ln_b[i, 1])
    return h[:, N_META:]
```

```python
import numpy as np
import ml_dtypes
from contextlib import ExitStack
import concourse.bass as bass
import concourse.mybir as mybir
from concourse.bass_utils import run_bass_kernel_spmd

F32 = mybir.dt.float32
BF16 = mybir.dt.bfloat16
I32 = mybir.dt.int32
ALU = mybir.AluOpType
AF = mybir.ActivationFunctionType

D = 1024
KC = 8
NE = 32
NH = 16
HD = 64
N_META = 16
SEQ = 16384
ALPHA = float((2 * 2) ** 0.25)
LN_EPS = 1e-5
BIG = float(2 ** 20)
NEG = -30000.0
CC_INC = 1


def ts(i, n):
    return slice(i * n, (i + 1) * n)


class DSem:
    def __init__(self, sem):
        self.sem = sem
        self.cnt = 0


class Buf:
    def __init__(self, name, t=None):
        self.name = name
        self.t = t
        self.w = {}
        self.r = {}
        self.dsem = None

    def __getitem__(self, idx):
        return self.t[idx]


class Sch:
    def __init__(self, nc, es, n_dsem=96):
        self.nc = nc
        self.es = es
        self.E = {'pe': nc.tensor, 'dve': nc.vector, 'act': nc.scalar, 'pool': nc.gpsimd, 'sp': nc.sync}
        self.sem = {k: es.enter_context(nc.semaphore('s_' + k)) for k in self.E}
        self.cnt = {k: 0 for k in self.E}
        self.seen = {k: {} for k in self.E}
        self.dpool = [DSem(es.enter_context(nc.semaphore('d%d' % i))) for i in range(n_dsem)]
        self.dfree = list(self.dpool)
        self.nbuf = 0

    def sb(self, es, name, shape, dt):
        self.nbuf += 1
        t = es.enter_context(self.nc.sbuf_tensor('%s_%d' % (name, self.nbuf), list(shape), dt))
        return Buf(name, t)

    def ps(self, es, name, shape, dt=F32):
        self.nbuf += 1
        t = es.enter_context(self.nc.psum_tensor('%s_%d' % (name, self.nbuf), list(shape), dt))
        return Buf(name, t)

    def view(self, name):
        return Buf(name)

    def _wait(self, eng, toks):
        best = {}
        for (sem, val, owner) in toks:
            if owner == 'pe' and eng == 'pe':
                continue
            k = id(sem)
            if self.seen[eng].get(k, 0) >= val:
                continue
            if k not in best or best[k][1] < val:
                best[k] = (sem, val)
        for k, (sem, val) in best.items():
            self.E[eng].wait_ge(sem, val)
            self.seen[eng][k] = val

    @staticmethod
    def _deps(reads, writes, skip_w=None):
        toks = []
        for b in reads:
            toks.extend(b.w.values())
        for b in writes:
            if b is not skip_w:
                toks.extend(b.w.values())
            toks.extend(b.r.values())
        return toks

    @staticmethod
    def _rec(tok, reads, writes, join=False):
        k = id(tok[0])
        for b in writes:
            if join:
                b.w[k] = tok
            else:
                b.w = {k: tok}
                b.r = {}
        for b in reads:
            b.r[k] = tok

    def op(self, eng, fn, R=(), W=()):
        self._wait(eng, self._deps(R, W))
        ins = fn(self.E[eng])
        self.cnt[eng] += 1
        ins.then_inc(self.sem[eng], 1)
        self._rec((self.sem[eng], self.cnt[eng], eng), R, W)
        return ins

    def _dsem_of(self, b):
        if b.dsem is None:
            b.dsem = self.dfree.pop()
        return b.dsem

    def dma(self, q, fn, sbuf, R=(), W=(), join=False):
        ds = self._dsem_of(sbuf)
        toks = self._deps(R, W)
        if join:
            toks = [t for t in toks if t[2] != 'dma']
        self._wait(q, toks)
        ins = fn(self.E[q])
        ds.cnt += 16
        ins.then_inc(ds.sem, 16)
        self._rec((ds.sem, ds.cnt, 'dma'), R, W, join=join)
        return ins

    def coll(self, kind, groups, src, dst, src_ap=None, dst_ap=None):
        ds = self._dsem_of(dst)
        self._wait('pool', self._deps([src], [dst]))
        ins = self.nc.gpsimd.collective_compute(kind, ALU.bypass, replica_groups=groups,
                                                ins=[src.t if src_ap is None else src_ap],
                                                outs=[dst.t if dst_ap is None else dst_ap])
        ds.cnt += CC_INC
        ins.then_inc(ds.sem, CC_INC)
        self._rec((ds.sem, ds.cnt, 'dma'), [src], [dst])
        return ins

    def release(self, bufs):
        for b in bufs:
            if b.dsem is not None:
                self.dfree.append(b.dsem)
                b.dsem = None

    def barrier(self):
        toks = [(self.sem[k], self.cnt[k], k + '_b') for k in self.E if self.cnt[k] > 0]
        toks += [(d.sem, d.cnt, 'dma') for d in self.dpool if d.cnt > 0 and not getattr(d, 'nobar', False)]
        for e in self.E:
            self._wait(e, toks)


class Ring:
    def __init__(self, bufs):
        self.bufs = bufs
        self.i = -1

    def next(self):
        self.i = (self.i + 1) % len(self.bufs)
        return self.bufs[self.i]


class Prog:
    def __init__(self, cfg):
        self.cfg = cfg
        self.NT = cfg['NT']
        self.NSEG = cfg['NSEG']
        self.CAP = cfg['CAP']
        self.CT = self.CAP // 128
        self.NE = cfg.get('NE', NE)
        self.nc = bass.Bass("TRN2", target_bir_lowering=False)
        self.dram = {}

    def dt(self, name, shape, dtype, kind):
        t = self.nc.dram_tensor(name, list(shape), dtype, kind=kind)
        self.dram[name] = t
        b = Buf(name, t.ap())
        return b

    def consts(self, S, es):
        c = {}
        cin = self.dt('cst_f', [128, 128 * 2 + 32 + 128 * 4], F32, 'ExternalInput')
        cb = self.dt('cst_b', [128, 128 * 3], BF16, 'ExternalInput')
        c['f'] = S.sb(es, 'cst_f', [128, 128 * 2 + 32 + 512], F32)
        c['b'] = S.sb(es, 'cst_b', [128, 384], BF16)
        S.dma('sp', lambda e: e.dma_start(out=c['f'][:], in_=cin[:, :]), c['f'], R=[cin], W=[c['f']])
        S.dma('sp', lambda e: e.dma_start(out=c['b'][:], in_=cb[:, :]), c['b'], R=[cb], W=[c['b']])
        c['ident'] = c['f'].t[:, 0:128]
        c['ecol'] = c['f'].t[:, 256:288]
        c['rcnt'] = c['f'].t[:, 288:800]
        c['identb'] = c['b'].t[:, 0:128]
        c['U'] = c['b'].t[:, 128:256]
        c['ones'] = c['b'].t[:, 256:384]
        self.c = c
        return c

    def ln_tile(self, S, R, src_ap, src_buf, gb, out_buf):
        st = R['st'].next(); mv = R['mv'].next(); sd = R['sd'].next(); hn = R['hn'].next()
        for k in range(2):
            S.op('dve', lambda e, k=k: e.bn_stats(out=st[:, ts(k, 6)], in_=src_ap[:, ts(k, 512)]), R=[src_buf], W=[st])
        S.op('dve', lambda e: e.bn_aggr(out=mv[:, :], in_=st[:, :]), R=[st], W=[mv])
        S.op('dve', lambda e: e.tensor_scalar(out=sd[:, 0:1], in0=mv[:, 1:2], scalar1=LN_EPS, scalar2=None,
                                              op0=ALU.add), R=[mv], W=[sd])
        S.op('act', lambda e: e.activation(out=sd[:, 1:2], in_=sd[:, 0:1], func=AF.Sqrt), R=[sd], W=[sd])
        S.op('dve', lambda e: e.reciprocal(out=sd[:, 2:3], in_=sd[:, 1:2]), R=[sd], W=[sd])
        S.op('dve', lambda e: e.tensor_scalar(out=hn[:, :], in0=src_ap, scalar1=mv[:, 0:1], scalar2=sd[:, 2:3],
                                              op0=ALU.subtract, op1=ALU.mult), R=[src_buf, mv, sd], W=[hn])
        S.op('pool', lambda e: e.tensor_tensor(out=hn[:, :], in0=hn[:, :], in1=gb[:, 0, :], op=ALU.mult),
             R=[hn, gb], W=[hn])
        S.op('dve', lambda e: e.tensor_tensor(out=out_buf[:, :], in0=hn[:, :], in1=gb[:, 1, :], op=ALU.add),
             R=[hn, gb], W=[out_buf])

    def load_gb(self, S, gbuf, lng, lnb, idx):
        S.dma('sp', lambda e: e.dma_start(out=gbuf[:, 0, :], in_=lng[idx:idx + 1, :].to_broadcast([128, D])),
              gbuf, R=[lng], W=[gbuf])
        S.dma('sp', lambda e: e.dma_start(out=gbuf[:, 1, :], in_=lnb[idx:idx + 1, :].to_broadcast([128, D])),
              gbuf, R=[lnb], W=[gbuf], join=True)

    def route_tile(self, S, R, i, h_buf, ctx):
        c = self.c
        CAP = self.CAP
        H, XS = ctx['H'], ctx['XS']
        dest4, gate4, valid = ctx['dest4'], ctx['gate4'], ctx['valid']
        rw, rbb = ctx['rw'], ctx['rbb']
        S.dma('sp', lambda e: e.dma_start(out=H[ts(i, 128), :], in_=h_buf[:, :]), h_buf, R=[h_buf], W=[H], join=True)
        hb = R['hb'].next()
        S.op('act', lambda e: e.activation(out=hb[:, :], in_=h_buf[:, :], func=AF.Copy), R=[h_buf], W=[hb])
        tp = R['big'].next()
        for kc in range(KC):
            S.op('pe', lambda e, kc=kc: e.transpose(out=tp[:, ts(kc, 128)], in_=h_buf[:, ts(kc, 128)],
                                                    identity=c['ident']), R=[h_buf, c['f']], W=[tp])
        hT = R['hT'].next()
        S.op('act', lambda e: e.activation(out=hT[:, :], in_=tp[:, :], func=AF.Copy), R=[tp], W=[hT])
        lgp = R['lgp'].next()
        for kc in range(KC):
            S.op('pe', lambda e, kc=kc: e.matmul(lgp[:, 0:32], lhsT=hT[:, ts(kc, 128)], rhs=rw[:, kc, :],
                                                 start=(kc == 0), stop=(kc == KC - 1)), R=[hT, rw], W=[lgp])
        sm = R['sm'].next()
        lg = sm[:, 0:32]; top8 = sm[:, 32:40]
        S.op('dve', lambda e: e.tensor_tensor(out=lg, in0=lgp[:, 0:32], in1=rbb[:, :], op=ALU.add), R=[lgp, rbb], W=[sm])
        S.op('dve', lambda e: e.max(out=top8, in_=lg), R=[sm], W=[sm])
        S.op('dve', lambda e: e.tensor_scalar(out=sm[:, 40:41], in0=sm[:, 32:33], scalar1=-1.0, scalar2=None,
                                              op0=ALU.mult), R=[sm], W=[sm])
        S.op('act', lambda e: e.activation(out=sm[:, 44:48], in_=sm[:, 32:36], func=AF.Exp, bias=sm[:, 40:41],
                                           scale=1.0), R=[sm], W=[sm])
        S.op('dve', lambda e: e.tensor_reduce(out=sm[:, 41:42], in_=sm[:, 44:48], axis=mybir.AxisListType.X,
                                              op=ALU.add), R=[sm], W=[sm])
        S.op('dve', lambda e: e.reciprocal(out=sm[:, 42:43], in_=sm[:, 41:42]), R=[sm], W=[sm])
        mb = R['mb'].next()
        S.op('dve', lambda e: e.tensor_scalar(out=mb[:, :], in0=lg, scalar1=sm[:, 35:36], scalar2=valid[:, i:i + 1],
                                              op0=ALU.is_ge, op1=ALU.mult), R=[sm, valid], W=[mb])
        p12 = lgp
        S.op('pe', lambda e: e.matmul(p12[:, 32:64], lhsT=c['U'], rhs=mb[:, :], start=True, stop=True),
             R=[mb, c['b']], W=[p12])
        S.op('pe', lambda e: e.matmul(p12[:, 64:96], lhsT=c['ones'], rhs=mb[:, :], start=True, stop=True),
             R=[mb, c['b']], W=[p12])
        cnt_old = ctx['cnt'][ctx['cnt_i'] % 2]
        cnt_new = ctx['cnt'][(ctx['cnt_i'] + 1) % 2]
        ctx['cnt_i'] += 1
        S.op('dve', lambda e: e.tensor_tensor(out=sm[:, 64:96], in0=p12[:, 32:64], in1=cnt_old[:, :], op=ALU.add),
             R=[p12, cnt_old], W=[sm])
        S.op('dve', lambda e: e.tensor_tensor(out=cnt_new[:, :], in0=p12[:, 64:96], in1=cnt_old[:, :], op=ALU.add),
             R=[p12, cnt_old], W=[cnt_new])
        S.op('dve', lambda e: e.tensor_scalar(out=sm[:, 96:128], in0=sm[:, 64:96], scalar1=float(CAP), scalar2=None,
                                              op0=ALU.is_lt), R=[sm], W=[sm])
        S.op('dve', lambda e: e.tensor_tensor(out=sm[:, 128:160], in0=sm[:, 64:96], in1=c['ecol'], op=ALU.add),
             R=[sm, c['f']], W=[sm])
        S.op('dve', lambda e: e.scalar_tensor_tensor(out=sm[:, 160:192], in0=sm[:, 128:160], scalar=valid[:, i:i + 1],
                                                     in1=sm[:, 96:128], op0=ALU.mult, op1=ALU.mult),
             R=[sm, valid], W=[sm])
        dk = R['dk'].next()
        for k in range(4):
            S.op('dve', lambda e, k=k: e.scalar_tensor_tensor(out=sm[:, 192:224], in0=lg, scalar=sm[:, 32 + k:33 + k],
                                                              in1=sm[:, 160:192], op0=ALU.is_equal, op1=ALU.mult,
                                                              accum_out=dk[:, k:k + 1]), R=[sm], W=[sm, dk])
        S.op('dve', lambda e: e.tensor_scalar(out=dk[:, 4:8], in0=dk[:, 0:4], scalar1=-0.5, scalar2=None,
                                              op0=ALU.is_lt), R=[dk], W=[dk])
        S.op('dve', lambda e: e.scalar_tensor_tensor(out=gate4[:, 4 * i:4 * i + 4], in0=sm[:, 44:48], scalar=sm[:, 42:43],
                                                     in1=dk[:, 4:8], op0=ALU.mult, op1=ALU.mult),
             R=[sm, dk], W=[gate4])
        S.op('dve', lambda e: e.tensor_scalar(out=dest4[:, 4 * i:4 * i + 4], in0=dk[:, 0:4], scalar1=BIG, scalar2=None,
                                              op0=ALU.add), R=[dk], W=[dest4])
        for k in range(4):
            S.dma('pool', lambda e, k=k: e.indirect_dma_start(
                out=XS[:, :], out_offset=bass.IndirectOffsetOnAxis(ap=dest4[:, 4 * i + k:4 * i + k + 1], axis=0),
                in_=hb[:, :], in_offset=None, bounds_check=self.bc_reg, oob_is_err=False),
                hb, R=[hb, dest4], W=[XS], join=True)

    def route_bufs(self, S, es):
        R = {}
        R['hb'] = Ring([S.sb(es, 'hb', [128, D], BF16) for _ in range(2)])
        R['hT'] = Ring([S.sb(es, 'hT', [128, D], F32) for _ in range(2)])
        R['sm'] = Ring([S.sb(es, 'sm', [128, 256], F32) for _ in range(2)])
        R['mb'] = Ring([S.sb(es, 'mb', [128, 32], BF16) for _ in range(2)])
        R['dk'] = Ring([S.sb(es, 'dk', [128, 8], F32) for _ in range(2)])
        R['lgp'] = Ring([S.ps(es, 'lgp', [128, 512])])
        return R

    def ln_bufs(self, S, es, R):
        R['st'] = Ring([S.sb(es, 'st', [128, 12], F32) for _ in range(2)])
        R['mv'] = Ring([S.sb(es, 'mv', [128, 2], F32) for _ in range(2)])
        R['sd'] = Ring([S.sb(es, 'sd', [128, 4], F32) for _ in range(2)])
        R['hn'] = Ring([S.sb(es, 'hn', [128, D], F32) for _ in range(2)])
        return R

    def moe_ctx(self, S, es, layer, inp):
        ctx = {}
        ctx['dest4'] = S.sb(es, 'dest4', [128, self.NT * 4], I32)
        ctx['gate4'] = S.sb(es, 'gate4', [128, self.NT * 4], F32)
        ctx['cnt'] = [S.sb(es, 'cnt', [128, 32], F32) for _ in range(2)]
        ctx['cnt_i'] = 0
        S.op('dve', lambda e: e.memset(ctx['cnt'][0][:, :], 0.0), W=[ctx['cnt'][0]])
        ctx['rw'] = S.sb(es, 'rw', [128, KC, 32], F32)
        ctx['rbb'] = S.sb(es, 'rbb', [128, 32], F32)
        rw_d, rb_d = inp['router_w'], inp['router_b']
        S.dma('sp', lambda e: e.dma_start(out=ctx['rw'][:, :, :],
                                          in_=rw_d[layer].rearrange("(kc p) e -> p kc e", p=128)),
              ctx['rw'], R=[rw_d], W=[ctx['rw']])
        S.dma('sp', lambda e: e.dma_start(out=ctx['rbb'][:, :], in_=rb_d[layer:layer + 1, :].to_broadcast([128, 32])),
              ctx['rbb'], R=[rb_d], W=[ctx['rbb']])
        ctx['valid'] = inp['valid_sb']
        if not hasattr(self, 'bc_reg'):
            self.bc_reg = self.nc.gpsimd.to_reg(self.NE * self.CAP - 1)
        ctx['H'] = inp['H']; ctx['XS'] = inp['XS']; ctx['YS'] = inp['YS']
        return ctx

    def mixer0(self, S, inp, ctx, gidx):
        nc = self.nc
        c = self.c
        xin = inp['xin']
        with ExitStack() as es:
            R = self.route_bufs(S, es)
            self.ln_bufs(S, es, R)
            R['big'] = Ring([S.ps(es, 'big', [128, D]) for _ in range(2)])
            mm = Ring([S.ps(es, 'mm', [128, 512]) for _ in range(2)])
            tph = S.ps(es, 'tph', [128, KC, 16])
            xt = Ring([S.sb(es, 'xt', [128, 4, D], F32) for _ in range(2)])
            xh = Ring([S.sb(es, 'xh', [16, D], F32) for _ in range(2)])
            xT = S.sb(es, 'xT', [128, KC, 528], F32)
            xTk = [S.view('xT%d' % k) for k in range(KC)]
            sA = Ring([S.sb(es, 'sA', [128, 528], F32) for _ in range(2)])
            sB = Ring([S.sb(es, 'sB', [128, 528], F32) for _ in range(2)])
            uT = S.sb(es, 'uT', [128, KC, 512], BF16)
            uTk = [S.view('uT%d' % k) for k in range(KC)]
            rT = S.sb(es, 'rT', [128, KC, 512], F32)
            rTk = [S.view('rT%d' % k) for k in range(KC)]
            tmp = Ring([S.sb(es, 'tmp', [128, 512], F32) for _ in range(2)])
            h1 = Ring([S.sb(es, 'h1', [128, D], F32) for _ in range(2)])
            pw = S.sb(es, 'pw', [128, 4, 2, 256], BF16)
            psc = S.sb(es, 'psc', [128, KC], F32)
            gb = S.sb(es, 'gb', [128, 2, D], F32)
            pw_d, ps_d = inp['pool_w'], inp['pool_scale']
            S.dma('pool', lambda e: e.dma_start(out=pw[:, :, :, :],
                                                in_=pw_d[0].rearrange("g (ic p) d -> p g ic d", p=128)),
                  pw, R=[pw_d], W=[pw])
            with nc.allow_non_contiguous_dma(reason="tiny per-partition vector"):
                S.dma('sp', lambda e: e.dma_start(out=psc[:, :], in_=ps_d[0].rearrange("(kc p) -> p kc", p=128)),
                      psc, R=[ps_d], W=[psc])
            self.load_gb(S, gb, inp['ln_g'], inp['ln_b'], gidx)

            for seg in range(self.NSEG + 1):
                meta = (seg == self.NSEG)
                ntile = 1 if meta else 4
                W = ntile * 128
                base = seg * 528
                x_t = xt.next(); x_h = xh.next()
                S.dma('sp', lambda e: e.dma_start(out=x_h[:, :], in_=xin[base:base + 16, :]), x_h, R=[xin], W=[x_h])
                S.dma('sp', lambda e: e.dma_start(
                    out=x_t[:, 0:ntile, :], in_=xin[base + 16:base + 16 + W, :].rearrange("(t p) d -> p t d", p=128)),
                    x_t, R=[xin], W=[x_t])
                for kc in range(KC):
                    S.op('pe', lambda e, kc=kc: e.transpose(out=tph[:, kc, :], in_=x_h[:, ts(kc, 128)],
                                                            identity=c['ident'][0:16, 0:16]), R=[x_h, c['f']], W=[tph])
                for kc in range(KC):
                    S.op('act', lambda e, kc=kc: e.activation(out=xT[:, kc, 0:16], in_=tph[:, kc, :], func=AF.Copy),
                         R=[tph], W=[xTk[kc]])
                for kc in range(KC):
                    p = mm.next()
                    for t in range(ntile):
                        S.op('pe', lambda e, kc=kc, t=t: e.transpose(out=p[:, ts(t, 128)], in_=x_t[:, t, ts(kc, 128)],
                                                                     identity=c['ident']), R=[x_t, c['f']], W=[p])
                    S.op('act', lambda e, kc=kc: e.activation(out=xT[:, kc, 16:16 + W], in_=p[:, 0:W], func=AF.Copy),
                         R=[p], W=[xTk[kc]])
                for kc in range(KC):
                    g = kc // 2
                    w = 2 << g
                    cur_ap = xT[:, kc, :]
                    cur_buf = xTk[kc]
                    lo = 16 - (w - 1)
                    sh = 1
                    start = 1
                    bufs = [sA.next(), sB.next()]
                    bi = 0
                    first = 0
                    while sh < w:
                        first = first + sh
                        nb = bufs[bi]; bi ^= 1
                        S.op('dve', lambda e, nb=nb, cur_ap=cur_ap, first=first, sh=sh, W=W: e.tensor_tensor(
                            out=nb[:, first:16 + W], in0=cur_ap[:, first:16 + W], in1=cur_ap[:, first - sh:16 + W - sh],
                            op=ALU.add), R=[cur_buf], W=[nb])
                        cur_ap = nb[:, :]; cur_buf = nb
                        sh *= 2
                    if not meta:
                        S.op('dve', lambda e, kc=kc, cur_ap=cur_ap, w=w, W=W: e.scalar_tensor_tensor(
                            out=uT[:, kc, 0:W], in0=cur_ap[:, 16:16 + W], scalar=1.0 / w, in1=xT[:, kc, 16:16 + W],
                            op0=ALU.mult, op1=ALU.subtract), R=[cur_buf, xTk[kc]], W=[uTk[kc]])
                    else:
                        nb = bufs[bi]
                        S.op('dve', lambda e, nb=nb, cur_ap=cur_ap, g=g, W=W: e.tensor_tensor(
                            out=nb[:, 16:16 + W], in0=cur_ap[:, 16:16 + W], in1=c['rcnt'][:, ts(g, 128)], op=ALU.mult),
                            R=[cur_buf, c['f']], W=[nb])
                        S.op('dve', lambda e, nb=nb, kc=kc, W=W: e.tensor_tensor(
                            out=uT[:, kc, 0:W], in0=nb[:, 16:16 + W], in1=xT[:, kc, 16:16 + W], op=ALU.subtract),
                            R=[nb, xTk[kc]], W=[uTk[kc]])
                for oc8 in range(KC):
                    g = oc8 // 2
                    oc = oc8 % 2
                    p = mm.next()
                    for ic in range(2):
                        S.op('pe', lambda e, g=g, oc=oc, ic=ic, p=p, W=W: e.matmul(
                            p[:, 0:W], lhsT=pw[:, g, ic, ts(oc, 128)], rhs=uT[:, 2 * g + ic, 0:W],
                            start=(ic == 0), stop=(ic == 1)), R=[pw, uTk[2 * g + ic]], W=[p])
                    tm = tmp.next()
                    S.op('act', lambda e, p=p, tm=tm, oc8=oc8, W=W: e.activation(
                        out=tm[:, 0:W], in_=p[:, 0:W], func=AF.Copy, scale=psc[:, oc8:oc8 + 1]), R=[p, psc], W=[tm])
                    S.op('dve', lambda e, tm=tm, oc8=oc8, W=W: e.scalar_tensor_tensor(
                        out=rT[:, oc8, 0:W], in0=xT[:, oc8, 16:16 + W], scalar=ALPHA, in1=tm[:, 0:W],
                        op0=ALU.mult, op1=ALU.add), R=[xTk[oc8], tm], W=[rTk[oc8]])
                for t in range(ntile):
                    i = seg * 4 + t
                    rp = R['big'].next()
                    for kc in range(KC):
                        S.op('pe', lambda e, kc=kc, t=t, rp=rp: e.transpose(
                            out=rp[:, ts(kc, 128)], in_=rT[:, kc, ts(t, 128)], identity=c['ident']),
                            R=[rTk[kc], c['f']], W=[rp])
                    h = h1.next()
                    self.ln_tile(S, R, rp[:, :], rp, gb, h)
                    self.route_tile(S, R, i, h, ctx)
            S.barrier()
            S.release([b for r in R.values() for b in r.bufs] + xt.bufs + xh.bufs + [pw, psc, gb])

    def experts(self, S, inp, ctx, layer):
        nc = self.nc
        layer = layer - getattr(self, 'wbase', 0)
        c = self.c
        CAP, CT = self.CAP, self.CT
        XS, YS = ctx['XS'], ctx['YS']
        w1_d, w2_d, b1_d, b2_d = inp['w1'], inp['w2'], inp['b1'], inp['b2']
        cgs = []
        o = 0
        while o < CAP:
            n = min(512, CAP - o)
            cgs.append((o, n))
            o += n
        with ExitStack() as es:
            tpb = Ring([S.ps(es, 'tpb', [128, KC, 128], BF16) for _ in range(2)])
            hp = Ring([S.ps(es, 'hp', [128, 512]) for _ in range(4)])
            yp = Ring([S.ps(es, 'yp', [128, 512]) for _ in range(2)])
            b1T = S.sb(es, 'b1T', [128, 16, 32], F32)
            with ExitStack() as esb:
                b1r = S.sb(esb, 'b1r', [32, 2 * D], F32)
                S.dma('sp', lambda e: e.dma_start(out=b1r[:, :], in_=b1_d[layer]), b1r, R=[b1_d], W=[b1r])
                for fc in range(16):
                    p = yp.next()
                    S.op('pe', lambda e, fc=fc, p=p: e.transpose(out=p[:, 0:32], in_=b1r[:, ts(fc, 128)],
                                                                 identity=c['ident'][0:32, 0:32]), R=[b1r, c['f']], W=[p])
                    S.op('dve', lambda e, fc=fc, p=p: e.tensor_copy(out=b1T[:, fc, :], in_=p[:, 0:32]), R=[p], W=[b1T])
                S.barrier()
                S.release([b1r])
            w1b = Ring([S.sb(es, 'w1b', [128, KC, 2 * D], BF16) for _ in range(2)])
            w2b = Ring([S.sb(es, 'w2b', [128, KC, D], BF16) for _ in range(2)])
            b2b = Ring([S.sb(es, 'b2b', [128, D], F32) for _ in range(2)])
            xst = Ring([S.sb(es, 'xst', [128, CT, D], BF16) for _ in range(2)])
            xeT = Ring([S.sb(es, 'xeT', [128, KC, CAP], BF16) for _ in range(2)])
            aT = S.sb(es, 'aT', [128, KC, CAP], BF16)
            aTk = [S.view('aT%d' % k) for k in range(KC)]
            ys = Ring([S.sb(es, 'ys', [128, D], F32) for _ in range(2)])
            gc = Ring([S.sb(es, 'gc', [128, 512], F32) for _ in range(3)])
            sg = Ring([S.sb(es, 'sg', [128, 512], F32) for _ in range(3)])
            u0 = Ring([S.sb(es, 'u0', [128, 512], F32) for _ in range(3)])

            def load_w(ex):
                a = w1b.next(); b = w2b.next(); bb = b2b.next()
                for hlf in range(2):
                    S.dma('pool', lambda e, hlf=hlf: e.dma_start(
                        out=a[:, ts(hlf, 4), :],
                        in_=w1_d[layer, ex, ts(hlf, 512), :].rearrange("(kc p) f -> p kc f", p=128)),
                        a, R=[w1_d], W=[a], join=(hlf == 1))
                S.dma('pool', lambda e: e.dma_start(
                    out=b[:, :, :], in_=w2_d[layer, ex].rearrange("(kc p) f -> p kc f", p=128)), b, R=[w2_d], W=[b])
                S.dma('sp', lambda e: e.dma_start(out=bb[:, :], in_=b2_d[layer, ex:ex + 1, :].to_broadcast([128, D])),
                      bb, R=[b2_d], W=[bb])
                return a, b, bb

            def load_x(ex):
                xs_ = xst.next()
                S.dma('sp', lambda e: e.dma_start(
                    out=xs_[:, :, :], in_=XS[ex * CAP:(ex + 1) * CAP, :].rearrange("(ct p) d -> p ct d", p=128)),
                    xs_, R=[XS], W=[xs_])
                return xs_

            nxt_w = load_w(0)
            nxt_x = load_x(0)
            for ex in range(self.NE):
                wa, wb_, bb = nxt_w
                xs_ = nxt_x
                if ex + 1 < self.NE:
                    nxt_w = load_w(ex + 1)
                    nxt_x = load_x(ex + 1)
                xT_ = xeT.next()
                for ct in range(CT):
                    p = tpb.next()
                    for kc in range(KC):
                        S.op('pe', lambda e, ct=ct, kc=kc, p=p: e.transpose(
                            out=p[:, kc, :], in_=xs_[:, ct, ts(kc, 128)], identity=c['identb']),
                            R=[xs_, c['b']], W=[p])
                    eng = 'act' if ct % 2 == 0 else 'dve'
                    if eng == 'act':
                        S.op('act', lambda e, ct=ct, p=p: e.activation(out=xT_[:, :, ts(ct, 128)], in_=p[:, :, :],
                                                                        func=AF.Copy), R=[p], W=[xT_])
                    else:
                        S.op('dve', lambda e, ct=ct, p=p: e.tensor_copy(out=xT_[:, :, ts(ct, 128)], in_=p[:, :, :]),
                             R=[p], W=[xT_])
                def stage_a(j, o, n):
                    pa = hp.next(); pb = hp.next()
                    for kc in range(KC):
                        S.op('pe', lambda e, kc=kc: e.matmul(
                            pa[:, 0:n], lhsT=wa[:, kc, ts(j, 128)], rhs=xT_[:, kc, o:o + n],
                            start=(kc == 0), stop=(kc == KC - 1)), R=[wa, xT_], W=[pa])
                    for kc in range(KC):
                        S.op('pe', lambda e, kc=kc: e.matmul(
                            pb[:, 0:n], lhsT=wa[:, kc, ts(8 + j, 128)], rhs=xT_[:, kc, o:o + n],
                            start=(kc == 0), stop=(kc == KC - 1)), R=[wa, xT_], W=[pb])
                    g_ = gc.next(); s_ = sg.next(); u_ = u0.next()
                    S.op('dve', lambda e: e.tensor_scalar(
                        out=g_[:, 0:n], in0=pa[:, 0:n], scalar1=b1T[:, j, ex:ex + 1], scalar2=7.0,
                        op0=ALU.add, op1=ALU.min), R=[pa, b1T], W=[g_])
                    S.op('act', lambda e: e.activation(
                        out=u_[:, 0:n], in_=pb[:, 0:n], func=AF.Identity, bias=b1T[:, 8 + j, ex:ex + 1], scale=1.0),
                        R=[pb, b1T], W=[u_])
                    S.op('act', lambda e: e.activation(
                        out=s_[:, 0:n], in_=g_[:, 0:n], func=AF.Sigmoid, scale=1.702), R=[g_], W=[s_])
                    return (j, o, n, g_, s_, u_)

                def stage_b(st):
                    j, o, n, g_, s_, u_ = st
                    S.op('dve', lambda e: e.tensor_scalar(
                        out=u_[:, 0:n], in0=u_[:, 0:n], scalar1=7.0, scalar2=-7.0, op0=ALU.min, op1=ALU.max),
                        R=[u_], W=[u_])
                    S.op('pool', lambda e: e.tensor_tensor(
                        out=s_[:, 0:n], in0=g_[:, 0:n], in1=s_[:, 0:n], op=ALU.mult), R=[g_, s_], W=[s_])
                    S.op('dve', lambda e: e.scalar_tensor_tensor(
                        out=aT[:, j, o:o + n], in0=u_[:, 0:n], scalar=1.0, in1=s_[:, 0:n],
                        op0=ALU.add, op1=ALU.mult), R=[u_, s_], W=[aTk[j]])

                prev = None
                for j in range(KC):
                    for (o, n) in cgs:
                        cur = stage_a(j, o, n)
                        if prev is not None:
                            stage_b(prev)
                        prev = cur
                stage_b(prev)
                for ct in range(CT):
                    y_ = ys.next()
                    for dh in range(2):
                        p = yp.next()
                        for fc in range(KC):
                            S.op('pe', lambda e, fc=fc, p=p, ct=ct, dh=dh: e.matmul(
                                p[:, :], lhsT=aT[:, fc, ts(ct, 128)], rhs=wb_[:, fc, ts(dh, 512)],
                                start=(fc == 0), stop=(fc == KC - 1)), R=[aTk[fc], wb_], W=[p])
                        S.op('dve', lambda e, p=p, y_=y_, dh=dh: e.tensor_tensor(
                            out=y_[:, ts(dh, 512)], in0=p[:, :], in1=bb[:, ts(dh, 512)], op=ALU.add),
                            R=[p, bb], W=[y_])
                    r0 = ex * CAP + ct * 128
                    S.dma('sp', lambda e, y_=y_, r0=r0: e.dma_start(out=YS[r0:r0 + 128, :], in_=y_[:, :]),
                          y_, R=[y_], W=[YS], join=True)
            S.barrier()
            S.release(w1b.bufs + w2b.bufs + b2b.bufs + xst.bufs + ys.bufs)

    def combine_tile(self, S, R, i, ctx, gb, out_buf):
        H, YS = ctx['H'], ctx['YS']
        dest4, gate4 = ctx['dest4'], ctx['gate4']
        hres = R['hres'].next()
        S.dma('sp', lambda e: e.dma_start(out=hres[:, :], in_=H[ts(i, 128), :]), hres, R=[H], W=[hres])
        yk = []
        for k in range(4):
            y = R['yk'].next()
            S.dma('pool', lambda e, y=y, k=k: e.indirect_dma_start(
                out=y[:, :], out_offset=None, in_=YS[:, :],
                in_offset=bass.IndirectOffsetOnAxis(ap=dest4[:, 4 * i + k:4 * i + k + 1], axis=0),
                bounds_check=self.bc_reg, oob_is_err=False), y, R=[YS, dest4], W=[y])
            yk.append(y)
        acc = R['acc'].next()
        S.op('act', lambda e: e.activation(out=acc[:, :], in_=hres[:, :], func=AF.Copy, scale=ALPHA),
             R=[hres], W=[acc])
        for k in range(4):
            S.op('dve', lambda e, k=k: e.scalar_tensor_tensor(
                out=acc[:, :], in0=yk[k][:, :], scalar=gate4[:, 4 * i + k:4 * i + k + 1], in1=acc[:, :],
                op0=ALU.mult, op1=ALU.add), R=[yk[k], gate4, acc], W=[acc])
        self.ln_tile(S, R, acc[:, :], acc, gb, out_buf)

    def combine_bufs(self, S, es):
        R = {}
        self.ln_bufs(S, es, R)
        R['hres'] = Ring([S.sb(es, 'hres', [128, D], F32) for _ in range(2)])
        R['yk'] = Ring([S.sb(es, 'yk', [128, D], F32) for _ in range(8)])
        R['acc'] = Ring([S.sb(es, 'acc', [128, D], F32) for _ in range(2)])
        for y in R['yk'].bufs:
            S.op('dve', lambda e, y=y: e.memset(y[:, :], 0.0), W=[y])
        return R

    def combine_qkv(self, S, inp, ctx, gidx, outs):
        nc = self.nc
        c = self.c
        H2, QT, KT, VV, LF = outs['H2'], outs['QT'], outs['KT'], outs['V'], outs['LF']
        win_d, bf_d = inp['attn_w_in'], inp['attn_b_f']
        with ExitStack() as es:
            R = self.combine_bufs(S, es)
            gb = S.sb(es, 'gb2', [128, 2, D], F32)
            self.load_gb(S, gb, inp['ln_g'], inp['ln_b'], gidx)
            ho = Ring([S.sb(es, 'ho', [128, D], F32) for _ in range(2)])
            big = Ring([S.ps(es, 'big', [128, D]) for _ in range(2)])
            mm = Ring([S.ps(es, 'mm', [128, 512]) for _ in range(3)])
            fps = S.ps(es, 'fps', [16, 512])
            win = S.sb(es, 'win', [128, KC, 3088], BF16)
            for hf in range(2):
                S.dma('pool', lambda e, hf=hf: e.dma_start(
                    out=win[:, :, ts(hf, 1544)],
                    in_=win_d[0, :, ts(hf, 1544)].rearrange("(kc p) f -> p kc f", p=128)),
                    win, R=[win_d], W=[win], join=(hf == 1))
            nbf = S.sb(es, 'nbf', [16, 2], F32)
            with nc.allow_non_contiguous_dma(reason="tiny per-partition vector"):
                S.dma('sp', lambda e: e.dma_start(out=nbf[:, 0:1], in_=bf_d[0].rearrange("(h o) -> h o", o=1)),
                      nbf, R=[bf_d], W=[nbf])
            S.op('dve', lambda e: e.tensor_scalar(out=nbf[:, 1:2], in0=nbf[:, 0:1], scalar1=-1.0, scalar2=None,
                                                  op0=ALU.mult), R=[nbf], W=[nbf])
            h2T = Ring([S.sb(es, 'h2T', [128, KC, 512], BF16) for _ in range(2)])
            qks = Ring([S.sb(es, 'qks', [128, 512], BF16) for _ in range(3)])
            vsb = Ring([S.sb(es, 'vsb', [128, NH, 65], BF16) for _ in range(2)])
            for v_ in vsb.bufs:
                S.op('pool', lambda e, v_=v_: e.memset(v_[:, :, :], 1.0), W=[v_])
            fsb = Ring([S.sb(es, 'fsb', [16, 2, 512], F32) for _ in range(2)])
            for seg in range(self.NSEG + 1):
                ntile = 1 if seg == self.NSEG else 4
                W = ntile * 128
                t0 = seg * 512
                hT_ = h2T.next()
                for t in range(ntile):
                    i = seg * 4 + t
                    h = ho.next()
                    self.combine_tile(S, R, i, ctx, gb, h)
                    S.dma('sp', lambda e, h=h, i=i: e.dma_start(out=H2[ts(i, 128), :], in_=h[:, :]), h,
                          R=[h], W=[H2], join=True)
                    tp = big.next()
                    for kc in range(KC):
                        S.op('pe', lambda e, kc=kc, tp=tp, h=h: e.transpose(
                            out=tp[:, ts(kc, 128)], in_=h[:, ts(kc, 128)], identity=c['ident']),
                            R=[h, c['f']], W=[tp])
                    S.op('act', lambda e, tp=tp, t=t: e.activation(
                        out=hT_[:, :, ts(t, 128)], in_=tp[:, :].rearrange("p (kc t) -> p kc t", kc=KC), func=AF.Copy),
                        R=[tp], W=[hT_])
                if seg > 0:
                    outs['exchange'](seg - 1)
                for m in range(16):
                    p = mm.next()
                    for kc in range(KC):
                        S.op('pe', lambda e, kc=kc, p=p, m=m: e.matmul(
                            p[:, 0:W], lhsT=win[:, kc, ts(m, 128)], rhs=hT_[:, kc, 0:W],
                            start=(kc == 0), stop=(kc == KC - 1)), R=[win, hT_], W=[p])
                    q_ = qks.next()
                    S.op('act', lambda e, p=p, q_=q_, m=m: e.activation(
                        out=q_[:, 0:W], in_=p[:, 0:W], func=AF.Copy, scale=(0.125 if m < 8 else 1.0)), R=[p], W=[q_])
                    for hh in range(2):
                        hd_ = 2 * (m % 8) + hh
                        if m < 8:
                            S.dma('sp', lambda e, q_=q_, hh=hh, hd_=hd_: e.dma_start(
                                out=QT[hd_, :, t0:t0 + W], in_=q_[ts(hh, 64), 0:W]), q_, R=[q_], W=[QT], join=True)
                        else:
                            kd = KT[seg][hd_ // 8]
                            S.dma('sp', lambda e, q_=q_, hh=hh, hd_=hd_, kd=kd: e.dma_start(
                                out=kd[ts(hd_ % 8, 64), 0:W], in_=q_[ts(hh, 64), 0:W]), q_, R=[q_], W=[kd], join=True)
                for t in range(ntile):
                    v_ = vsb.next()
                    for hf in range(2):
                        p = mm.next()
                        for kc in range(KC):
                            S.op('pe', lambda e, kc=kc, p=p, t=t, hf=hf: e.matmul(
                                p[:, :], lhsT=hT_[:, kc, ts(t, 128)], rhs=win[:, kc, 2048 + hf * 512:2560 + hf * 512],
                                start=(kc == 0), stop=(kc == KC - 1)), R=[win, hT_], W=[p])
                        S.op('dve', lambda e, p=p, v_=v_, hf=hf: e.tensor_copy(
                            out=v_[:, ts(hf, 8), 0:64], in_=p[:, :].rearrange("p (h d) -> p h d", d=64)),
                             R=[p], W=[v_])
                    for q4 in range(4):
                        vd = VV[seg][q4]
                        S.dma('sp', lambda e, v_=v_, t=t, q4=q4, vd=vd: e.dma_start(
                            out=vd.t.rearrange("(h p) (k d) -> p h k d", p=128, d=65)[:, :, t, :],
                            in_=v_[:, ts(q4, 4), :]), v_, R=[v_], W=[vd], join=True)
                for kc in range(KC):
                    S.op('pe', lambda e, kc=kc: e.matmul(
                        fps[:, 0:W], lhsT=win[:, kc, 3072:3088], rhs=hT_[:, kc, 0:W],
                        start=(kc == 0), stop=(kc == KC - 1)), R=[win, hT_], W=[fps])
                f_ = fsb.next()
                S.op('act', lambda e: e.activation(out=f_[:, 0, 0:W], in_=fps[:, 0:W], func=AF.Exp,
                                                   bias=nbf[:, 1:2], scale=-1.0), R=[fps, nbf], W=[f_])
                S.op('dve', lambda e: e.tensor_scalar(out=f_[:, 0, 0:W], in0=f_[:, 0, 0:W], scalar1=1.0, scalar2=None,
                                                      op0=ALU.add), R=[f_], W=[f_])
                S.op('act', lambda e: e.activation(out=f_[:, 1, 0:W], in_=f_[:, 0, 0:W], func=AF.Ln), R=[f_], W=[f_])
                S.op('dve', lambda e: e.tensor_scalar(out=f_[:, 1, 0:W], in0=f_[:, 1, 0:W], scalar1=-1.0, scalar2=None,
                                                      op0=ALU.mult), R=[f_], W=[f_])
                S.dma('sp', lambda e, f_=f_: e.dma_start(out=LF[:, t0:t0 + W], in_=f_[:, 1, 0:W]), f_,
                      R=[f_], W=[LF], join=True)
            outs['exchange'](self.NSEG)
            outs['exchange'](-1)
            S.barrier()

    def attention(self, S, inp, es_outer, after_init=None):
        nc = self.nc
        c = self.c
        NKB = 129
        CL = 2176
        QT, AT, CALL, QA = (inp[k] for k in ('QT', 'AT', 'CALL', 'QA'))
        RK, RV, RL, LFT = (inp[k] for k in ('RCVK', 'RCVV', 'RCVL', 'LFT'))
        c2d = inp['cst2']
        with ExitStack() as es:
            c2 = S.sb(es, 'c2', [128, 466], F32)
            S.dma('sp', lambda e: e.dma_start(out=c2[:, :], in_=c2d[:, :]), c2, R=[c2d], W=[c2])
            BT = c2.t[:, 0:128]; rowsel = c2.t[:, 128:256]; Dg = c2.t[:, 256:384]; padadd = c2.t[:, 384:385]
            E65 = c2.t[0:65, 385:449]
            oh16 = c2.t[:, 449:465]; ch0col = c2.t[:, 465:466]
            mk = S.sb(es, 'maskT', [128, 16, 512], BF16)
            S.dma('sp', lambda e: e.dma_start(out=mk[:, :, :], in_=inp['maskT'].t.rearrange("b p q -> p b q")),
                  mk, R=[inp['maskT']], W=[mk])
            idxq = S.sb(es, 'idxq', [128, 1], I32)
            S.dma('sp', lambda e: e.dma_start(out=idxq[:, :], in_=inp['idxq'][:, :]), idxq, R=[inp['idxq']], W=[idxq])
            cT = S.sb(es, 'cT', [128, 136, 16], F32)
            refbc = S.sb(es, 'refbc', [128, 128], F32)
            with ExitStack() as es1:
                lfa = S.sb(es1, 'lfa', [128, CL], F32)
                ones = S.sb(es1, 'onesf', [128, CL], F32)
                call = S.sb(es1, 'call', [128, CL], F32)
                sm = S.sb(es1, 'psm', [128, 8], F32)
                cq = S.sb(es1, 'cq', [128, 512], F32)
                wq = S.sb(es1, 'wq', [128, 3, 512], F32)
                qa = S.sb(es1, 'qa', [128, 2, 512], BF16)
                tmpd = S.sb(es1, 'tmpd', [128, 128], F32)
                psA = S.ps(es1, 'psA', [128, 512])
                psB = Ring([S.ps(es1, 'psB', [128, 512]) for _ in range(2)])
                with ExitStack() as es0:
                    lfh = S.sb(es0, 'lfh', [16, 17408], F32)
                    S.op('pool', lambda e: e.memset(lfh[:, :], 0.0), W=[lfh])
                    for cl in range(4):
                        S.dma('sp', lambda e, cl=cl: e.dma_start(
                            out=lfh.t[:, 128:16512].rearrange("h (j c t) -> h c j t", c=4, t=512)[:, cl],
                            in_=RL[cl * 16:(cl + 1) * 16, 0:4096].rearrange("h (j t) -> h j t", t=512)),
                            lfh, R=[RL], W=[lfh], join=(cl > 0))
                    S.dma('sp', lambda e: e.dma_start(out=lfh[:, 0:16], in_=RL[0:16, 4096:4112]), lfh,
                          R=[RL], W=[lfh], join=True)
                    S.dma('sp', lambda e: e.dma_start(out=LFT[:, :], in_=lfh[:, :]), lfh, R=[lfh], W=[LFT])
                    S.barrier()
                    S.release([lfh])
                S.dma('sp', lambda e: e.dma_start(out=lfa[:, :], in_=LFT.t.rearrange("h (ch c) -> (h ch) c", ch=8)),
                      lfa, R=[LFT], W=[lfa])
                S.op('pool', lambda e: e.memset(ones[:, :], 1.0), W=[ones])
                S.op('dve', lambda e: e.tensor_tensor_scan(out=call[:, :], data0=ones[:, :], data1=lfa[:, :], initial=0.0,
                                                           op0=ALU.mult, op1=ALU.add), R=[ones, lfa], W=[call])
                S.op('pe', lambda e: e.matmul(psA[:, 0:2], lhsT=BT, rhs=call[:, CL - 2:CL], start=True, stop=True),
                     R=[c2, call], W=[psA])
                S.op('dve', lambda e: e.tensor_copy(out=sm[:, 0:2], in_=psA[:, 0:2]), R=[psA], W=[sm])
                S.op('dve', lambda e: e.tensor_scalar(out=call[:, :], in0=call[:, :], scalar1=sm[:, 1:2], scalar2=None,
                                                      op0=ALU.add), R=[sm, call], W=[call])
                S.dma('sp', lambda e: e.dma_start(out=CALL.t.rearrange("h (ch c) -> (h ch) c", ch=8), in_=call[:, :]),
                      call, R=[call], W=[CALL])
                cT4 = cT.t.rearrange("p (ch bl) h -> p ch bl h", ch=8)
                for bl in range(17):
                    p = psB.next()
                    S.op('pe', lambda e, p=p, bl=bl: e.transpose(out=p[:, 0:128], in_=call[:, ts(bl, 128)],
                                                                 identity=c['ident']), R=[call, c['f']], W=[p])
                    S.op('act' if bl % 2 else 'dve',
                         (lambda e, p=p, bl=bl: e.activation(out=cT4[:, :, bl, :],
                                                             in_=p[:, 0:128].rearrange("p (h ch) -> p ch h", ch=8),
                                                             func=AF.Copy)) if bl % 2 else
                         (lambda e, p=p, bl=bl: e.tensor_copy(out=cT4[:, :, bl, :],
                                                              in_=p[:, 0:128].rearrange("p (h ch) -> p ch h", ch=8))),
                         R=[p], W=[cT])
                S.op('pe', lambda e: e.matmul(psA[:, 128:256], lhsT=rowsel, rhs=cT[:, 0:128:16, :], start=True, stop=True),
                     R=[c2, cT], W=[psA])
                S.op('dve', lambda e: e.tensor_copy(out=refbc[:, :], in_=psA[:, 128:256]), R=[psA], W=[refbc])
                KA = inp['KA']
                refhj = S.sb(es1, 'refhj', [128, 8], F32)
                padm = S.sb(es1, 'padm', [128, 128], F32)
                S.op('dve', lambda e: e.memset(padm[:, :], 0.0), W=[padm])
                S.op('dve', lambda e: e.tensor_scalar(out=padm[:, 16:128], in0=padm[:, 16:128], scalar1=ch0col, scalar2=None,
                                                      op0=ALU.add), R=[padm, c2], W=[padm])
                for j in range(8):
                    S.op('dve', lambda e, j=j: e.scalar_tensor_tensor(
                        out=tmpd[:, 0:16], in0=refbc[:, j * 16:(j + 1) * 16], scalar=1.0, in1=oh16, op0=ALU.mult,
                        op1=ALU.mult, accum_out=refhj[:, j:j + 1]), R=[refbc, c2], W=[tmpd, refhj])
                kv = Ring([S.sb(es1, 'kv', [128, 2, 2176], F32) for _ in range(2)])
                kb = Ring([S.sb(es1, 'kb', [128, 2, 2176], BF16) for _ in range(2)])
                for j in range(8):
                    v_ = kv.next(); b_ = kb.next()
                    S.op('dve', lambda e, j=j, v_=v_: e.tensor_scalar(out=v_[:, 0, :], in0=call[:, :], scalar1=-1.0,
                                                                      scalar2=refhj[:, j:j + 1], op0=ALU.mult, op1=ALU.add),
                         R=[call, refhj], W=[v_])
                    S.op('dve', lambda e, v_=v_: e.tensor_tensor(out=v_[:, 0, 0:128], in0=v_[:, 0, 0:128], in1=padm[:, :],
                                                                 op=ALU.subtract), R=[v_, padm], W=[v_])
                    S.op('act', lambda e, v_=v_, b_=b_: e.activation(out=b_[:, 0, :], in_=v_[:, 0, :], func=AF.Copy),
                         R=[v_], W=[b_])
                    S.op('act', lambda e, v_=v_, b_=b_: e.activation(out=v_[:, 1, :], in_=b_[:, 0, :], func=AF.Copy),
                         R=[b_], W=[v_])
                    S.op('dve', lambda e, v_=v_, b_=b_: e.tensor_tensor(out=b_[:, 1, :], in0=v_[:, 0, :], in1=v_[:, 1, :],
                                                                        op=ALU.subtract), R=[v_], W=[b_])
                    S.dma('sp', lambda e, j=j, b_=b_: e.dma_start(
                        out=KA.t[:, :, 2 * j:2 * j + 2, :].rearrange("h ch r c -> (h ch) r c"), in_=b_[:, :, :]),
                        b_, R=[b_], W=[KA], join=True)
                S.op('dve', lambda e: e.tensor_tensor(out=tmpd[:, :], in0=refbc[:, :], in1=Dg, op=ALU.mult),
                     R=[refbc, c2], W=[tmpd])
                S.op('dve', lambda e: e.tensor_reduce(out=sm[:, 2:3], in_=tmpd[:, :], axis=mybir.AxisListType.X, op=ALU.add),
                     R=[tmpd], W=[sm])
                bq = nc.gpsimd.to_reg(16 * 34 - 1)
                S.dma('pool', lambda e: e.indirect_dma_start(
                    out=cq[:, :], out_offset=None, in_=CALL.t.rearrange("h (a c) -> (h a) c", c=512),
                    in_offset=bass.IndirectOffsetOnAxis(ap=idxq[:, 0:1], axis=0), element_offset=128,
                    bounds_check=bq, oob_is_err=False), cq, R=[CALL, idxq], W=[cq])
                S.op('dve', lambda e: e.tensor_scalar(out=wq[:, 0, :], in0=cq[:, :], scalar1=sm[:, 2:3], scalar2=None,
                                                      op0=ALU.subtract), R=[cq, sm], W=[wq])
                S.op('dve', lambda e: e.tensor_copy(out=qa[:, 0, :], in_=wq[:, 0, :]), R=[wq], W=[qa])
                S.op('dve', lambda e: e.tensor_copy(out=wq[:, 1, :], in_=qa[:, 0, :]), R=[qa], W=[wq])
                S.op('dve', lambda e: e.tensor_tensor(out=qa[:, 1, :], in0=wq[:, 0, :], in1=wq[:, 1, :], op=ALU.subtract),
                     R=[wq], W=[qa])
                S.dma('sp', lambda e: e.dma_start(out=QA[:, :, :], in_=qa[:, :, :]), qa, R=[qa], W=[QA])
                S.barrier()
                S.release([lfa, call, cq, qa] + kb.bufs)
            Kt = Ring([S.sb(es, 'Kt', [96, 136 * 128], BF16) for _ in range(2)])
            Vt = Ring([S.sb(es, 'Vt', [128, NKB, 65], BF16) for _ in range(2)])
            Qt = Ring([S.sb(es, 'Qt', [96, 512], BF16) for _ in range(8)])
            Pt = Ring([S.sb(es, 'Pt', [128, 1024], BF16) for _ in range(3)])
            osb = Ring([S.sb(es, 'osb', [64, 512], F32) for _ in range(2)])
            r65 = Ring([S.sb(es, 'r65', [65, 512], F32) for _ in range(2)])
            asb = Ring([S.sb(es, 'asb', [64, 512], BF16) for _ in range(2)])
            Sp = Ring([S.ps(es, 'Sp', [128, 1024]) for _ in range(2)])
            Op = Ring([S.ps(es, 'Op', [128, 512]) for _ in range(2)])
            bcp = S.ps(es, 'bcp', [128, 512])
            for k_ in Kt.bufs:
                S.op('pool', lambda e, k_=k_: e.memset(k_[64:96, :], 0.0), W=[k_])
                S.op('pool', lambda e, k_=k_: e.memset(k_[64:66, :], 1.0), W=[k_])
                S.op('pool', lambda e, k_=k_: e.memset(k_[0:64, 0:128], 0.0), W=[k_])
            for jq, q_ in enumerate(Qt.bufs):
                S.dma('sp', lambda e, jq=jq, q_=q_: e.dma_start(out=q_[64:96, :], in_=inp['qone'][jq]), q_,
                      R=[inp['qone']], W=[q_])
            for v_ in Vt.bufs:
                S.op('pool', lambda e, v_=v_: e.memset(v_[:, 0, :], 0.0), W=[v_])
            for r_ in r65.bufs:
                S.op('pool', lambda e, r_=r_: e.memset(r_[:, :], 0.0), W=[r_])
            if after_init is not None:
                after_init()
            units = []
            for h in range(NH):
                for j in range(self.NSEG):
                    nblk = 1 + 16 * (j + 1)
                    b = 0
                    while b < nblk:
                        nb_ = min(2, nblk - b)
                        units.append((h, j, b, nb_, nblk))
                        b += nb_
            state = {}

            def prep_hj(h, j):
                if (h, j) in state:
                    return state[(h, j)]
                if j == 0:
                    k_ = Kt.next(); v_ = Vt.next()
                    for jj in range(8):
                        rk = RK[jj][h // 8]
                        S.dma('sp', lambda e, jj=jj, rk=rk: e.dma_start(
                            out=k_.t[0:64, 128 + jj * 2048:128 + (jj + 1) * 2048].rearrange("p (c t) -> p c t", c=4),
                            in_=rk.t.rearrange("(c r) t -> r c t", c=4)[ts(h % 8, 64)]),
                            k_, R=[rk], W=[k_], join=(jj > 0))
                    rk = RK[8][h // 8]
                    S.dma('sp', lambda e: e.dma_start(out=k_[0:64, 0:16], in_=rk[ts(h % 8, 64), 0:16]),
                          k_, R=[rk], W=[k_], join=True)
                    S.dma('sp', lambda e: e.dma_start(
                        out=k_.t[66:82, :].rearrange("r (ch c) -> r ch c", ch=8),
                        in_=inp['KA'].t[h].rearrange("ch r c -> r ch c")), k_, R=[inp['KA']], W=[k_], join=True)
                    for jj in range(8):
                        rv = RV[jj][h // 4]
                        S.dma('sp', lambda e, jj=jj, rv=rv: e.dma_start(
                            out=v_.t[:, 1 + 16 * jj:17 + 16 * jj, :].rearrange("p (c k) d -> p c (k d)", c=4),
                            in_=rv.t.rearrange("(c r) kd -> r c kd", c=4)[ts(h % 4, 128)]),
                            v_, R=[rv], W=[v_], join=(jj > 0))
                    rv = RV[8][h // 4]
                    S.dma('sp', lambda e: e.dma_start(out=v_[0:16, 0, :], in_=rv[(h % 4) * 128:(h % 4) * 128 + 16, 0:65]),
                          v_, R=[rv], W=[v_], join=True)
                    state[('kv', h)] = (k_, v_)
                k_, v_ = state[('kv', h)]
                q_ = Qt.bufs[j]
                S.dma('sp', lambda e: e.dma_start(out=q_[0:64, :], in_=QT[h, :, ts(j, 512)]), q_, R=[QT], W=[q_])
                S.dma('sp', lambda e: e.dma_start(out=q_[64:66, :], in_=QA[h * 8 + j, :, :]), q_, R=[QA], W=[q_], join=True)
                state[(h, j)] = (k_, v_, q_, Op.next())
                return state[(h, j)]

            def emit_qk(un):
                h, j, b0, nb_, nblk = un
                k_, v_, q_, o_ = prep_hj(h, j)
                s_ = Sp.next()
                for i in range(nb_):
                    b = b0 + i
                    lvl = b >= nblk - 16
                    S.op('pe', lambda e, b=b, i=i, lvl=lvl: e.matmul(s_[:, ts(i, 512)], lhsT=k_[:, ts(b, 128)], rhs=q_[:, :],
                                                                      start=True, stop=not lvl), R=[k_, q_], W=[s_])
                    if lvl:
                        br = b - (nblk - 16)
                        S.op('pe', lambda e, i=i, br=br: e.matmul(s_[:, ts(i, 512)], lhsT=c['identb'], rhs=mk[:, br, :],
                                                                    start=False, stop=True), R=[c['b'], mk], W=[s_])
                return s_

            sq = [emit_qk(units[0])]
            for n, un in enumerate(units):
                h, j, b0, nb_, nblk = un
                if n + 1 < len(units):
                    sq.append(emit_qk(units[n + 1]))
                k_, v_, q_, o_ = state[(h, j)]
                s_ = sq[n]
                p_ = Pt.next()
                w_ = nb_ * 512
                S.op('act', lambda e: e.activation(out=p_[:, 0:w_], in_=s_[:, 0:w_], func=AF.Exp), R=[s_], W=[p_])
                for i in range(nb_):
                    b = b0 + i
                    S.op('pe', lambda e, b=b, i=i: e.matmul(o_[0:65, :], lhsT=v_[:, b, :], rhs=p_[:, ts(i, 512)],
                                                            start=(b == 0), stop=(b == nblk - 1)), R=[v_, p_], W=[o_])
                if b0 + nb_ == nblk:
                    r_ = r65.next(); os_ = osb.next(); a_ = asb.next()
                    S.op('dve', lambda e: e.reciprocal(out=r_[64:65, :], in_=o_[64:65, :]), R=[o_], W=[r_])
                    S.op('act', lambda e: e.activation(out=os_[:, :], in_=o_[0:64, :], func=AF.Copy), R=[o_], W=[os_])
                    S.op('pe', lambda e: e.matmul(bcp[0:64, :], lhsT=E65, rhs=r_[:, :], start=True, stop=True),
                         R=[c2, r_], W=[bcp])
                    S.op('dve', lambda e: e.tensor_tensor(out=a_[:, :], in0=os_[:, :], in1=bcp[0:64, :], op=ALU.mult),
                         R=[os_, bcp], W=[a_])
                    S.dma('sp', lambda e: e.dma_start(out=AT[h // 2, (h % 2) * 64:(h % 2) * 64 + 64, ts(j, 512)],
                                                      in_=a_[:, :]), a_, R=[a_], W=[AT], join=True)
                    del state[(h, j)]
            S.barrier()

    def oproj_route(self, S, inp, ctx, gidx, dbg_out=None):
        c = self.c
        AT, H2 = inp['AT'], inp['H2']
        wo_d = inp['attn_w_out']
        with ExitStack() as es:
            R = self.route_bufs(S, es)
            self.ln_bufs(S, es, R)
            R['big'] = Ring([S.ps(es, 'big', [128, D]) for _ in range(2)])
            mm = Ring([S.ps(es, 'mm', [128, 512]) for _ in range(3)])
            wo = S.sb(es, 'wo', [128, KC, D], BF16)
            S.dma('pool', lambda e: e.dma_start(out=wo[:, :, :], in_=wo_d[0].rearrange("(kc p) f -> p kc f", p=128)),
                  wo, R=[wo_d], W=[wo])
            gb = S.sb(es, 'gb', [128, 2, D], F32)
            self.load_gb(S, gb, inp['ln_g'], inp['ln_b'], gidx)
            aT = Ring([S.sb(es, 'aT', [128, KC, 512], BF16) for _ in range(2)])
            hres = Ring([S.sb(es, 'hres', [128, D], F32) for _ in range(2)])
            acc = Ring([S.sb(es, 'acc', [128, D], F32) for _ in range(2)])
            h3 = Ring([S.sb(es, 'h3', [128, D], F32) for _ in range(2)])
            for j in range(self.NSEG):
                a_ = aT.next()
                S.dma('sp', lambda e: e.dma_start(out=a_[:, :, :], in_=AT[:, :, ts(j, 512)].rearrange("pr p t -> p pr t")),
                      a_, R=[AT], W=[a_])
                for t in range(4):
                    i = 4 * j + t
                    hr = hres.next()
                    S.dma('sp', lambda e: e.dma_start(out=hr[:, :], in_=H2[ts(i, 128), :]), hr, R=[H2], W=[hr])
                    ac = acc.next()
                    for hf in range(2):
                        p = mm.next()
                        for pr in range(KC):
                            S.op('pe', lambda e, pr=pr: e.matmul(p[:, :], lhsT=a_[:, pr, ts(t, 128)],
                                                                 rhs=wo[:, pr, ts(hf, 512)], start=(pr == 0),
                                                                 stop=(pr == KC - 1)), R=[a_, wo], W=[p])
                        S.op('dve', lambda e: e.scalar_tensor_tensor(out=ac[:, ts(hf, 512)], in0=hr[:, ts(hf, 512)],
                                                                     scalar=ALPHA, in1=p[:, :], op0=ALU.mult, op1=ALU.add),
                             R=[hr, p], W=[ac])
                    h = h3.next()
                    self.ln_tile(S, R, ac[:, :], ac, gb, h)
                    if dbg_out is None:
                        self.route_tile(S, R, i, h, ctx)
                    else:
                        S.dma('sp', lambda e: e.dma_start(out=dbg_out[ts(i, 128), :], in_=h[:, :]), h,
                              R=[h], W=[dbg_out], join=True)
            S.barrier()
            S.release([b for r in R.values() for b in r.bufs] + aT.bufs + hres.bufs + [wo, gb])


def make_consts(CAP):
    f = np.zeros((128, 800), np.float32)
    f[:, 0:128] = np.eye(128, dtype=np.float32)
    f[:, 256:288] = (np.arange(32, dtype=np.float32) * CAP - BIG)[None, :]
    for g, w in enumerate((2, 4, 8, 16)):
        t = np.arange(128)
        f[:, 288 + g * 128:288 + (g + 1) * 128] = (1.0 / np.minimum(t + 1, w))[None, :]
    b = np.zeros((128, 384), np.float32)
    b[:, 0:128] = np.eye(128)
    b[:, 128:256] = np.triu(np.ones((128, 128)), 1)
    b[:, 256:384] = 1.0
    return f, b.astype(ml_dtypes.bfloat16)


def make_xin(x, meta, core, nseg=8):
    b, cl = core // 4, core % 4
    rows = np.zeros((nseg * 528 + 144, D), np.float32)
    for j in range(nseg):
        G = cl + 4 * j
        s = G * 512
        if s == 0:
            rows[j * 528:j * 528 + 16] = meta
        else:
            rows[j * 528:j * 528 + 16] = x[b, s - 16:s]
        rows[j * 528 + 16:(j + 1) * 528] = x[b, s:s + 512]
    rows[nseg * 528 + 16:nseg * 528 + 32] = meta
    return rows


def make_valid(NT):
    v = np.ones((128, NT), np.float32)
    v[16:, NT - 1] = 0.0
    return v


def decl_weights(P, inp, layers=(0, 1)):
    nl = len(layers)
    P.wbase = layers[0]
    inp['ln_g'] = P.dt('ln_g', [4, D], F32, 'ExternalInput')
    inp['ln_b'] = P.dt('ln_b', [4, D], F32, 'ExternalInput')
    inp['router_w'] = P.dt('router_w', [2, D, 32], F32, 'ExternalInput')
    inp['router_b'] = P.dt('router_b', [2, 32], F32, 'ExternalInput')
    inp['w1'] = P.dt('w1', [nl, 32, D, 2 * D], F32, 'ExternalInput')
    inp['b1'] = P.dt('b1', [nl, 32, 2 * D], F32, 'ExternalInput')
    inp['w2'] = P.dt('w2', [nl, 32, D, D], F32, 'ExternalInput')
    inp['b2'] = P.dt('b2', [nl, 32, D], F32, 'ExternalInput')


def make_consts2():
    f = np.zeros((128, 466), np.float32)
    k = np.arange(128)
    f[k, 449 + k // 8] = 1.0
    f[k % 8 == 0, 465] = 30000.0
    f[:, 0:128] = ((k[:, None] // 8 == k[None, :] // 8) & (k[:, None] % 8 < k[None, :] % 8)).astype(np.float32)
    f[127, 128:256] = 1.0
    for p in range(128):
        h, j = p // 8, p % 8
        f[p, 256 + j * 16 + h] = 1.0
    f[16:, 384] = 30000.0
    f[64, 385:449] = 1.0
    return f


def make_qone():
    q = np.zeros((8, 32, 512), np.float32)
    for j in range(8):
        q[j, 2 + 2 * j:4 + 2 * j, :] = 1.0
    return q.astype(ml_dtypes.bfloat16)


def make_mask(cl):
    m = np.full((16, 128, 512), NEG, np.float32)
    ki = np.arange(128)[:, None]
    qi = np.arange(128)[None, :]
    for r in range(4):
        for kb in range(4):
            for qb in range(4):
                if r < cl or (r == cl and kb < qb):
                    m[r * 4 + kb, :, ts(qb, 128)] = 0.0
                elif r == cl and kb == qb:
                    m[r * 4 + kb, :, ts(qb, 128)] = np.where(ki <= qi, 0.0, NEG)
    return m.astype(ml_dtypes.bfloat16)


def make_idxq(cl):
    p = np.arange(128)
    return ((p // 8) * 34 + cl + 4 * (p % 8)).astype(np.int32).reshape(128, 1)


def build_fused(cfg):
    P = Prog(cfg)
    nc = P.nc
    CAP = P.CAP
    NTOK = 33 * 128
    inp = {}
    inp['xin'] = P.dt('xin', [cfg['NSEG'] * 528 + 144, D], F32, 'ExternalInput')
    valid_d = P.dt('valid', [128, 33], F32, 'ExternalInput')
    inp['pool_w'] = P.dt('pool_w', [1, 4, 256, 256], F32, 'ExternalInput')
    inp['pool_scale'] = P.dt('pool_scale', [1, D], F32, 'ExternalInput')
    inp['attn_w_in'] = P.dt('attn_w_in', [1, D, 3088], F32, 'ExternalInput')
    inp['attn_b_f'] = P.dt('attn_b_f', [1, 16], F32, 'ExternalInput')
    inp['attn_w_out'] = P.dt('attn_w_out', [1, D, D], F32, 'ExternalInput')
    inp['maskT'] = P.dt('maskT', [16, 128, 512], BF16, 'ExternalInput')
    inp['idxq'] = P.dt('idxq', [128, 1], I32, 'ExternalInput')
    inp['cst2'] = P.dt('cst2', [128, 466], F32, 'ExternalInput')
    inp['qone'] = P.dt('qone', [8, 32, 512], BF16, 'ExternalInput')
    inp['KA'] = P.dt('KAs', [16, 8, 16, 2176], BF16, 'Internal')
    decl_weights(P, inp, layers=(0, 1))
    inp['H'] = P.dt('Hs', [NTOK, D], F32, 'Internal')
    inp['XS'] = P.dt('XS', [32 * CAP, D], BF16, 'Internal')
    inp['YS'] = P.dt('YS', [32 * CAP, D], F32, 'Internal')
    inp['H2'] = P.dt('H2s', [NTOK, D], F32, 'Internal')
    inp['QT'] = P.dt('QTs', [16, 64, NTOK], BF16, 'Internal')
    SNDK, SNDV, RCVK, RCVV = [], [], [], []
    for sg in range(9):
        wk = 512 if sg < 8 else 128
        wv = 260 if sg < 8 else 65
        SNDK.append([P.dt('SK%d_%d' % (sg, i), [512, wk], BF16, 'Internal') for i in range(2)])
        RCVK.append([P.dt('RK%d_%d' % (sg, i), [2048, wk], BF16, 'Internal') for i in range(2)])
        SNDV.append([P.dt('SV%d_%d' % (sg, i), [512, wv], BF16, 'Internal') for i in range(4)])
        RCVV.append([P.dt('RV%d_%d' % (sg, i), [2048, wv], BF16, 'Internal') for i in range(4)])
    SNDL = P.dt('SNDL', [16, NTOK], F32, 'Internal')
    inp['RCVK'] = RCVK
    inp['RCVV'] = RCVV
    inp['RCVL'] = P.dt('RCVL', [64, NTOK], F32, 'Internal')
    inp['LFT'] = P.dt('LFT', [16, 17408], F32, 'Internal')
    inp['AT'] = P.dt('ATs', [8, 128, 4096], BF16, 'Internal')
    inp['CALL'] = P.dt('CALL', [16, 17408], F32, 'Internal')
    inp['QA'] = P.dt('QAs', [128, 2, 512], BF16, 'Internal')
    out = P.dt('out', [32 * 128, D], F32, 'ExternalOutput')
    outs = dict(H2=inp['H2'], QT=inp['QT'], KT=SNDK, V=SNDV, LF=SNDL)
    groups = [[0, 1, 2, 3], [4, 5, 6, 7]]
    with ExitStack() as es:
        S = Sch(nc, es, n_dsem=96)
        P.consts(S, es)

        deferred = {'B': [], 'C': [], 'D': []}

        def issue(pairs):
            ds = S.dfree.pop()
            ds.nobar = True
            for (a_, d_) in pairs:
                d_.dsem = ds
                S.coll("AllGather", groups, a_, d_)
            for (a_, d_) in pairs:
                d_.w = {id(ds.sem): (ds.sem, ds.cnt, 'dma')}

        def exchange(sg):
            if sg < 0:
                S.coll("AllGather", groups, SNDL, inp['RCVL'])
                return
            issue([(SNDK[sg][0], RCVK[sg][0]), (SNDV[sg][0], RCVV[sg][0])])
            deferred['B'].append((SNDV[sg][1], RCVV[sg][1]))
            deferred['C'].append((SNDK[sg][1], RCVK[sg][1]))
            deferred['C'].append((SNDV[sg][2], RCVV[sg][2]))
            deferred['D'].append((SNDV[sg][3], RCVV[sg][3]))
        outs['exchange'] = exchange
        inp['valid_sb'] = S.sb(es, 'valid', [128, 33], F32)
        S.dma('sp', lambda e: e.dma_start(out=inp['valid_sb'][:, :], in_=valid_d[:, :]), inp['valid_sb'],
              R=[valid_d], W=[inp['valid_sb']])
        P.NT = 33
        ctx0 = P.moe_ctx(S, es, 0, inp)
        P.mixer0(S, inp, ctx0, 0)
        P.experts(S, inp, ctx0, 0)
        P.combine_qkv(S, inp, ctx0, 1, outs)
        P.NT = 32
        def rest_of_exchange():
            for g_ in ('B', 'C', 'D'):
                issue(deferred[g_])
        P.attention(S, inp, es, after_init=rest_of_exchange)
        ctx1 = P.moe_ctx(S, es, 1, inp)
        P.oproj_route(S, inp, ctx1, 2)
        P.experts(S, inp, ctx1, 1)
        with ExitStack() as es2:
            R = P.combine_bufs(S, es2)
            gb = S.sb(es2, 'gb2', [128, 2, D], F32)
            P.load_gb(S, gb, inp['ln_g'], inp['ln_b'], 3)
            ho = Ring([S.sb(es2, 'ho', [128, D], F32) for _ in range(2)])
            for i in range(32):
                h = ho.next()
                P.combine_tile(S, R, i, ctx1, gb, h)
                S.dma('sp', lambda e, h=h, i=i: e.dma_start(out=out[ts(i, 128), :], in_=h[:, :]), h,
                      R=[h], W=[out], join=True)
            S.barrier()
    return P


CFG = dict(NT=33, NSEG=8, CAP=768)


def make_maps(inputs):
    cf, cb = make_consts(CFG['CAP'])
    c2 = make_consts2()
    maps = []
    for c in range(8):
        cl = c % 4
        maps.append(dict(
            xin=make_xin(inputs['x'], inputs['meta_tokens'], c), valid=make_valid(33), cst_f=cf, cst_b=cb, cst2=c2,
            maskT=make_mask(cl), idxq=make_idxq(cl), qone=make_qone(),
            pool_w=inputs['pool_w'], pool_scale=inputs['pool_scale'], ln_g=inputs['ln_g'].reshape(4, D),
            ln_b=inputs['ln_b'].reshape(4, D), router_w=inputs['router_w'], router_b=inputs['router_b'],
            w1=inputs['w1'], b1=inputs['b1'], w2=inputs['w2'], b2=inputs['b2'],
            attn_w_in=inputs['attn_w_in'], attn_b_f=inputs['attn_b_f'], attn_w_out=inputs['attn_w_out']))
    return maps


def assemble(res):
    out = np.zeros((2, SEQ, D), np.float32)
    for c in range(8):
        b, cl = c // 4, c % 4
        o = np.asarray(res[c]['out'], dtype=np.float32)
        for j in range(8):
            G = cl + 4 * j
            out[b, G * 512:(G + 1) * 512] = o[j * 512:(j + 1) * 512]
    return out


def kernel(**inputs):
    inputs = {k: np.ascontiguousarray(np.asarray(v)) for k, v in inputs.items()}
    P = build_fused(CFG)
    res = run_bass_kernel_spmd(P.nc, make_maps(inputs), core_ids=list(range(8))).results
    return assemble(res)
```

```python
import numpy as np
import ml_dtypes
from contextlib import ExitStack
import concourse.bass as bass
import concourse.mybir as mybir
from concourse.bass_utils import run_bass_kernel_spmd

F32 = mybir.dt.float32
BF16 = mybir.dt.bfloat16
I32 = mybir.dt.int32
ALU = mybir.AluOpType
AF = mybir.ActivationFunctionType

D = 1024
KC = 8
NE = 32
NH = 16
HD = 64
N_META = 16
SEQ = 16384
ALPHA = float((2 * 2) ** 0.25)
LN_EPS = 1e-5
BIG = float(2 ** 20)
NEG = -30000.0
CC_INC = 1


def ts(i, n):
    return slice(i * n, (i + 1) * n)


class DSem:
    def __init__(self, sem):
        self.sem = sem
        self.cnt = 0


class Buf:
    def __init__(self, name, t=None):
        self.name = name
        self.t = t
        self.w = {}
        self.r = {}
        self.dsem = None

    def __getitem__(self, idx):
        return self.t[idx]


class Sch:
    def __init__(self, nc, es, n_dsem=96):
        self.nc = nc
        self.es = es
        self.E = {'pe': nc.tensor, 'dve': nc.vector, 'act': nc.scalar, 'pool': nc.gpsimd, 'sp': nc.sync}
        self.sem = {k: es.enter_context(nc.semaphore('s_' + k)) for k in self.E}
        self.cnt = {k: 0 for k in self.E}
        self.seen = {k: {} for k in self.E}
        self.dpool = [DSem(es.enter_context(nc.semaphore('d%d' % i))) for i in range(n_dsem)]
        self.dfree = list(self.dpool)
        self.nbuf = 0

    def sb(self, es, name, shape, dt):
        self.nbuf += 1
        t = es.enter_context(self.nc.sbuf_tensor('%s_%d' % (name, self.nbuf), list(shape), dt))
        return Buf(name, t)

    def ps(self, es, name, shape, dt=F32):
        self.nbuf += 1
        t = es.enter_context(self.nc.psum_tensor('%s_%d' % (name, self.nbuf), list(shape), dt))
        return Buf(name, t)

    def view(self, name):
        return Buf(name)

    def _wait(self, eng, toks):
        best = {}
        for (sem, val, owner) in toks:
            if owner == 'pe' and eng == 'pe':
                continue
            k = id(sem)
            if self.seen[eng].get(k, 0) >= val:
                continue
            if k not in best or best[k][1] < val:
                best[k] = (sem, val)
        for k, (sem, val) in best.items():
            self.E[eng].wait_ge(sem, val)
            self.seen[eng][k] = val

    @staticmethod
    def _deps(reads, writes, skip_w=None):
        toks = []
        for b in reads:
            toks.extend(b.w.values())
        for b in writes:
            if b is not skip_w:
                toks.extend(b.w.values())
            toks.extend(b.r.values())
        return toks

    @staticmethod
    def _rec(tok, reads, writes, join=False):
        k = id(tok[0])
        for b in writes:
            if join:
                b.w[k] = tok
            else:
                b.w = {k: tok}
                b.r = {}
        for b in reads:
            b.r[k] = tok

    def op(self, eng, fn, R=(), W=()):
        self._wait(eng, self._deps(R, W))
        ins = fn(self.E[eng])
        self.cnt[eng] += 1
        ins.then_inc(self.sem[eng], 1)
        self._rec((self.sem[eng], self.cnt[eng], eng), R, W)
        return ins

    def _dsem_of(self, b):
        if b.dsem is None:
            b.dsem = self.dfree.pop()
        return b.dsem

    def dma(self, q, fn, sbuf, R=(), W=(), join=False):
        ds = self._dsem_of(sbuf)
        toks = self._deps(R, W)
        if join:
            toks = [t for t in toks if t[2] != 'dma']
        self._wait(q, toks)
        ins = fn(self.E[q])
        ds.cnt += 16
        ins.then_inc(ds.sem, 16)
        self._rec((ds.sem, ds.cnt, 'dma'), R, W, join=join)
        return ins

    def coll(self, kind, groups, src, dst, src_ap=None, dst_ap=None):
        ds = self._dsem_of(dst)
        self._wait('pool', self._deps([src], [dst]))
        ins = self.nc.gpsimd.collective_compute(kind, ALU.bypass, replica_groups=groups,
                                                ins=[src.t if src_ap is None else src_ap],
                                                outs=[dst.t if dst_ap is None else dst_ap])
        ds.cnt += CC_INC
        ins.then_inc(ds.sem, CC_INC)
        self._rec((ds.sem, ds.cnt, 'dma'), [src], [dst])
        return ins

    def release(self, bufs):
        for b in bufs:
            if b.dsem is not None:
                self.dfree.append(b.dsem)
                b.dsem = None

    def barrier(self):
        toks = [(self.sem[k], self.cnt[k], k + '_b') for k in self.E if self.cnt[k] > 0]
        toks += [(d.sem, d.cnt, 'dma') for d in self.dpool if d.cnt > 0 and not getattr(d, 'nobar', False)]
        for e in self.E:
            self._wait(e, toks)


class Ring:
    def __init__(self, bufs):
        self.bufs = bufs
        self.i = -1

    def next(self):
        self.i = (self.i + 1) % len(self.bufs)
        return self.bufs[self.i]


class Prog:
    def __init__(self, cfg):
        self.cfg = cfg
        self.NT = cfg['NT']
        self.NSEG = cfg['NSEG']
        self.CAP = cfg['CAP']
        self.CT = self.CAP // 128
        self.NE = cfg.get('NE', NE)
        self.nc = bass.Bass("TRN2", target_bir_lowering=False)
        self.dram = {}

    def dt(self, name, shape, dtype, kind):
        t = self.nc.dram_tensor(name, list(shape), dtype, kind=kind)
        self.dram[name] = t
        b = Buf(name, t.ap())
        return b

    def consts(self, S, es):
        c = {}
        cin = self.dt('cst_f', [128, 128 * 2 + 32 + 128 * 4], F32, 'ExternalInput')
        cb = self.dt('cst_b', [128, 128 * 3], BF16, 'ExternalInput')
        c['f'] = S.sb(es, 'cst_f', [128, 128 * 2 + 32 + 512], F32)
        c['b'] = S.sb(es, 'cst_b', [128, 384], BF16)
        S.dma('sp', lambda e: e.dma_start(out=c['f'][:], in_=cin[:, :]), c['f'], R=[cin], W=[c['f']])
        S.dma('sp', lambda e: e.dma_start(out=c['b'][:], in_=cb[:, :]), c['b'], R=[cb], W=[c['b']])
        c['ident'] = c['f'].t[:, 0:128]
        c['ecol'] = c['f'].t[:, 256:288]
        c['rcnt'] = c['f'].t[:, 288:800]
        c['identb'] = c['b'].t[:, 0:128]
        c['U'] = c['b'].t[:, 128:256]
        c['ones'] = c['b'].t[:, 256:384]
        self.c = c
        return c

    def ln_tile(self, S, R, src_ap, src_buf, gb, out_buf):
        st = R['st'].next(); mv = R['mv'].next(); sd = R['sd'].next(); hn = R['hn'].next()
        for k in range(2):
            S.op('dve', lambda e, k=k: e.bn_stats(out=st[:, ts(k, 6)], in_=src_ap[:, ts(k, 512)]), R=[src_buf], W=[st])
        S.op('dve', lambda e: e.bn_aggr(out=mv[:, :], in_=st[:, :]), R=[st], W=[mv])
        S.op('dve', lambda e: e.tensor_scalar(out=sd[:, 0:1], in0=mv[:, 1:2], scalar1=LN_EPS, scalar2=None,
                                              op0=ALU.add), R=[mv], W=[sd])
        S.op('act', lambda e: e.activation(out=sd[:, 1:2], in_=sd[:, 0:1], func=AF.Sqrt), R=[sd], W=[sd])
        S.op('dve', lambda e: e.reciprocal(out=sd[:, 2:3], in_=sd[:, 1:2]), R=[sd], W=[sd])
        S.op('dve', lambda e: e.tensor_scalar(out=hn[:, :], in0=src_ap, scalar1=mv[:, 0:1], scalar2=sd[:, 2:3],
                                              op0=ALU.subtract, op1=ALU.mult), R=[src_buf, mv, sd], W=[hn])
        S.op('pool', lambda e: e.tensor_tensor(out=hn[:, :], in0=hn[:, :], in1=gb[:, 0, :], op=ALU.mult),
             R=[hn, gb], W=[hn])
        S.op('dve', lambda e: e.tensor_tensor(out=out_buf[:, :], in0=hn[:, :], in1=gb[:, 1, :], op=ALU.add),
             R=[hn, gb], W=[out_buf])

    def load_gb(self, S, gbuf, lng, lnb, idx):
        S.dma('sp', lambda e: e.dma_start(out=gbuf[:, 0, :], in_=lng[idx:idx + 1, :].to_broadcast([128, D])),
              gbuf, R=[lng], W=[gbuf])
        S.dma('sp', lambda e: e.dma_start(out=gbuf[:, 1, :], in_=lnb[idx:idx + 1, :].to_broadcast([128, D])),
              gbuf, R=[lnb], W=[gbuf], join=True)

    def route_tile(self, S, R, i, h_buf, ctx):
        c = self.c
        CAP = self.CAP
        H, XS = ctx['H'], ctx['XS']
        dest4, gate4, valid = ctx['dest4'], ctx['gate4'], ctx['valid']
        rw, rbb = ctx['rw'], ctx['rbb']
        S.dma('sp', lambda e: e.dma_start(out=H[ts(i, 128), :], in_=h_buf[:, :]), h_buf, R=[h_buf], W=[H], join=True)
        hb = R['hb'].next()
        S.op('act', lambda e: e.activation(out=hb[:, :], in_=h_buf[:, :], func=AF.Copy), R=[h_buf], W=[hb])
        tp = R['big'].next()
        for kc in range(KC):
            S.op('pe', lambda e, kc=kc: e.transpose(out=tp[:, ts(kc, 128)], in_=h_buf[:, ts(kc, 128)],
                                                    identity=c['ident']), R=[h_buf, c['f']], W=[tp])
        hT = R['hT'].next()
        S.op('act', lambda e: e.activation(out=hT[:, :], in_=tp[:, :], func=AF.Copy), R=[tp], W=[hT])
        lgp = R['lgp'].next()
        for kc in range(KC):
            S.op('pe', lambda e, kc=kc: e.matmul(lgp[:, 0:32], lhsT=hT[:, ts(kc, 128)], rhs=rw[:, kc, :],
                                                 start=(kc == 0), stop=(kc == KC - 1)), R=[hT, rw], W=[lgp])
        sm = R['sm'].next()
        lg = sm[:, 0:32]; top8 = sm[:, 32:40]
        S.op('dve', lambda e: e.tensor_tensor(out=lg, in0=lgp[:, 0:32], in1=rbb[:, :], op=ALU.add), R=[lgp, rbb], W=[sm])
        S.op('dve', lambda e: e.max(out=top8, in_=lg), R=[sm], W=[sm])
        S.op('dve', lambda e: e.tensor_scalar(out=sm[:, 40:41], in0=sm[:, 32:33], scalar1=-1.0, scalar2=None,
                                              op0=ALU.mult), R=[sm], W=[sm])
        S.op('act', lambda e: e.activation(out=sm[:, 44:48], in_=sm[:, 32:36], func=AF.Exp, bias=sm[:, 40:41],
                                           scale=1.0), R=[sm], W=[sm])
        S.op('dve', lambda e: e.tensor_reduce(out=sm[:, 41:42], in_=sm[:, 44:48], axis=mybir.AxisListType.X,
                                              op=ALU.add), R=[sm], W=[sm])
        S.op('dve', lambda e: e.reciprocal(out=sm[:, 42:43], in_=sm[:, 41:42]), R=[sm], W=[sm])
        mb = R['mb'].next()
        S.op('dve', lambda e: e.tensor_scalar(out=mb[:, :], in0=lg, scalar1=sm[:, 35:36], scalar2=valid[:, i:i + 1],
                                              op0=ALU.is_ge, op1=ALU.mult), R=[sm, valid], W=[mb])
        p12 = lgp
        S.op('pe', lambda e: e.matmul(p12[:, 32:64], lhsT=c['U'], rhs=mb[:, :], start=True, stop=True),
             R=[mb, c['b']], W=[p12])
        S.op('pe', lambda e: e.matmul(p12[:, 64:96], lhsT=c['ones'], rhs=mb[:, :], start=True, stop=True),
             R=[mb, c['b']], W=[p12])
        cnt_old = ctx['cnt'][ctx['cnt_i'] % 2]
        cnt_new = ctx['cnt'][(ctx['cnt_i'] + 1) % 2]
        ctx['cnt_i'] += 1
        S.op('dve', lambda e: e.tensor_tensor(out=sm[:, 64:96], in0=p12[:, 32:64], in1=cnt_old[:, :], op=ALU.add),
             R=[p12, cnt_old], W=[sm])
        S.op('dve', lambda e: e.tensor_tensor(out=cnt_new[:, :], in0=p12[:, 64:96], in1=cnt_old[:, :], op=ALU.add),
             R=[p12, cnt_old], W=[cnt_new])
        S.op('dve', lambda e: e.tensor_scalar(out=sm[:, 96:128], in0=sm[:, 64:96], scalar1=float(CAP), scalar2=None,
                                              op0=ALU.is_lt), R=[sm], W=[sm])
        S.op('dve', lambda e: e.tensor_tensor(out=sm[:, 128:160], in0=sm[:, 64:96], in1=c['ecol'], op=ALU.add),
             R=[sm, c['f']], W=[sm])
        S.op('dve', lambda e: e.scalar_tensor_tensor(out=sm[:, 160:192], in0=sm[:, 128:160], scalar=valid[:, i:i + 1],
                                                     in1=sm[:, 96:128], op0=ALU.mult, op1=ALU.mult),
             R=[sm, valid], W=[sm])
        dk = R['dk'].next()
        for k in range(4):
            S.op('dve', lambda e, k=k: e.scalar_tensor_tensor(out=sm[:, 192:224], in0=lg, scalar=sm[:, 32 + k:33 + k],
                                                              in1=sm[:, 160:192], op0=ALU.is_equal, op1=ALU.mult,
                                                              accum_out=dk[:, k:k + 1]), R=[sm], W=[sm, dk])
        S.op('dve', lambda e: e.tensor_scalar(out=dk[:, 4:8], in0=dk[:, 0:4], scalar1=-0.5, scalar2=None,
                                              op0=ALU.is_lt), R=[dk], W=[dk])
        S.op('dve', lambda e: e.scalar_tensor_tensor(out=gate4[:, 4 * i:4 * i + 4], in0=sm[:, 44:48], scalar=sm[:, 42:43],
                                                     in1=dk[:, 4:8], op0=ALU.mult, op1=ALU.mult),
             R=[sm, dk], W=[gate4])
        S.op('dve', lambda e: e.tensor_scalar(out=dest4[:, 4 * i:4 * i + 4], in0=dk[:, 0:4], scalar1=BIG, scalar2=None,
                                              op0=ALU.add), R=[dk], W=[dest4])
        for k in range(4):
            S.dma('pool', lambda e, k=k: e.indirect_dma_start(
                out=XS[:, :], out_offset=bass.IndirectOffsetOnAxis(ap=dest4[:, 4 * i + k:4 * i + k + 1], axis=0),
                in_=hb[:, :], in_offset=None, bounds_check=self.bc_reg, oob_is_err=False),
                hb, R=[hb, dest4], W=[XS], join=True)

    def route_bufs(self, S, es):
        R = {}
        R['hb'] = Ring([S.sb(es, 'hb', [128, D], BF16) for _ in range(2)])
        R['hT'] = Ring([S.sb(es, 'hT', [128, D], F32) for _ in range(2)])
        R['sm'] = Ring([S.sb(es, 'sm', [128, 256], F32) for _ in range(2)])
        R['mb'] = Ring([S.sb(es, 'mb', [128, 32], BF16) for _ in range(2)])
        R['dk'] = Ring([S.sb(es, 'dk', [128, 8], F32) for _ in range(2)])
        R['lgp'] = Ring([S.ps(es, 'lgp', [128, 512])])
        return R

    def ln_bufs(self, S, es, R):
        R['st'] = Ring([S.sb(es, 'st', [128, 12], F32) for _ in range(2)])
        R['mv'] = Ring([S.sb(es, 'mv', [128, 2], F32) for _ in range(2)])
        R['sd'] = Ring([S.sb(es, 'sd', [128, 4], F32) for _ in range(2)])
        R['hn'] = Ring([S.sb(es, 'hn', [128, D], F32) for _ in range(2)])
        return R

    def moe_ctx(self, S, es, layer, inp):
        ctx = {}
        ctx['dest4'] = S.sb(es, 'dest4', [128, self.NT * 4], I32)
        ctx['gate4'] = S.sb(es, 'gate4', [128, self.NT * 4], F32)
        ctx['cnt'] = [S.sb(es, 'cnt', [128, 32], F32) for _ in range(2)]
        ctx['cnt_i'] = 0
        S.op('dve', lambda e: e.memset(ctx['cnt'][0][:, :], 0.0), W=[ctx['cnt'][0]])
        ctx['rw'] = S.sb(es, 'rw', [128, KC, 32], F32)
        ctx['rbb'] = S.sb(es, 'rbb', [128, 32], F32)
        rw_d, rb_d = inp['router_w'], inp['router_b']
        S.dma('sp', lambda e: e.dma_start(out=ctx['rw'][:, :, :],
                                          in_=rw_d[layer].rearrange("(kc p) e -> p kc e", p=128)),
              ctx['rw'], R=[rw_d], W=[ctx['rw']])
        S.dma('sp', lambda e: e.dma_start(out=ctx['rbb'][:, :], in_=rb_d[layer:layer + 1, :].to_broadcast([128, 32])),
              ctx['rbb'], R=[rb_d], W=[ctx['rbb']])
        ctx['valid'] = inp['valid_sb']
        if not hasattr(self, 'bc_reg'):
            self.bc_reg = self.nc.gpsimd.to_reg(self.NE * self.CAP - 1)
        ctx['H'] = inp['H']; ctx['XS'] = inp['XS']; ctx['YS'] = inp['YS']
        return ctx

    def mixer0(self, S, inp, ctx, gidx):
        nc = self.nc
        c = self.c
        xin = inp['xin']
        with ExitStack() as es:
            R = self.route_bufs(S, es)
            self.ln_bufs(S, es, R)
            R['big'] = Ring([S.ps(es, 'big', [128, D]) for _ in range(2)])
            mm = Ring([S.ps(es, 'mm', [128, 512]) for _ in range(2)])
            tph = S.ps(es, 'tph', [128, KC, 16])
            xt = Ring([S.sb(es, 'xt', [128, 4, D], F32) for _ in range(2)])
            xh = Ring([S.sb(es, 'xh', [16, D], F32) for _ in range(2)])
            xT = S.sb(es, 'xT', [128, KC, 528], F32)
            xTk = [S.view('xT%d' % k) for k in range(KC)]
            sA = Ring([S.sb(es, 'sA', [128, 528], F32) for _ in range(2)])
            sB = Ring([S.sb(es, 'sB', [128, 528], F32) for _ in range(2)])
            uT = S.sb(es, 'uT', [128, KC, 512], BF16)
            uTk = [S.view('uT%d' % k) for k in range(KC)]
            rT = S.sb(es, 'rT', [128, KC, 512], F32)
            rTk = [S.view('rT%d' % k) for k in range(KC)]
            tmp = Ring([S.sb(es, 'tmp', [128, 512], F32) for _ in range(2)])
            h1 = Ring([S.sb(es, 'h1', [128, D], F32) for _ in range(2)])
            pw = S.sb(es, 'pw', [128, 4, 2, 256], BF16)
            psc = S.sb(es, 'psc', [128, KC], F32)
            gb = S.sb(es, 'gb', [128, 2, D], F32)
            pw_d, ps_d = inp['pool_w'], inp['pool_scale']
            S.dma('pool', lambda e: e.dma_start(out=pw[:, :, :, :],
                                                in_=pw_d[0].rearrange("g (ic p) d -> p g ic d", p=128)),
                  pw, R=[pw_d], W=[pw])
            with nc.allow_non_contiguous_dma(reason="tiny per-partition vector"):
                S.dma('sp', lambda e: e.dma_start(out=psc[:, :], in_=ps_d[0].rearrange("(kc p) -> p kc", p=128)),
                      psc, R=[ps_d], W=[psc])
            self.load_gb(S, gb, inp['ln_g'], inp['ln_b'], gidx)

            for seg in range(self.NSEG + 1):
                meta = (seg == self.NSEG)
                ntile = 1 if meta else 4
                W = ntile * 128
                base = seg * 528
                x_t = xt.next(); x_h = xh.next()
                S.dma('sp', lambda e: e.dma_start(out=x_h[:, :], in_=xin[base:base + 16, :]), x_h, R=[xin], W=[x_h])
                S.dma('sp', lambda e: e.dma_start(
                    out=x_t[:, 0:ntile, :], in_=xin[base + 16:base + 16 + W, :].rearrange("(t p) d -> p t d", p=128)),
                    x_t, R=[xin], W=[x_t])
                for kc in range(KC):
                    S.op('pe', lambda e, kc=kc: e.transpose(out=tph[:, kc, :], in_=x_h[:, ts(kc, 128)],
                                                            identity=c['ident'][0:16, 0:16]), R=[x_h, c['f']], W=[tph])
                for kc in range(KC):
                    S.op('act', lambda e, kc=kc: e.activation(out=xT[:, kc, 0:16], in_=tph[:, kc, :], func=AF.Copy),
                         R=[tph], W=[xTk[kc]])
                for kc in range(KC):
                    p = mm.next()
                    for t in range(ntile):
                        S.op('pe', lambda e, kc=kc, t=t: e.transpose(out=p[:, ts(t, 128)], in_=x_t[:, t, ts(kc, 128)],
                                                                     identity=c['ident']), R=[x_t, c['f']], W=[p])
                    S.op('act', lambda e, kc=kc: e.activation(out=xT[:, kc, 16:16 + W], in_=p[:, 0:W], func=AF.Copy),
                         R=[p], W=[xTk[kc]])
                for kc in range(KC):
                    g = kc // 2
                    w = 2 << g
                    cur_ap = xT[:, kc, :]
                    cur_buf = xTk[kc]
                    lo = 16 - (w - 1)
                    sh = 1
                    start = 1
                    bufs = [sA.next(), sB.next()]
                    bi = 0
                    first = 0
                    while sh < w:
                        first = first + sh
                        nb = bufs[bi]; bi ^= 1
                        S.op('dve', lambda e, nb=nb, cur_ap=cur_ap, first=first, sh=sh, W=W: e.tensor_tensor(
                            out=nb[:, first:16 + W], in0=cur_ap[:, first:16 + W], in1=cur_ap[:, first - sh:16 + W - sh],
                            op=ALU.add), R=[cur_buf], W=[nb])
                        cur_ap = nb[:, :]; cur_buf = nb
                        sh *= 2
                    if not meta:
                        S.op('dve', lambda e, kc=kc, cur_ap=cur_ap, w=w, W=W: e.scalar_tensor_tensor(
                            out=uT[:, kc, 0:W], in0=cur_ap[:, 16:16 + W], scalar=1.0 / w, in1=xT[:, kc, 16:16 + W],
                            op0=ALU.mult, op1=ALU.subtract), R=[cur_buf, xTk[kc]], W=[uTk[kc]])
                    else:
                        nb = bufs[bi]
                        S.op('dve', lambda e, nb=nb, cur_ap=cur_ap, g=g, W=W: e.tensor_tensor(
                            out=nb[:, 16:16 + W], in0=cur_ap[:, 16:16 + W], in1=c['rcnt'][:, ts(g, 128)], op=ALU.mult),
                            R=[cur_buf, c['f']], W=[nb])
                        S.op('dve', lambda e, nb=nb, kc=kc, W=W: e.tensor_tensor(
                            out=uT[:, kc, 0:W], in0=nb[:, 16:16 + W], in1=xT[:, kc, 16:16 + W], op=ALU.subtract),
                            R=[nb, xTk[kc]], W=[uTk[kc]])
                for oc8 in range(KC):
                    g = oc8 // 2
                    oc = oc8 % 2
                    p = mm.next()
                    for ic in range(2):
                        S.op('pe', lambda e, g=g, oc=oc, ic=ic, p=p, W=W: e.matmul(
                            p[:, 0:W], lhsT=pw[:, g, ic, ts(oc, 128)], rhs=uT[:, 2 * g + ic, 0:W],
                            start=(ic == 0), stop=(ic == 1)), R=[pw, uTk[2 * g + ic]], W=[p])
                    tm = tmp.next()
                    S.op('act', lambda e, p=p, tm=tm, oc8=oc8, W=W: e.activation(
                        out=tm[:, 0:W], in_=p[:, 0:W], func=AF.Copy, scale=psc[:, oc8:oc8 + 1]), R=[p, psc], W=[tm])
                    S.op('dve', lambda e, tm=tm, oc8=oc8, W=W: e.scalar_tensor_tensor(
                        out=rT[:, oc8, 0:W], in0=xT[:, oc8, 16:16 + W], scalar=ALPHA, in1=tm[:, 0:W],
                        op0=ALU.mult, op1=ALU.add), R=[xTk[oc8], tm], W=[rTk[oc8]])
                for t in range(ntile):
                    i = seg * 4 + t
                    rp = R['big'].next()
                    for kc in range(KC):
                        S.op('pe', lambda e, kc=kc, t=t, rp=rp: e.transpose(
                            out=rp[:, ts(kc, 128)], in_=rT[:, kc, ts(t, 128)], identity=c['ident']),
                            R=[rTk[kc], c['f']], W=[rp])
                    h = h1.next()
                    self.ln_tile(S, R, rp[:, :], rp, gb, h)
                    self.route_tile(S, R, i, h, ctx)
            S.barrier()
            S.release([b for r in R.values() for b in r.bufs] + xt.bufs + xh.bufs + [pw, psc, gb])

    def experts(self, S, inp, ctx, layer):
        nc = self.nc
        layer = layer - getattr(self, 'wbase', 0)
        c = self.c
        CAP, CT = self.CAP, self.CT
        XS, YS = ctx['XS'], ctx['YS']
        w1_d, w2_d, b1_d, b2_d = inp['w1'], inp['w2'], inp['b1'], inp['b2']
        cgs = []
        o = 0
        while o < CAP:
            n = min(512, CAP - o)
            cgs.append((o, n))
            o += n
        with ExitStack() as es:
            tpb = Ring([S.ps(es, 'tpb', [128, KC, 128], BF16) for _ in range(2)])
            hp = Ring([S.ps(es, 'hp', [128, 512]) for _ in range(4)])
            yp = Ring([S.ps(es, 'yp', [128, 512]) for _ in range(2)])
            b1T = S.sb(es, 'b1T', [128, 16, 32], F32)
            with ExitStack() as esb:
                b1r = S.sb(esb, 'b1r', [32, 2 * D], F32)
                S.dma('sp', lambda e: e.dma_start(out=b1r[:, :], in_=b1_d[layer]), b1r, R=[b1_d], W=[b1r])
                for fc in range(16):
                    p = yp.next()
                    S.op('pe', lambda e, fc=fc, p=p: e.transpose(out=p[:, 0:32], in_=b1r[:, ts(fc, 128)],
                                                                 identity=c['ident'][0:32, 0:32]), R=[b1r, c['f']], W=[p])
                    S.op('dve', lambda e, fc=fc, p=p: e.tensor_copy(out=b1T[:, fc, :], in_=p[:, 0:32]), R=[p], W=[b1T])
                S.barrier()
                S.release([b1r])
            w1b = Ring([S.sb(es, 'w1b', [128, KC, 2 * D], BF16) for _ in range(2)])
            w2b = Ring([S.sb(es, 'w2b', [128, KC, D], BF16) for _ in range(2)])
            b2b = Ring([S.sb(es, 'b2b', [128, D], F32) for _ in range(2)])
            xst = Ring([S.sb(es, 'xst', [128, CT, D], BF16) for _ in range(2)])
            xeT = Ring([S.sb(es, 'xeT', [128, KC, CAP], BF16) for _ in range(2)])
            aT = S.sb(es, 'aT', [128, KC, CAP], BF16)
            aTk = [S.view('aT%d' % k) for k in range(KC)]
            ys = Ring([S.sb(es, 'ys', [128, D], F32) for _ in range(2)])
            gc = Ring([S.sb(es, 'gc', [128, 512], F32) for _ in range(3)])
            sg = Ring([S.sb(es, 'sg', [128, 512], F32) for _ in range(3)])
            u0 = Ring([S.sb(es, 'u0', [128, 512], F32) for _ in range(3)])

            def load_w(ex):
                a = w1b.next(); b = w2b.next(); bb = b2b.next()
                for hlf in range(2):
                    S.dma('pool', lambda e, hlf=hlf: e.dma_start(
                        out=a[:, ts(hlf, 4), :],
                        in_=w1_d[layer, ex, ts(hlf, 512), :].rearrange("(kc p) f -> p kc f", p=128)),
                        a, R=[w1_d], W=[a], join=(hlf == 1))
                S.dma('pool', lambda e: e.dma_start(
                    out=b[:, :, :], in_=w2_d[layer, ex].rearrange("(kc p) f -> p kc f", p=128)), b, R=[w2_d], W=[b])
                S.dma('sp', lambda e: e.dma_start(out=bb[:, :], in_=b2_d[layer, ex:ex + 1, :].to_broadcast([128, D])),
                      bb, R=[b2_d], W=[bb])
                return a, b, bb

            def load_x(ex):
                xs_ = xst.next()
                S.dma('sp', lambda e: e.dma_start(
                    out=xs_[:, :, :], in_=XS[ex * CAP:(ex + 1) * CAP, :].rearrange("(ct p) d -> p ct d", p=128)),
                    xs_, R=[XS], W=[xs_])
                return xs_

            nxt_w = load_w(0)
            nxt_x = load_x(0)
            for ex in range(self.NE):
                wa, wb_, bb = nxt_w
                xs_ = nxt_x
                if ex + 1 < self.NE:
                    nxt_w = load_w(ex + 1)
                    nxt_x = load_x(ex + 1)
                xT_ = xeT.next()
                for ct in range(CT):
                    p = tpb.next()
                    for kc in range(KC):
                        S.op('pe', lambda e, ct=ct, kc=kc, p=p: e.transpose(
                            out=p[:, kc, :], in_=xs_[:, ct, ts(kc, 128)], identity=c['identb']),
                            R=[xs_, c['b']], W=[p])
                    eng = 'act' if ct % 2 == 0 else 'dve'
                    if eng == 'act':
                        S.op('act', lambda e, ct=ct, p=p: e.activation(out=xT_[:, :, ts(ct, 128)], in_=p[:, :, :],
                                                                        func=AF.Copy), R=[p], W=[xT_])
                    else:
                        S.op('dve', lambda e, ct=ct, p=p: e.tensor_copy(out=xT_[:, :, ts(ct, 128)], in_=p[:, :, :]),
                             R=[p], W=[xT_])
                def stage_a(j, o, n):
                    pa = hp.next(); pb = hp.next()
                    for kc in range(KC):
                        S.op('pe', lambda e, kc=kc: e.matmul(
                            pa[:, 0:n], lhsT=wa[:, kc, ts(j, 128)], rhs=xT_[:, kc, o:o + n],
                            start=(kc == 0), stop=(kc == KC - 1)), R=[wa, xT_], W=[pa])
                    for kc in range(KC):
                        S.op('pe', lambda e, kc=kc: e.matmul(
                            pb[:, 0:n], lhsT=wa[:, kc, ts(8 + j, 128)], rhs=xT_[:, kc, o:o + n],
                            start=(kc == 0), stop=(kc == KC - 1)), R=[wa, xT_], W=[pb])
                    g_ = gc.next(); s_ = sg.next(); u_ = u0.next()
                    S.op('dve', lambda e: e.tensor_scalar(
                        out=g_[:, 0:n], in0=pa[:, 0:n], scalar1=b1T[:, j, ex:ex + 1], scalar2=7.0,
                        op0=ALU.add, op1=ALU.min), R=[pa, b1T], W=[g_])
                    S.op('act', lambda e: e.activation(
                        out=u_[:, 0:n], in_=pb[:, 0:n], func=AF.Identity, bias=b1T[:, 8 + j, ex:ex + 1], scale=1.0),
                        R=[pb, b1T], W=[u_])
                    S.op('act', lambda e: e.activation(
                        out=s_[:, 0:n], in_=g_[:, 0:n], func=AF.Sigmoid, scale=1.702), R=[g_], W=[s_])
                    return (j, o, n, g_, s_, u_)

                def stage_b(st):
                    j, o, n, g_, s_, u_ = st
                    S.op('dve', lambda e: e.tensor_scalar(
                        out=u_[:, 0:n], in0=u_[:, 0:n], scalar1=7.0, scalar2=-7.0, op0=ALU.min, op1=ALU.max),
                        R=[u_], W=[u_])
                    S.op('pool', lambda e: e.tensor_tensor(
                        out=s_[:, 0:n], in0=g_[:, 0:n], in1=s_[:, 0:n], op=ALU.mult), R=[g_, s_], W=[s_])
                    S.op('dve', lambda e: e.scalar_tensor_tensor(
                        out=aT[:, j, o:o + n], in0=u_[:, 0:n], scalar=1.0, in1=s_[:, 0:n],
                        op0=ALU.add, op1=ALU.mult), R=[u_, s_], W=[aTk[j]])

                prev = None
                for j in range(KC):
                    for (o, n) in cgs:
                        cur = stage_a(j, o, n)
                        if prev is not None:
                            stage_b(prev)
                        prev = cur
                stage_b(prev)
                for ct in range(CT):
                    y_ = ys.next()
                    for dh in range(2):
                        p = yp.next()
                        for fc in range(KC):
                            S.op('pe', lambda e, fc=fc, p=p, ct=ct, dh=dh: e.matmul(
                                p[:, :], lhsT=aT[:, fc, ts(ct, 128)], rhs=wb_[:, fc, ts(dh, 512)],
                                start=(fc == 0), stop=(fc == KC - 1)), R=[aTk[fc], wb_], W=[p])
                        S.op('dve', lambda e, p=p, y_=y_, dh=dh: e.tensor_tensor(
                            out=y_[:, ts(dh, 512)], in0=p[:, :], in1=bb[:, ts(dh, 512)], op=ALU.add),
                            R=[p, bb], W=[y_])
                    r0 = ex * CAP + ct * 128
                    S.dma('sp', lambda e, y_=y_, r0=r0: e.dma_start(out=YS[r0:r0 + 128, :], in_=y_[:, :]),
                          y_, R=[y_], W=[YS], join=True)
            S.barrier()
            S.release(w1b.bufs + w2b.bufs + b2b.bufs + xst.bufs + ys.bufs)

    def combine_tile(self, S, R, i, ctx, gb, out_buf):
        H, YS = ctx['H'], ctx['YS']
        dest4, gate4 = ctx['dest4'], ctx['gate4']
        hres = R['hres'].next()
        S.dma('sp', lambda e: e.dma_start(out=hres[:, :], in_=H[ts(i, 128), :]), hres, R=[H], W=[hres])
        yk = []
        for k in range(4):
            y = R['yk'].next()
            S.dma('pool', lambda e, y=y, k=k: e.indirect_dma_start(
                out=y[:, :], out_offset=None, in_=YS[:, :],
                in_offset=bass.IndirectOffsetOnAxis(ap=dest4[:, 4 * i + k:4 * i + k + 1], axis=0),
                bounds_check=self.bc_reg, oob_is_err=False), y, R=[YS, dest4], W=[y])
            yk.append(y)
        acc = R['acc'].next()
        S.op('act', lambda e: e.activation(out=acc[:, :], in_=hres[:, :], func=AF.Copy, scale=ALPHA),
             R=[hres], W=[acc])
        for k in range(4):
            S.op('dve', lambda e, k=k: e.scalar_tensor_tensor(
                out=acc[:, :], in0=yk[k][:, :], scalar=gate4[:, 4 * i + k:4 * i + k + 1], in1=acc[:, :],
                op0=ALU.mult, op1=ALU.add), R=[yk[k], gate4, acc], W=[acc])
        self.ln_tile(S, R, acc[:, :], acc, gb, out_buf)

    def combine_bufs(self, S, es):
        R = {}
        self.ln_bufs(S, es, R)
        R['hres'] = Ring([S.sb(es, 'hres', [128, D], F32) for _ in range(2)])
        R['yk'] = Ring([S.sb(es, 'yk', [128, D], F32) for _ in range(8)])
        R['acc'] = Ring([S.sb(es, 'acc', [128, D], F32) for _ in range(2)])
        for y in R['yk'].bufs:
            S.op('dve', lambda e, y=y: e.memset(y[:, :], 0.0), W=[y])
        return R

    def combine_qkv(self, S, inp, ctx, gidx, outs):
        nc = self.nc
        c = self.c
        H2, QT, KT, VV, LF = outs['H2'], outs['QT'], outs['KT'], outs['V'], outs['LF']
        win_d, bf_d = inp['attn_w_in'], inp['attn_b_f']
        with ExitStack() as es:
            R = self.combine_bufs(S, es)
            gb = S.sb(es, 'gb2', [128, 2, D], F32)
            self.load_gb(S, gb, inp['ln_g'], inp['ln_b'], gidx)
            ho = Ring([S.sb(es, 'ho', [128, D], F32) for _ in range(2)])
            big = Ring([S.ps(es, 'big', [128, D]) for _ in range(2)])
            mm = Ring([S.ps(es, 'mm', [128, 512]) for _ in range(3)])
            fps = S.ps(es, 'fps', [16, 512])
            win = S.sb(es, 'win', [128, KC, 3088], BF16)
            for hf in range(2):
                S.dma('pool', lambda e, hf=hf: e.dma_start(
                    out=win[:, :, ts(hf, 1544)],
                    in_=win_d[0, :, ts(hf, 1544)].rearrange("(kc p) f -> p kc f", p=128)),
                    win, R=[win_d], W=[win], join=(hf == 1))
            nbf = S.sb(es, 'nbf', [16, 2], F32)
            with nc.allow_non_contiguous_dma(reason="tiny per-partition vector"):
                S.dma('sp', lambda e: e.dma_start(out=nbf[:, 0:1], in_=bf_d[0].rearrange("(h o) -> h o", o=1)),
                      nbf, R=[bf_d], W=[nbf])
            S.op('dve', lambda e: e.tensor_scalar(out=nbf[:, 1:2], in0=nbf[:, 0:1], scalar1=-1.0, scalar2=None,
                                                  op0=ALU.mult), R=[nbf], W=[nbf])
            h2T = Ring([S.sb(es, 'h2T', [128, KC, 512], BF16) for _ in range(2)])
            qks = Ring([S.sb(es, 'qks', [128, 512], BF16) for _ in range(3)])
            vsb = Ring([S.sb(es, 'vsb', [128, NH, 65], BF16) for _ in range(2)])
            for v_ in vsb.bufs:
                S.op('pool', lambda e, v_=v_: e.memset(v_[:, :, :], 1.0), W=[v_])
            fsb = Ring([S.sb(es, 'fsb', [16, 2, 512], F32) for _ in range(2)])
            for seg in range(self.NSEG + 1):
                ntile = 1 if seg == self.NSEG else 4
                W = ntile * 128
                t0 = seg * 512
                hT_ = h2T.next()
                for t in range(ntile):
                    i = seg * 4 + t
                    h = ho.next()
                    self.combine_tile(S, R, i, ctx, gb, h)
                    S.dma('sp', lambda e, h=h, i=i: e.dma_start(out=H2[ts(i, 128), :], in_=h[:, :]), h,
                          R=[h], W=[H2], join=True)
                    tp = big.next()
                    for kc in range(KC):
                        S.op('pe', lambda e, kc=kc, tp=tp, h=h: e.transpose(
                            out=tp[:, ts(kc, 128)], in_=h[:, ts(kc, 128)], identity=c['ident']),
                            R=[h, c['f']], W=[tp])
                    S.op('act', lambda e, tp=tp, t=t: e.activation(
                        out=hT_[:, :, ts(t, 128)], in_=tp[:, :].rearrange("p (kc t) -> p kc t", kc=KC), func=AF.Copy),
                        R=[tp], W=[hT_])
                if seg > 0:
                    outs['exchange'](seg - 1)
                for m in range(16):
                    p = mm.next()
                    for kc in range(KC):
                        S.op('pe', lambda e, kc=kc, p=p, m=m: e.matmul(
                            p[:, 0:W], lhsT=win[:, kc, ts(m, 128)], rhs=hT_[:, kc, 0:W],
                            start=(kc == 0), stop=(kc == KC - 1)), R=[win, hT_], W=[p])
                    q_ = qks.next()
                    S.op('act', lambda e, p=p, q_=q_, m=m: e.activation(
                        out=q_[:, 0:W], in_=p[:, 0:W], func=AF.Copy, scale=(0.125 if m < 8 else 1.0)), R=[p], W=[q_])
                    for hh in range(2):
                        hd_ = 2 * (m % 8) + hh
                        if m < 8:
                            S.dma('sp', lambda e, q_=q_, hh=hh, hd_=hd_: e.dma_start(
                                out=QT[hd_, :, t0:t0 + W], in_=q_[ts(hh, 64), 0:W]), q_, R=[q_], W=[QT], join=True)
                        else:
                            kd = KT[seg][hd_ // 8]
                            S.dma('sp', lambda e, q_=q_, hh=hh, hd_=hd_, kd=kd: e.dma_start(
                                out=kd[ts(hd_ % 8, 64), 0:W], in_=q_[ts(hh, 64), 0:W]), q_, R=[q_], W=[kd], join=True)
                for t in range(ntile):
                    v_ = vsb.next()
                    for hf in range(2):
                        p = mm.next()
                        for kc in range(KC):
                            S.op('pe', lambda e, kc=kc, p=p, t=t, hf=hf: e.matmul(
                                p[:, :], lhsT=hT_[:, kc, ts(t, 128)], rhs=win[:, kc, 2048 + hf * 512:2560 + hf * 512],
                                start=(kc == 0), stop=(kc == KC - 1)), R=[win, hT_], W=[p])
                        S.op('dve', lambda e, p=p, v_=v_, hf=hf: e.tensor_copy(
                            out=v_[:, ts(hf, 8), 0:64], in_=p[:, :].rearrange("p (h d) -> p h d", d=64)),
                             R=[p], W=[v_])
                    for q4 in range(4):
                        vd = VV[seg][q4]
                        S.dma('sp', lambda e, v_=v_, t=t, q4=q4, vd=vd: e.dma_start(
                            out=vd.t.rearrange("(h p) (k d) -> p h k d", p=128, d=65)[:, :, t, :],
                            in_=v_[:, ts(q4, 4), :]), v_, R=[v_], W=[vd], join=True)
                for kc in range(KC):
                    S.op('pe', lambda e, kc=kc: e.matmul(
                        fps[:, 0:W], lhsT=win[:, kc, 3072:3088], rhs=hT_[:, kc, 0:W],
                        start=(kc == 0), stop=(kc == KC - 1)), R=[win, hT_], W=[fps])
                f_ = fsb.next()
                S.op('act', lambda e: e.activation(out=f_[:, 0, 0:W], in_=fps[:, 0:W], func=AF.Exp,
                                                   bias=nbf[:, 1:2], scale=-1.0), R=[fps, nbf], W=[f_])
                S.op('dve', lambda e: e.tensor_scalar(out=f_[:, 0, 0:W], in0=f_[:, 0, 0:W], scalar1=1.0, scalar2=None,
                                                      op0=ALU.add), R=[f_], W=[f_])
                S.op('act', lambda e: e.activation(out=f_[:, 1, 0:W], in_=f_[:, 0, 0:W], func=AF.Ln), R=[f_], W=[f_])
                S.op('dve', lambda e: e.tensor_scalar(out=f_[:, 1, 0:W], in0=f_[:, 1, 0:W], scalar1=-1.0, scalar2=None,
                                                      op0=ALU.mult), R=[f_], W=[f_])
                S.dma('sp', lambda e, f_=f_: e.dma_start(out=LF[:, t0:t0 + W], in_=f_[:, 1, 0:W]), f_,
                      R=[f_], W=[LF], join=True)
            outs['exchange'](self.NSEG)
            outs['exchange'](-1)
            S.barrier()

    def attention(self, S, inp, es_outer, after_init=None):
        nc = self.nc
        c = self.c
        NKB = 129
        CL = 2176
        QT, AT, CALL, QA = (inp[k] for k in ('QT', 'AT', 'CALL', 'QA'))
        RK, RV, RL, LFT = (inp[k] for k in ('RCVK', 'RCVV', 'RCVL', 'LFT'))
        c2d = inp['cst2']
        with ExitStack() as es:
            c2 = S.sb(es, 'c2', [128, 466], F32)
            S.dma('sp', lambda e: e.dma_start(out=c2[:, :], in_=c2d[:, :]), c2, R=[c2d], W=[c2])
            BT = c2.t[:, 0:128]; rowsel = c2.t[:, 128:256]; Dg = c2.t[:, 256:384]; padadd = c2.t[:, 384:385]
            E65 = c2.t[0:65, 385:449]
            oh16 = c2.t[:, 449:465]; ch0col = c2.t[:, 465:466]
            mk = S.sb(es, 'maskT', [128, 16, 512], BF16)
            S.dma('sp', lambda e: e.dma_start(out=mk[:, :, :], in_=inp['maskT'].t.rearrange("b p q -> p b q")),
                  mk, R=[inp['maskT']], W=[mk])
            idxq = S.sb(es, 'idxq', [128, 1], I32)
            S.dma('sp', lambda e: e.dma_start(out=idxq[:, :], in_=inp['idxq'][:, :]), idxq, R=[inp['idxq']], W=[idxq])
            cT = S.sb(es, 'cT', [128, 136, 16], F32)
            refbc = S.sb(es, 'refbc', [128, 128], F32)
            with ExitStack() as es1:
                lfa = S.sb(es1, 'lfa', [128, CL], F32)
                ones = S.sb(es1, 'onesf', [128, CL], F32)
                call = S.sb(es1, 'call', [128, CL], F32)
                sm = S.sb(es1, 'psm', [128, 8], F32)
                cq = S.sb(es1, 'cq', [128, 512], F32)
                wq = S.sb(es1, 'wq', [128, 3, 512], F32)
                qa = S.sb(es1, 'qa', [128, 2, 512], BF16)
                tmpd = S.sb(es1, 'tmpd', [128, 128], F32)
                psA = S.ps(es1, 'psA', [128, 512])
                psB = Ring([S.ps(es1, 'psB', [128, 512]) for _ in range(2)])
                with ExitStack() as es0:
                    lfh = S.sb(es0, 'lfh', [16, 17408], F32)
                    S.op('pool', lambda e: e.memset(lfh[:, :], 0.0), W=[lfh])
                    for cl in range(4):
                        S.dma('sp', lambda e, cl=cl: e.dma_start(
                            out=lfh.t[:, 128:16512].rearrange("h (j c t) -> h c j t", c=4, t=512)[:, cl],
                            in_=RL[cl * 16:(cl + 1) * 16, 0:4096].rearrange("h (j t) -> h j t", t=512)),
                            lfh, R=[RL], W=[lfh], join=(cl > 0))
                    S.dma('sp', lambda e: e.dma_start(out=lfh[:, 0:16], in_=RL[0:16, 4096:4112]), lfh,
                          R=[RL], W=[lfh], join=True)
                    S.dma('sp', lambda e: e.dma_start(out=LFT[:, :], in_=lfh[:, :]), lfh, R=[lfh], W=[LFT])
                    S.barrier()
                    S.release([lfh])
                S.dma('sp', lambda e: e.dma_start(out=lfa[:, :], in_=LFT.t.rearrange("h (ch c) -> (h ch) c", ch=8)),
                      lfa, R=[LFT], W=[lfa])
                S.op('pool', lambda e: e.memset(ones[:, :], 1.0), W=[ones])
                S.op('dve', lambda e: e.tensor_tensor_scan(out=call[:, :], data0=ones[:, :], data1=lfa[:, :], initial=0.0,
                                                           op0=ALU.mult, op1=ALU.add), R=[ones, lfa], W=[call])
                S.op('pe', lambda e: e.matmul(psA[:, 0:2], lhsT=BT, rhs=call[:, CL - 2:CL], start=True, stop=True),
                     R=[c2, call], W=[psA])
                S.op('dve', lambda e: e.tensor_copy(out=sm[:, 0:2], in_=psA[:, 0:2]), R=[psA], W=[sm])
                S.op('dve', lambda e: e.tensor_scalar(out=call[:, :], in0=call[:, :], scalar1=sm[:, 1:2], scalar2=None,
                                                      op0=ALU.add), R=[sm, call], W=[call])
                S.dma('sp', lambda e: e.dma_start(out=CALL.t.rearrange("h (ch c) -> (h ch) c", ch=8), in_=call[:, :]),
                      call, R=[call], W=[CALL])
                cT4 = cT.t.rearrange("p (ch bl) h -> p ch bl h", ch=8)
                for bl in range(17):
                    p = psB.next()
                    S.op('pe', lambda e, p=p, bl=bl: e.transpose(out=p[:, 0:128], in_=call[:, ts(bl, 128)],
                                                                 identity=c['ident']), R=[call, c['f']], W=[p])
                    S.op('act' if bl % 2 else 'dve',
                         (lambda e, p=p, bl=bl: e.activation(out=cT4[:, :, bl, :],
                                                             in_=p[:, 0:128].rearrange("p (h ch) -> p ch h", ch=8),
                                                             func=AF.Copy)) if bl % 2 else
                         (lambda e, p=p, bl=bl: e.tensor_copy(out=cT4[:, :, bl, :],
                                                              in_=p[:, 0:128].rearrange("p (h ch) -> p ch h", ch=8))),
                         R=[p], W=[cT])
                S.op('pe', lambda e: e.matmul(psA[:, 128:256], lhsT=rowsel, rhs=cT[:, 0:128:16, :], start=True, stop=True),
                     R=[c2, cT], W=[psA])
                S.op('dve', lambda e: e.tensor_copy(out=refbc[:, :], in_=psA[:, 128:256]), R=[psA], W=[refbc])
                KA = inp['KA']
                refhj = S.sb(es1, 'refhj', [128, 8], F32)
                padm = S.sb(es1, 'padm', [128, 128], F32)
                S.op('dve', lambda e: e.memset(padm[:, :], 0.0), W=[padm])
                S.op('dve', lambda e: e.tensor_scalar(out=padm[:, 16:128], in0=padm[:, 16:128], scalar1=ch0col, scalar2=None,
                                                      op0=ALU.add), R=[padm, c2], W=[padm])
                for j in range(8):
                    S.op('dve', lambda e, j=j: e.scalar_tensor_tensor(
                        out=tmpd[:, 0:16], in0=refbc[:, j * 16:(j + 1) * 16], scalar=1.0, in1=oh16, op0=ALU.mult,
                        op1=ALU.mult, accum_out=refhj[:, j:j + 1]), R=[refbc, c2], W=[tmpd, refhj])
                kv = Ring([S.sb(es1, 'kv', [128, 2, 2176], F32) for _ in range(2)])
                kb = Ring([S.sb(es1, 'kb', [128, 2, 2176], BF16) for _ in range(2)])
                for j in range(8):
                    v_ = kv.next(); b_ = kb.next()
                    S.op('dve', lambda e, j=j, v_=v_: e.tensor_scalar(out=v_[:, 0, :], in0=call[:, :], scalar1=-1.0,
                                                                      scalar2=refhj[:, j:j + 1], op0=ALU.mult, op1=ALU.add),
                         R=[call, refhj], W=[v_])
                    S.op('dve', lambda e, v_=v_: e.tensor_tensor(out=v_[:, 0, 0:128], in0=v_[:, 0, 0:128], in1=padm[:, :],
                                                                 op=ALU.subtract), R=[v_, padm], W=[v_])
                    S.op('act', lambda e, v_=v_, b_=b_: e.activation(out=b_[:, 0, :], in_=v_[:, 0, :], func=AF.Copy),
                         R=[v_], W=[b_])
                    S.op('act', lambda e, v_=v_, b_=b_: e.activation(out=v_[:, 1, :], in_=b_[:, 0, :], func=AF.Copy),
                         R=[b_], W=[v_])
                    S.op('dve', lambda e, v_=v_, b_=b_: e.tensor_tensor(out=b_[:, 1, :], in0=v_[:, 0, :], in1=v_[:, 1, :],
                                                                        op=ALU.subtract), R=[v_], W=[b_])
                    S.dma('sp', lambda e, j=j, b_=b_: e.dma_start(
                        out=KA.t[:, :, 2 * j:2 * j + 2, :].rearrange("h ch r c -> (h ch) r c"), in_=b_[:, :, :]),
                        b_, R=[b_], W=[KA], join=True)
                S.op('dve', lambda e: e.tensor_tensor(out=tmpd[:, :], in0=refbc[:, :], in1=Dg, op=ALU.mult),
                     R=[refbc, c2], W=[tmpd])
                S.op('dve', lambda e: e.tensor_reduce(out=sm[:, 2:3], in_=tmpd[:, :], axis=mybir.AxisListType.X, op=ALU.add),
                     R=[tmpd], W=[sm])
                bq = nc.gpsimd.to_reg(16 * 34 - 1)
                S.dma('pool', lambda e: e.indirect_dma_start(
                    out=cq[:, :], out_offset=None, in_=CALL.t.rearrange("h (a c) -> (h a) c", c=512),
                    in_offset=bass.IndirectOffsetOnAxis(ap=idxq[:, 0:1], axis=0), element_offset=128,
                    bounds_check=bq, oob_is_err=False), cq, R=[CALL, idxq], W=[cq])
                S.op('dve', lambda e: e.tensor_scalar(out=wq[:, 0, :], in0=cq[:, :], scalar1=sm[:, 2:3], scalar2=None,
                                                      op0=ALU.subtract), R=[cq, sm], W=[wq])
                S.op('dve', lambda e: e.tensor_copy(out=qa[:, 0, :], in_=wq[:, 0, :]), R=[wq], W=[qa])
                S.op('dve', lambda e: e.tensor_copy(out=wq[:, 1, :], in_=qa[:, 0, :]), R=[qa], W=[wq])
                S.op('dve', lambda e: e.tensor_tensor(out=qa[:, 1, :], in0=wq[:, 0, :], in1=wq[:, 1, :], op=ALU.subtract),
                     R=[wq], W=[qa])
                S.dma('sp', lambda e: e.dma_start(out=QA[:, :, :], in_=qa[:, :, :]), qa, R=[qa], W=[QA])
                S.barrier()
                S.release([lfa, call, cq, qa] + kb.bufs)
            Kt = Ring([S.sb(es, 'Kt', [96, 136 * 128], BF16) for _ in range(2)])
            Vt = Ring([S.sb(es, 'Vt', [128, NKB, 65], BF16) for _ in range(2)])
            Qt = Ring([S.sb(es, 'Qt', [96, 512], BF16) for _ in range(8)])
            Pt = Ring([S.sb(es, 'Pt', [128, 1024], BF16) for _ in range(4)])
            osb = Ring([S.sb(es, 'osb', [64, 512], F32) for _ in range(2)])
            r65 = Ring([S.sb(es, 'r65', [65, 512], F32) for _ in range(2)])
            asb = Ring([S.sb(es, 'asb', [64, 512], BF16) for _ in range(2)])
            Sp = Ring([S.ps(es, 'Sp', [128, 1024]) for _ in range(3)])
            Op = Ring([S.ps(es, 'Op', [128, 512]) for _ in range(1)])
            bcp = S.ps(es, 'bcp', [128, 512])
            for k_ in Kt.bufs:
                S.op('pool', lambda e, k_=k_: e.memset(k_[64:96, :], 0.0), W=[k_])
                S.op('pool', lambda e, k_=k_: e.memset(k_[64:66, :], 1.0), W=[k_])
                S.op('pool', lambda e, k_=k_: e.memset(k_[0:64, 0:128], 0.0), W=[k_])
            for jq, q_ in enumerate(Qt.bufs):
                S.dma('sp', lambda e, jq=jq, q_=q_: e.dma_start(out=q_[64:96, :], in_=inp['qone'][jq]), q_,
                      R=[inp['qone']], W=[q_])
            for v_ in Vt.bufs:
                S.op('pool', lambda e, v_=v_: e.memset(v_[:, 0, :], 0.0), W=[v_])
            for r_ in r65.bufs:
                S.op('pool', lambda e, r_=r_: e.memset(r_[:, :], 0.0), W=[r_])
            if after_init is not None:
                after_init()
            units = []
            for h in range(NH):
                for j in range(self.NSEG):
                    nblk = 1 + 16 * (j + 1)
                    b = 0
                    while b < nblk:
                        nb_ = min(2, nblk - b)
                        units.append((h, j, b, nb_, nblk))
                        b += nb_
            state = {}

            def prep_hj(h, j):
                if (h, j) in state:
                    return state[(h, j)]
                if j == 0:
                    k_ = Kt.next(); v_ = Vt.next()
                    for jj in range(8):
                        rk = RK[jj][h // 8]
                        S.dma('sp', lambda e, jj=jj, rk=rk: e.dma_start(
                            out=k_.t[0:64, 128 + jj * 2048:128 + (jj + 1) * 2048].rearrange("p (c t) -> p c t", c=4),
                            in_=rk.t.rearrange("(c r) t -> r c t", c=4)[ts(h % 8, 64)]),
                            k_, R=[rk], W=[k_], join=(jj > 0))
                    rk = RK[8][h // 8]
                    S.dma('sp', lambda e: e.dma_start(out=k_[0:64, 0:16], in_=rk[ts(h % 8, 64), 0:16]),
                          k_, R=[rk], W=[k_], join=True)
                    S.dma('sp', lambda e: e.dma_start(
                        out=k_.t[66:82, :].rearrange("r (ch c) -> r ch c", ch=8),
                        in_=inp['KA'].t[h].rearrange("ch r c -> r ch c")), k_, R=[inp['KA']], W=[k_], join=True)
                    for jj in range(8):
                        rv = RV[jj][h // 4]
                        S.dma('sp', lambda e, jj=jj, rv=rv: e.dma_start(
                            out=v_.t[:, 1 + 16 * jj:17 + 16 * jj, :].rearrange("p (c k) d -> p c (k d)", c=4),
                            in_=rv.t.rearrange("(c r) kd -> r c kd", c=4)[ts(h % 4, 128)]),
                            v_, R=[rv], W=[v_], join=(jj > 0))
                    rv = RV[8][h // 4]
                    S.dma('sp', lambda e: e.dma_start(out=v_[0:16, 0, :], in_=rv[(h % 4) * 128:(h % 4) * 128 + 16, 0:65]),
                          v_, R=[rv], W=[v_], join=True)
                    state[('kv', h)] = (k_, v_)
                k_, v_ = state[('kv', h)]
                q_ = Qt.bufs[j]
                S.dma('sp', lambda e: e.dma_start(out=q_[0:64, :], in_=QT[h, :, ts(j, 512)]), q_, R=[QT], W=[q_])
                S.dma('sp', lambda e: e.dma_start(out=q_[64:66, :], in_=QA[h * 8 + j, :, :]), q_, R=[QA], W=[q_], join=True)
                state[(h, j)] = (k_, v_, q_, Op.next())
                return state[(h, j)]

            def emit_qk(un):
                h, j, b0, nb_, nblk = un
                k_, v_, q_, o_ = prep_hj(h, j)
                s_ = Sp.next()
                for i in range(nb_):
                    b = b0 + i
                    lvl = b >= nblk - 16
                    S.op('pe', lambda e, b=b, i=i, lvl=lvl: e.matmul(s_[:, ts(i, 512)], lhsT=k_[:, ts(b, 128)], rhs=q_[:, :],
                                                                      start=True, stop=not lvl), R=[k_, q_], W=[s_])
                    if lvl:
                        br = b - (nblk - 16)
                        S.op('pe', lambda e, i=i, br=br: e.matmul(s_[:, ts(i, 512)], lhsT=c['identb'], rhs=mk[:, br, :],
                                                                    start=False, stop=True), R=[c['b'], mk], W=[s_])
                return s_

            LOOK = 2
            sq = [emit_qk(units[n]) for n in range(LOOK)]
            for n, un in enumerate(units):
                h, j, b0, nb_, nblk = un
                if n + LOOK < len(units):
                    sq.append(emit_qk(units[n + LOOK]))
                k_, v_, q_, o_ = state[(h, j)]
                s_ = sq[n]
                p_ = Pt.next()
                w_ = nb_ * 512
                S.op('act', lambda e: e.activation(out=p_[:, 0:w_], in_=s_[:, 0:w_], func=AF.Exp), R=[s_], W=[p_])
                for i in range(nb_):
                    b = b0 + i
                    S.op('pe', lambda e, b=b, i=i: e.matmul(o_[0:65, :], lhsT=v_[:, b, :], rhs=p_[:, ts(i, 512)],
                                                            start=(b == 0), stop=(b == nblk - 1)), R=[v_, p_], W=[o_])
                if b0 + nb_ == nblk:
                    r_ = r65.next(); os_ = osb.next(); a_ = asb.next()
                    S.op('dve', lambda e: e.reciprocal(out=r_[64:65, :], in_=o_[64:65, :]), R=[o_], W=[r_])
                    S.op('act', lambda e: e.activation(out=os_[:, :], in_=o_[0:64, :], func=AF.Copy), R=[o_], W=[os_])
                    S.op('pe', lambda e: e.matmul(bcp[0:64, :], lhsT=E65, rhs=r_[:, :], start=True, stop=True),
                         R=[c2, r_], W=[bcp])
                    S.op('dve', lambda e: e.tensor_tensor(out=a_[:, :], in0=os_[:, :], in1=bcp[0:64, :], op=ALU.mult),
                         R=[os_, bcp], W=[a_])
                    S.dma('sp', lambda e: e.dma_start(out=AT[h // 2, (h % 2) * 64:(h % 2) * 64 + 64, ts(j, 512)],
                                                      in_=a_[:, :]), a_, R=[a_], W=[AT], join=True)
                    del state[(h, j)]
            S.barrier()

    def oproj_route(self, S, inp, ctx, gidx, dbg_out=None):
        c = self.c
        AT, H2 = inp['AT'], inp['H2']
        wo_d = inp['attn_w_out']
        with ExitStack() as es:
            R = self.route_bufs(S, es)
            self.ln_bufs(S, es, R)
            R['big'] = Ring([S.ps(es, 'big', [128, D]) for _ in range(2)])
            mm = Ring([S.ps(es, 'mm', [128, 512]) for _ in range(3)])
            wo = S.sb(es, 'wo', [128, KC, D], BF16)
            S.dma('pool', lambda e: e.dma_start(out=wo[:, :, :], in_=wo_d[0].rearrange("(kc p) f -> p kc f", p=128)),
                  wo, R=[wo_d], W=[wo])
            gb = S.sb(es, 'gb', [128, 2, D], F32)
            self.load_gb(S, gb, inp['ln_g'], inp['ln_b'], gidx)
            aT = Ring([S.sb(es, 'aT', [128, KC, 512], BF16) for _ in range(2)])
            hres = Ring([S.sb(es, 'hres', [128, D], F32) for _ in range(2)])
            acc = Ring([S.sb(es, 'acc', [128, D], F32) for _ in range(2)])
            h3 = Ring([S.sb(es, 'h3', [128, D], F32) for _ in range(2)])
            for j in range(self.NSEG):
                a_ = aT.next()
                S.dma('sp', lambda e: e.dma_start(out=a_[:, :, :], in_=AT[:, :, ts(j, 512)].rearrange("pr p t -> p pr t")),
                      a_, R=[AT], W=[a_])
                for t in range(4):
                    i = 4 * j + t
                    hr = hres.next()
                    S.dma('sp', lambda e: e.dma_start(out=hr[:, :], in_=H2[ts(i, 128), :]), hr, R=[H2], W=[hr])
                    ac = acc.next()
                    for hf in range(2):
                        p = mm.next()
                        for pr in range(KC):
                            S.op('pe', lambda e, pr=pr: e.matmul(p[:, :], lhsT=a_[:, pr, ts(t, 128)],
                                                                 rhs=wo[:, pr, ts(hf, 512)], start=(pr == 0),
                                                                 stop=(pr == KC - 1)), R=[a_, wo], W=[p])
                        S.op('dve', lambda e: e.scalar_tensor_tensor(out=ac[:, ts(hf, 512)], in0=hr[:, ts(hf, 512)],
                                                                     scalar=ALPHA, in1=p[:, :], op0=ALU.mult, op1=ALU.add),
                             R=[hr, p], W=[ac])
                    h = h3.next()
                    self.ln_tile(S, R, ac[:, :], ac, gb, h)
                    if dbg_out is None:
                        self.route_tile(S, R, i, h, ctx)
                    else:
                        S.dma('sp', lambda e: e.dma_start(out=dbg_out[ts(i, 128), :], in_=h[:, :]), h,
                              R=[h], W=[dbg_out], join=True)
            S.barrier()
            S.release([b for r in R.values() for b in r.bufs] + aT.bufs + hres.bufs + [wo, gb])


def make_consts(CAP):
    f = np.zeros((128, 800), np.float32)
    f[:, 0:128] = np.eye(128, dtype=np.float32)
    f[:, 256:288] = (np.arange(32, dtype=np.float32) * CAP - BIG)[None, :]
    for g, w in enumerate((2, 4, 8, 16)):
        t = np.arange(128)
        f[:, 288 + g * 128:288 + (g + 1) * 128] = (1.0 / np.minimum(t + 1, w))[None, :]
    b = np.zeros((128, 384), np.float32)
    b[:, 0:128] = np.eye(128)
    b[:, 128:256] = np.triu(np.ones((128, 128)), 1)
    b[:, 256:384] = 1.0
    return f, b.astype(ml_dtypes.bfloat16)


def make_xin(x, meta, core, nseg=8):
    b, cl = core // 4, core % 4
    rows = np.zeros((nseg * 528 + 144, D), np.float32)
    for j in range(nseg):
        G = cl + 4 * j
        s = G * 512
        if s == 0:
            rows[j * 528:j * 528 + 16] = meta
        else:
            rows[j * 528:j * 528 + 16] = x[b, s - 16:s]
        rows[j * 528 + 16:(j + 1) * 528] = x[b, s:s + 512]
    rows[nseg * 528 + 16:nseg * 528 + 32] = meta
    return rows


def make_valid(NT):
    v = np.ones((128, NT), np.float32)
    v[16:, NT - 1] = 0.0
    return v


def decl_weights(P, inp, layers=(0, 1)):
    nl = len(layers)
    P.wbase = layers[0]
    inp['ln_g'] = P.dt('ln_g', [4, D], F32, 'ExternalInput')
    inp['ln_b'] = P.dt('ln_b', [4, D], F32, 'ExternalInput')
    inp['router_w'] = P.dt('router_w', [2, D, 32], F32, 'ExternalInput')
    inp['router_b'] = P.dt('router_b', [2, 32], F32, 'ExternalInput')
    inp['w1'] = P.dt('w1', [nl, 32, D, 2 * D], F32, 'ExternalInput')
    inp['b1'] = P.dt('b1', [nl, 32, 2 * D], F32, 'ExternalInput')
    inp['w2'] = P.dt('w2', [nl, 32, D, D], F32, 'ExternalInput')
    inp['b2'] = P.dt('b2', [nl, 32, D], F32, 'ExternalInput')


def make_consts2():
    f = np.zeros((128, 466), np.float32)
    k = np.arange(128)
    f[k, 449 + k // 8] = 1.0
    f[k % 8 == 0, 465] = 30000.0
    f[:, 0:128] = ((k[:, None] // 8 == k[None, :] // 8) & (k[:, None] % 8 < k[None, :] % 8)).astype(np.float32)
    f[127, 128:256] = 1.0
    for p in range(128):
        h, j = p // 8, p % 8
        f[p, 256 + j * 16 + h] = 1.0
    f[16:, 384] = 30000.0
    f[64, 385:449] = 1.0
    return f


def make_qone():
    q = np.zeros((8, 32, 512), np.float32)
    for j in range(8):
        q[j, 2 + 2 * j:4 + 2 * j, :] = 1.0
    return q.astype(ml_dtypes.bfloat16)


def make_mask(cl):
    m = np.full((16, 128, 512), NEG, np.float32)
    ki = np.arange(128)[:, None]
    qi = np.arange(128)[None, :]
    for r in range(4):
        for kb in range(4):
            for qb in range(4):
                if r < cl or (r == cl and kb < qb):
                    m[r * 4 + kb, :, ts(qb, 128)] = 0.0
                elif r == cl and kb == qb:
                    m[r * 4 + kb, :, ts(qb, 128)] = np.where(ki <= qi, 0.0, NEG)
    return m.astype(ml_dtypes.bfloat16)


def make_idxq(cl):
    p = np.arange(128)
    return ((p // 8) * 34 + cl + 4 * (p % 8)).astype(np.int32).reshape(128, 1)


def build_fused(cfg):
    P = Prog(cfg)
    nc = P.nc
    CAP = P.CAP
    NTOK = 33 * 128
    inp = {}
    inp['xin'] = P.dt('xin', [cfg['NSEG'] * 528 + 144, D], F32, 'ExternalInput')
    valid_d = P.dt('valid', [128, 33], F32, 'ExternalInput')
    inp['pool_w'] = P.dt('pool_w', [1, 4, 256, 256], F32, 'ExternalInput')
    inp['pool_scale'] = P.dt('pool_scale', [1, D], F32, 'ExternalInput')
    inp['attn_w_in'] = P.dt('attn_w_in', [1, D, 3088], F32, 'ExternalInput')
    inp['attn_b_f'] = P.dt('attn_b_f', [1, 16], F32, 'ExternalInput')
    inp['attn_w_out'] = P.dt('attn_w_out', [1, D, D], F32, 'ExternalInput')
    inp['maskT'] = P.dt('maskT', [16, 128, 512], BF16, 'ExternalInput')
    inp['idxq'] = P.dt('idxq', [128, 1], I32, 'ExternalInput')
    inp['cst2'] = P.dt('cst2', [128, 466], F32, 'ExternalInput')
    inp['qone'] = P.dt('qone', [8, 32, 512], BF16, 'ExternalInput')
    inp['KA'] = P.dt('KAs', [16, 8, 16, 2176], BF16, 'Internal')
    decl_weights(P, inp, layers=(0, 1))
    inp['H'] = P.dt('Hs', [NTOK, D], F32, 'Internal')
    inp['XS'] = P.dt('XS', [32 * CAP, D], BF16, 'Internal')
    inp['YS'] = P.dt('YS', [32 * CAP, D], F32, 'Internal')
    inp['H2'] = P.dt('H2s', [NTOK, D], F32, 'Internal')
    inp['QT'] = P.dt('QTs', [16, 64, NTOK], BF16, 'Internal')
    SNDK, SNDV, RCVK, RCVV = [], [], [], []
    for sg in range(9):
        wk = 512 if sg < 8 else 128
        wv = 260 if sg < 8 else 65
        SNDK.append([P.dt('SK%d_%d' % (sg, i), [512, wk], BF16, 'Internal') for i in range(2)])
        RCVK.append([P.dt('RK%d_%d' % (sg, i), [2048, wk], BF16, 'Internal') for i in range(2)])
        SNDV.append([P.dt('SV%d_%d' % (sg, i), [512, wv], BF16, 'Internal') for i in range(4)])
        RCVV.append([P.dt('RV%d_%d' % (sg, i), [2048, wv], BF16, 'Internal') for i in range(4)])
    SNDL = P.dt('SNDL', [16, NTOK], F32, 'Internal')
    inp['RCVK'] = RCVK
    inp['RCVV'] = RCVV
    inp['RCVL'] = P.dt('RCVL', [64, NTOK], F32, 'Internal')
    inp['LFT'] = P.dt('LFT', [16, 17408], F32, 'Internal')
    inp['AT'] = P.dt('ATs', [8, 128, 4096], BF16, 'Internal')
    inp['CALL'] = P.dt('CALL', [16, 17408], F32, 'Internal')
    inp['QA'] = P.dt('QAs', [128, 2, 512], BF16, 'Internal')
    out = P.dt('out', [32 * 128, D], F32, 'ExternalOutput')
    outs = dict(H2=inp['H2'], QT=inp['QT'], KT=SNDK, V=SNDV, LF=SNDL)
    groups = [[0, 1, 2, 3], [4, 5, 6, 7]]
    with ExitStack() as es:
        S = Sch(nc, es, n_dsem=96)
        P.consts(S, es)

        deferred = {'A': [], 'B': [], 'C': [], 'D': []}

        def issue(pairs):
            ds = S.dfree.pop()
            ds.nobar = True
            for (a_, d_) in pairs:
                d_.dsem = ds
                S.coll("AllGather", groups, a_, d_)
            for (a_, d_) in pairs:
                d_.w = {id(ds.sem): (ds.sem, ds.cnt, 'dma')}

        def exchange(sg):
            if sg < 0:
                S.coll("AllGather", groups, SNDL, inp['RCVL'])
                issue(deferred['A'])
                return
            deferred['A'].append((SNDK[sg][0], RCVK[sg][0]))
            deferred['A'].append((SNDV[sg][0], RCVV[sg][0]))
            deferred['B'].append((SNDV[sg][1], RCVV[sg][1]))
            deferred['C'].append((SNDK[sg][1], RCVK[sg][1]))
            deferred['C'].append((SNDV[sg][2], RCVV[sg][2]))
            deferred['D'].append((SNDV[sg][3], RCVV[sg][3]))
        outs['exchange'] = exchange
        inp['valid_sb'] = S.sb(es, 'valid', [128, 33], F32)
        S.dma('sp', lambda e: e.dma_start(out=inp['valid_sb'][:, :], in_=valid_d[:, :]), inp['valid_sb'],
              R=[valid_d], W=[inp['valid_sb']])
        P.NT = 33
        ctx0 = P.moe_ctx(S, es, 0, inp)
        P.mixer0(S, inp, ctx0, 0)
        P.experts(S, inp, ctx0, 0)
        P.combine_qkv(S, inp, ctx0, 1, outs)
        P.NT = 32
        def rest_of_exchange():
            for g_ in ('B', 'C', 'D'):
                issue(deferred[g_])
        P.attention(S, inp, es, after_init=rest_of_exchange)
        ctx1 = P.moe_ctx(S, es, 1, inp)
        P.oproj_route(S, inp, ctx1, 2)
        P.experts(S, inp, ctx1, 1)
        with ExitStack() as es2:
            R = P.combine_bufs(S, es2)
            gb = S.sb(es2, 'gb2', [128, 2, D], F32)
            P.load_gb(S, gb, inp['ln_g'], inp['ln_b'], 3)
            ho = Ring([S.sb(es2, 'ho', [128, D], F32) for _ in range(2)])
            for i in range(32):
                h = ho.next()
                P.combine_tile(S, R, i, ctx1, gb, h)
                S.dma('sp', lambda e, h=h, i=i: e.dma_start(out=out[ts(i, 128), :], in_=h[:, :]), h,
                      R=[h], W=[out], join=True)
            S.barrier()
    return P


CFG = dict(NT=33, NSEG=8, CAP=768)


def make_maps(inputs):
    cf, cb = make_consts(CFG['CAP'])
    c2 = make_consts2()
    maps = []
    for c in range(8):
        cl = c % 4
        maps.append(dict(
            xin=make_xin(inputs['x'], inputs['meta_tokens'], c), valid=make_valid(33), cst_f=cf, cst_b=cb, cst2=c2,
            maskT=make_mask(cl), idxq=make_idxq(cl), qone=make_qone(),
            pool_w=inputs['pool_w'], pool_scale=inputs['pool_scale'], ln_g=inputs['ln_g'].reshape(4, D),
            ln_b=inputs['ln_b'].reshape(4, D), router_w=inputs['router_w'], router_b=inputs['router_b'],
            w1=inputs['w1'], b1=inputs['b1'], w2=inputs['w2'], b2=inputs['b2'],
            attn_w_in=inputs['attn_w_in'], attn_b_f=inputs['attn_b_f'], attn_w_out=inputs['attn_w_out']))
    return maps


def assemble(res):
    out = np.zeros((2, SEQ, D), np.float32)
    for c in range(8):
        b, cl = c // 4, c % 4
        o = np.asarray(res[c]['out'], dtype=np.float32)
        for j in range(8):
            G = cl + 4 * j
            out[b, G * 512:(G + 1) * 512] = o[j * 512:(j + 1) * 512]
    return out


def kernel(**inputs):
    inputs = {k: np.ascontiguousarray(np.asarray(v)) for k, v in inputs.items()}
    P = build_fused(CFG)
    res = run_bass_kernel_spmd(P.nc, make_maps(inputs), core_ids=list(range(8))).results
    return assemble(res)
```

```python
import numpy as np
import ml_dtypes
from contextlib import ExitStack
import concourse.bass as bass
import concourse.mybir as mybir
from concourse.bass_utils import run_bass_kernel_spmd

F32 = mybir.dt.float32
BF16 = mybir.dt.bfloat16
I32 = mybir.dt.int32
ALU = mybir.AluOpType
AF = mybir.ActivationFunctionType

D = 1024
KC = 8
NE = 32
NH = 16
HD = 64
N_META = 16
SEQ = 16384
ALPHA = float((2 * 2) ** 0.25)
LN_EPS = 1e-5
BIG = float(2 ** 20)
NEG = -30000.0
CC_INC = 1


def ts(i, n):
    return slice(i * n, (i + 1) * n)


class DSem:
    def __init__(self, sem):
        self.sem = sem
        self.cnt = 0


class Buf:
    def __init__(self, name, t=None):
        self.name = name
        self.t = t
        self.w = {}
        self.r = {}
        self.dsem = None

    def __getitem__(self, idx):
        return self.t[idx]


class Sch:
    def __init__(self, nc, es, n_dsem=96):
        self.nc = nc
        self.es = es
        self.E = {'pe': nc.tensor, 'dve': nc.vector, 'act': nc.scalar, 'pool': nc.gpsimd, 'sp': nc.sync}
        self.sem = {k: es.enter_context(nc.semaphore('s_' + k)) for k in self.E}
        self.cnt = {k: 0 for k in self.E}
        self.seen = {k: {} for k in self.E}
        self.dpool = [DSem(es.enter_context(nc.semaphore('d%d' % i))) for i in range(n_dsem)]
        self.dfree = list(self.dpool)
        self.nbuf = 0

    def sb(self, es, name, shape, dt):
        self.nbuf += 1
        t = es.enter_context(self.nc.sbuf_tensor('%s_%d' % (name, self.nbuf), list(shape), dt))
        return Buf(name, t)

    def ps(self, es, name, shape, dt=F32):
        self.nbuf += 1
        t = es.enter_context(self.nc.psum_tensor('%s_%d' % (name, self.nbuf), list(shape), dt))
        return Buf(name, t)

    def view(self, name):
        return Buf(name)

    def _wait(self, eng, toks):
        best = {}
        for (sem, val, owner) in toks:
            if owner == 'pe' and eng == 'pe':
                continue
            k = id(sem)
            if self.seen[eng].get(k, 0) >= val:
                continue
            if k not in best or best[k][1] < val:
                best[k] = (sem, val)
        for k, (sem, val) in best.items():
            self.E[eng].wait_ge(sem, val)
            self.seen[eng][k] = val

    @staticmethod
    def _deps(reads, writes, skip_w=None):
        toks = []
        for b in reads:
            toks.extend(b.w.values())
        for b in writes:
            if b is not skip_w:
                toks.extend(b.w.values())
            toks.extend(b.r.values())
        return toks

    @staticmethod
    def _rec(tok, reads, writes, join=False):
        k = id(tok[0])
        for b in writes:
            if join:
                b.w[k] = tok
            else:
                b.w = {k: tok}
                b.r = {}
        for b in reads:
            b.r[k] = tok

    def op(self, eng, fn, R=(), W=()):
        self._wait(eng, self._deps(R, W))
        ins = fn(self.E[eng])
        self.cnt[eng] += 1
        ins.then_inc(self.sem[eng], 1)
        self._rec((self.sem[eng], self.cnt[eng], eng), R, W)
        return ins

    def _dsem_of(self, b):
        if b.dsem is None:
            b.dsem = self.dfree.pop()
        return b.dsem

    def dma(self, q, fn, sbuf, R=(), W=(), join=False):
        ds = self._dsem_of(sbuf)
        toks = self._deps(R, W)
        if join:
            toks = [t for t in toks if t[2] != 'dma']
        self._wait(q, toks)
        ins = fn(self.E[q])
        ds.cnt += 16
        ins.then_inc(ds.sem, 16)
        self._rec((ds.sem, ds.cnt, 'dma'), R, W, join=join)
        return ins

    def coll(self, kind, groups, src, dst, src_ap=None, dst_ap=None):
        ds = self._dsem_of(dst)
        self._wait('pool', self._deps([src], [dst]))
        ins = self.nc.gpsimd.collective_compute(kind, ALU.bypass, replica_groups=groups,
                                                ins=[src.t if src_ap is None else src_ap],
                                                outs=[dst.t if dst_ap is None else dst_ap])
        ds.cnt += CC_INC
        ins.then_inc(ds.sem, CC_INC)
        self._rec((ds.sem, ds.cnt, 'dma'), [src], [dst])
        return ins

    def release(self, bufs):
        for b in bufs:
            if b.dsem is not None:
                self.dfree.append(b.dsem)
                b.dsem = None

    def barrier(self):
        toks = [(self.sem[k], self.cnt[k], k + '_b') for k in self.E if self.cnt[k] > 0]
        toks += [(d.sem, d.cnt, 'dma') for d in self.dpool if d.cnt > 0 and not getattr(d, 'nobar', False)]
        for e in self.E:
            self._wait(e, toks)


class Ring:
    def __init__(self, bufs):
        self.bufs = bufs
        self.i = -1

    def next(self):
        self.i = (self.i + 1) % len(self.bufs)
        return self.bufs[self.i]


class Prog:
    def __init__(self, cfg):
        self.cfg = cfg
        self.NT = cfg['NT']
        self.NSEG = cfg['NSEG']
        self.CAP = cfg['CAP']
        self.CT = self.CAP // 128
        self.NE = cfg.get('NE', NE)
        self.nc = bass.Bass("TRN2", target_bir_lowering=False)
        self.dram = {}

    def dt(self, name, shape, dtype, kind):
        t = self.nc.dram_tensor(name, list(shape), dtype, kind=kind)
        self.dram[name] = t
        b = Buf(name, t.ap())
        return b

    def consts(self, S, es):
        c = {}
        cin = self.dt('cst_f', [128, 128 * 2 + 32 + 128 * 4], F32, 'ExternalInput')
        cb = self.dt('cst_b', [128, 128 * 3], BF16, 'ExternalInput')
        c['f'] = S.sb(es, 'cst_f', [128, 128 * 2 + 32 + 512], F32)
        c['b'] = S.sb(es, 'cst_b', [128, 384], BF16)
        S.dma('sp', lambda e: e.dma_start(out=c['f'][:], in_=cin[:, :]), c['f'], R=[cin], W=[c['f']])
        S.dma('sp', lambda e: e.dma_start(out=c['b'][:], in_=cb[:, :]), c['b'], R=[cb], W=[c['b']])
        c['ident'] = c['f'].t[:, 0:128]
        c['ecol'] = c['f'].t[:, 256:288]
        c['rcnt'] = c['f'].t[:, 288:800]
        c['identb'] = c['b'].t[:, 0:128]
        c['U'] = c['b'].t[:, 128:256]
        c['ones'] = c['b'].t[:, 256:384]
        self.c = c
        return c

    def ln_tile(self, S, R, src_ap, src_buf, gb, out_buf):
        st = R['st'].next(); mv = R['mv'].next(); sd = R['sd'].next(); hn = R['hn'].next()
        for k in range(2):
            S.op('dve', lambda e, k=k: e.bn_stats(out=st[:, ts(k, 6)], in_=src_ap[:, ts(k, 512)]), R=[src_buf], W=[st])
        S.op('dve', lambda e: e.bn_aggr(out=mv[:, :], in_=st[:, :]), R=[st], W=[mv])
        S.op('dve', lambda e: e.tensor_scalar(out=sd[:, 0:1], in0=mv[:, 1:2], scalar1=LN_EPS, scalar2=None,
                                              op0=ALU.add), R=[mv], W=[sd])
        S.op('act', lambda e: e.activation(out=sd[:, 1:2], in_=sd[:, 0:1], func=AF.Sqrt), R=[sd], W=[sd])
        S.op('dve', lambda e: e.reciprocal(out=sd[:, 2:3], in_=sd[:, 1:2]), R=[sd], W=[sd])
        S.op('dve', lambda e: e.tensor_scalar(out=hn[:, :], in0=src_ap, scalar1=mv[:, 0:1], scalar2=sd[:, 2:3],
                                              op0=ALU.subtract, op1=ALU.mult), R=[src_buf, mv, sd], W=[hn])
        S.op('pool', lambda e: e.tensor_tensor(out=hn[:, :], in0=hn[:, :], in1=gb[:, 0, :], op=ALU.mult),
             R=[hn, gb], W=[hn])
        S.op('dve', lambda e: e.tensor_tensor(out=out_buf[:, :], in0=hn[:, :], in1=gb[:, 1, :], op=ALU.add),
             R=[hn, gb], W=[out_buf])

    def load_gb(self, S, gbuf, lng, lnb, idx):
        S.dma('sp', lambda e: e.dma_start(out=gbuf[:, 0, :], in_=lng[idx:idx + 1, :].to_broadcast([128, D])),
              gbuf, R=[lng], W=[gbuf])
        S.dma('sp', lambda e: e.dma_start(out=gbuf[:, 1, :], in_=lnb[idx:idx + 1, :].to_broadcast([128, D])),
              gbuf, R=[lnb], W=[gbuf], join=True)

    def route_tile(self, S, R, i, h_buf, ctx):
        c = self.c
        CAP = self.CAP
        H, XS = ctx['H'], ctx['XS']
        dest4, gate4, valid = ctx['dest4'], ctx['gate4'], ctx['valid']
        rw, rbb = ctx['rw'], ctx['rbb']
        S.dma('sp', lambda e: e.dma_start(out=H[ts(i, 128), :], in_=h_buf[:, :]), h_buf, R=[h_buf], W=[H], join=True)
        hb = R['hb'].next()
        S.op('act', lambda e: e.activation(out=hb[:, :], in_=h_buf[:, :], func=AF.Copy), R=[h_buf], W=[hb])
        tp = R['big'].next()
        for kc in range(KC):
            S.op('pe', lambda e, kc=kc: e.transpose(out=tp[:, ts(kc, 128)], in_=h_buf[:, ts(kc, 128)],
                                                    identity=c['ident']), R=[h_buf, c['f']], W=[tp])
        hT = R['hT'].next()
        S.op('act', lambda e: e.activation(out=hT[:, :], in_=tp[:, :], func=AF.Copy), R=[tp], W=[hT])
        lgp = R['lgp'].next()
        for kc in range(KC):
            S.op('pe', lambda e, kc=kc: e.matmul(lgp[:, 0:32], lhsT=hT[:, ts(kc, 128)], rhs=rw[:, kc, :],
                                                 start=(kc == 0), stop=(kc == KC - 1)), R=[hT, rw], W=[lgp])
        sm = R['sm'].next()
        lg = sm[:, 0:32]; top8 = sm[:, 32:40]
        S.op('dve', lambda e: e.tensor_tensor(out=lg, in0=lgp[:, 0:32], in1=rbb[:, :], op=ALU.add), R=[lgp, rbb], W=[sm])
        S.op('dve', lambda e: e.max(out=top8, in_=lg), R=[sm], W=[sm])
        S.op('dve', lambda e: e.tensor_scalar(out=sm[:, 40:41], in0=sm[:, 32:33], scalar1=-1.0, scalar2=None,
                                              op0=ALU.mult), R=[sm], W=[sm])
        S.op('act', lambda e: e.activation(out=sm[:, 44:48], in_=sm[:, 32:36], func=AF.Exp, bias=sm[:, 40:41],
                                           scale=1.0), R=[sm], W=[sm])
        S.op('dve', lambda e: e.tensor_reduce(out=sm[:, 41:42], in_=sm[:, 44:48], axis=mybir.AxisListType.X,
                                              op=ALU.add), R=[sm], W=[sm])
        S.op('dve', lambda e: e.reciprocal(out=sm[:, 42:43], in_=sm[:, 41:42]), R=[sm], W=[sm])
        mb = R['mb'].next()
        S.op('dve', lambda e: e.tensor_scalar(out=mb[:, :], in0=lg, scalar1=sm[:, 35:36], scalar2=valid[:, i:i + 1],
                                              op0=ALU.is_ge, op1=ALU.mult), R=[sm, valid], W=[mb])
        p12 = lgp
        S.op('pe', lambda e: e.matmul(p12[:, 32:64], lhsT=c['U'], rhs=mb[:, :], start=True, stop=True),
             R=[mb, c['b']], W=[p12])
        S.op('pe', lambda e: e.matmul(p12[:, 64:96], lhsT=c['ones'], rhs=mb[:, :], start=True, stop=True),
             R=[mb, c['b']], W=[p12])
        cnt_old = ctx['cnt'][ctx['cnt_i'] % 2]
        cnt_new = ctx['cnt'][(ctx['cnt_i'] + 1) % 2]
        ctx['cnt_i'] += 1
        S.op('dve', lambda e: e.tensor_tensor(out=sm[:, 64:96], in0=p12[:, 32:64], in1=cnt_old[:, :], op=ALU.add),
             R=[p12, cnt_old], W=[sm])
        S.op('dve', lambda e: e.tensor_tensor(out=cnt_new[:, :], in0=p12[:, 64:96], in1=cnt_old[:, :], op=ALU.add),
             R=[p12, cnt_old], W=[cnt_new])
        S.op('dve', lambda e: e.tensor_scalar(out=sm[:, 96:128], in0=sm[:, 64:96], scalar1=float(CAP), scalar2=None,
                                              op0=ALU.is_lt), R=[sm], W=[sm])
        S.op('dve', lambda e: e.tensor_tensor(out=sm[:, 128:160], in0=sm[:, 64:96], in1=c['ecol'], op=ALU.add),
             R=[sm, c['f']], W=[sm])
        S.op('dve', lambda e: e.scalar_tensor_tensor(out=sm[:, 160:192], in0=sm[:, 128:160], scalar=valid[:, i:i + 1],
                                                     in1=sm[:, 96:128], op0=ALU.mult, op1=ALU.mult),
             R=[sm, valid], W=[sm])
        dk = R['dk'].next()
        for k in range(4):
            S.op('dve', lambda e, k=k: e.scalar_tensor_tensor(out=sm[:, 192:224], in0=lg, scalar=sm[:, 32 + k:33 + k],
                                                              in1=sm[:, 160:192], op0=ALU.is_equal, op1=ALU.mult,
                                                              accum_out=dk[:, k:k + 1]), R=[sm], W=[sm, dk])
        S.op('dve', lambda e: e.tensor_scalar(out=dk[:, 4:8], in0=dk[:, 0:4], scalar1=-0.5, scalar2=None,
                                              op0=ALU.is_lt), R=[dk], W=[dk])
        S.op('dve', lambda e: e.scalar_tensor_tensor(out=gate4[:, 4 * i:4 * i + 4], in0=sm[:, 44:48], scalar=sm[:, 42:43],
                                                     in1=dk[:, 4:8], op0=ALU.mult, op1=ALU.mult),
             R=[sm, dk], W=[gate4])
        S.op('dve', lambda e: e.tensor_scalar(out=dest4[:, 4 * i:4 * i + 4], in0=dk[:, 0:4], scalar1=BIG, scalar2=None,
                                              op0=ALU.add), R=[dk], W=[dest4])
        for k in range(4):
            S.dma('pool', lambda e, k=k: e.indirect_dma_start(
                out=XS[:, :], out_offset=bass.IndirectOffsetOnAxis(ap=dest4[:, 4 * i + k:4 * i + k + 1], axis=0),
                in_=hb[:, :], in_offset=None, bounds_check=self.bc_reg, oob_is_err=False),
                hb, R=[hb, dest4], W=[XS], join=True)

    def route_bufs(self, S, es):
        R = {}
        R['hb'] = Ring([S.sb(es, 'hb', [128, D], BF16) for _ in range(2)])
        R['hT'] = Ring([S.sb(es, 'hT', [128, D], F32) for _ in range(2)])
        R['sm'] = Ring([S.sb(es, 'sm', [128, 256], F32) for _ in range(2)])
        R['mb'] = Ring([S.sb(es, 'mb', [128, 32], BF16) for _ in range(2)])
        R['dk'] = Ring([S.sb(es, 'dk', [128, 8], F32) for _ in range(2)])
        R['lgp'] = Ring([S.ps(es, 'lgp', [128, 512])])
        return R

    def ln_bufs(self, S, es, R):
        R['st'] = Ring([S.sb(es, 'st', [128, 12], F32) for _ in range(2)])
        R['mv'] = Ring([S.sb(es, 'mv', [128, 2], F32) for _ in range(2)])
        R['sd'] = Ring([S.sb(es, 'sd', [128, 4], F32) for _ in range(2)])
        R['hn'] = Ring([S.sb(es, 'hn', [128, D], F32) for _ in range(2)])
        return R

    def moe_ctx(self, S, es, layer, inp):
        ctx = {}
        ctx['dest4'] = S.sb(es, 'dest4', [128, self.NT * 4], I32)
        ctx['gate4'] = S.sb(es, 'gate4', [128, self.NT * 4], F32)
        ctx['cnt'] = [S.sb(es, 'cnt', [128, 32], F32) for _ in range(2)]
        ctx['cnt_i'] = 0
        S.op('dve', lambda e: e.memset(ctx['cnt'][0][:, :], 0.0), W=[ctx['cnt'][0]])
        ctx['rw'] = S.sb(es, 'rw', [128, KC, 32], F32)
        ctx['rbb'] = S.sb(es, 'rbb', [128, 32], F32)
        rw_d, rb_d = inp['router_w'], inp['router_b']
        S.dma('sp', lambda e: e.dma_start(out=ctx['rw'][:, :, :],
                                          in_=rw_d[layer].rearrange("(kc p) e -> p kc e", p=128)),
              ctx['rw'], R=[rw_d], W=[ctx['rw']])
        S.dma('sp', lambda e: e.dma_start(out=ctx['rbb'][:, :], in_=rb_d[layer:layer + 1, :].to_broadcast([128, 32])),
              ctx['rbb'], R=[rb_d], W=[ctx['rbb']])
        ctx['valid'] = inp['valid_sb']
        if not hasattr(self, 'bc_reg'):
            self.bc_reg = self.nc.gpsimd.to_reg(self.NE * self.CAP - 1)
        ctx['H'] = inp['H']; ctx['XS'] = inp['XS']; ctx['YS'] = inp['YS']
        return ctx

    def mixer0(self, S, inp, ctx, gidx):
        nc = self.nc
        c = self.c
        xin = inp['xin']
        with ExitStack() as es:
            R = self.route_bufs(S, es)
            self.ln_bufs(S, es, R)
            R['big'] = Ring([S.ps(es, 'big', [128, D]) for _ in range(2)])
            mm = Ring([S.ps(es, 'mm', [128, 512]) for _ in range(2)])
            tph = S.ps(es, 'tph', [128, KC, 16])
            xt = Ring([S.sb(es, 'xt', [128, 4, D], F32) for _ in range(2)])
            xh = Ring([S.sb(es, 'xh', [16, D], F32) for _ in range(2)])
            xT = S.sb(es, 'xT', [128, KC, 528], F32)
            xTk = [S.view('xT%d' % k) for k in range(KC)]
            sA = Ring([S.sb(es, 'sA', [128, 528], F32) for _ in range(2)])
            sB = Ring([S.sb(es, 'sB', [128, 528], F32) for _ in range(2)])
            uT = S.sb(es, 'uT', [128, KC, 512], BF16)
            uTk = [S.view('uT%d' % k) for k in range(KC)]
            rT = S.sb(es, 'rT', [128, KC, 512], F32)
            rTk = [S.view('rT%d' % k) for k in range(KC)]
            tmp = Ring([S.sb(es, 'tmp', [128, 512], F32) for _ in range(2)])
            h1 = Ring([S.sb(es, 'h1', [128, D], F32) for _ in range(2)])
            pw = S.sb(es, 'pw', [128, 4, 2, 256], BF16)
            psc = S.sb(es, 'psc', [128, KC], F32)
            gb = S.sb(es, 'gb', [128, 2, D], F32)
            pw_d, ps_d = inp['pool_w'], inp['pool_scale']
            S.dma('pool', lambda e: e.dma_start(out=pw[:, :, :, :],
                                                in_=pw_d[0].rearrange("g (ic p) d -> p g ic d", p=128)),
                  pw, R=[pw_d], W=[pw])
            with nc.allow_non_contiguous_dma(reason="tiny per-partition vector"):
                S.dma('sp', lambda e: e.dma_start(out=psc[:, :], in_=ps_d[0].rearrange("(kc p) -> p kc", p=128)),
                      psc, R=[ps_d], W=[psc])
            self.load_gb(S, gb, inp['ln_g'], inp['ln_b'], gidx)

            for seg in range(self.NSEG + 1):
                meta = (seg == self.NSEG)
                ntile = 1 if meta else 4
                W = ntile * 128
                base = seg * 528
                x_t = xt.next(); x_h = xh.next()
                S.dma('sp', lambda e: e.dma_start(out=x_h[:, :], in_=xin[base:base + 16, :]), x_h, R=[xin], W=[x_h])
                S.dma('sp', lambda e: e.dma_start(
                    out=x_t[:, 0:ntile, :], in_=xin[base + 16:base + 16 + W, :].rearrange("(t p) d -> p t d", p=128)),
                    x_t, R=[xin], W=[x_t])
                for kc in range(KC):
                    S.op('pe', lambda e, kc=kc: e.transpose(out=tph[:, kc, :], in_=x_h[:, ts(kc, 128)],
                                                            identity=c['ident'][0:16, 0:16]), R=[x_h, c['f']], W=[tph])
                for kc in range(KC):
                    S.op('act', lambda e, kc=kc: e.activation(out=xT[:, kc, 0:16], in_=tph[:, kc, :], func=AF.Copy),
                         R=[tph], W=[xTk[kc]])
                for kc in range(KC):
                    p = mm.next()
                    for t in range(ntile):
                        S.op('pe', lambda e, kc=kc, t=t: e.transpose(out=p[:, ts(t, 128)], in_=x_t[:, t, ts(kc, 128)],
                                                                     identity=c['ident']), R=[x_t, c['f']], W=[p])
                    S.op('act', lambda e, kc=kc: e.activation(out=xT[:, kc, 16:16 + W], in_=p[:, 0:W], func=AF.Copy),
                         R=[p], W=[xTk[kc]])
                for kc in range(KC):
                    g = kc // 2
                    w = 2 << g
                    cur_ap = xT[:, kc, :]
                    cur_buf = xTk[kc]
                    lo = 16 - (w - 1)
                    sh = 1
                    start = 1
                    bufs = [sA.next(), sB.next()]
                    bi = 0
                    first = 0
                    while sh < w:
                        first = first + sh
                        nb = bufs[bi]; bi ^= 1
                        S.op('dve', lambda e, nb=nb, cur_ap=cur_ap, first=first, sh=sh, W=W: e.tensor_tensor(
                            out=nb[:, first:16 + W], in0=cur_ap[:, first:16 + W], in1=cur_ap[:, first - sh:16 + W - sh],
                            op=ALU.add), R=[cur_buf], W=[nb])
                        cur_ap = nb[:, :]; cur_buf = nb
                        sh *= 2
                    if not meta:
                        S.op('dve', lambda e, kc=kc, cur_ap=cur_ap, w=w, W=W: e.scalar_tensor_tensor(
                            out=uT[:, kc, 0:W], in0=cur_ap[:, 16:16 + W], scalar=1.0 / w, in1=xT[:, kc, 16:16 + W],
                            op0=ALU.mult, op1=ALU.subtract), R=[cur_buf, xTk[kc]], W=[uTk[kc]])
                    else:
                        nb = bufs[bi]
                        S.op('dve', lambda e, nb=nb, cur_ap=cur_ap, g=g, W=W: e.tensor_tensor(
                            out=nb[:, 16:16 + W], in0=cur_ap[:, 16:16 + W], in1=c['rcnt'][:, ts(g, 128)], op=ALU.mult),
                            R=[cur_buf, c['f']], W=[nb])
                        S.op('dve', lambda e, nb=nb, kc=kc, W=W: e.tensor_tensor(
                            out=uT[:, kc, 0:W], in0=nb[:, 16:16 + W], in1=xT[:, kc, 16:16 + W], op=ALU.subtract),
                            R=[nb, xTk[kc]], W=[uTk[kc]])
                for oc8 in range(KC):
                    g = oc8 // 2
                    oc = oc8 % 2
                    p = mm.next()
                    for ic in range(2):
                        S.op('pe', lambda e, g=g, oc=oc, ic=ic, p=p, W=W: e.matmul(
                            p[:, 0:W], lhsT=pw[:, g, ic, ts(oc, 128)], rhs=uT[:, 2 * g + ic, 0:W],
                            start=(ic == 0), stop=(ic == 1)), R=[pw, uTk[2 * g + ic]], W=[p])
                    tm = tmp.next()
                    S.op('act', lambda e, p=p, tm=tm, oc8=oc8, W=W: e.activation(
                        out=tm[:, 0:W], in_=p[:, 0:W], func=AF.Copy, scale=psc[:, oc8:oc8 + 1]), R=[p, psc], W=[tm])
                    S.op('dve', lambda e, tm=tm, oc8=oc8, W=W: e.scalar_tensor_tensor(
                        out=rT[:, oc8, 0:W], in0=xT[:, oc8, 16:16 + W], scalar=ALPHA, in1=tm[:, 0:W],
                        op0=ALU.mult, op1=ALU.add), R=[xTk[oc8], tm], W=[rTk[oc8]])
                for t in range(ntile):
                    i = seg * 4 + t
                    rp = R['big'].next()
                    for kc in range(KC):
                        S.op('pe', lambda e, kc=kc, t=t, rp=rp: e.transpose(
                            out=rp[:, ts(kc, 128)], in_=rT[:, kc, ts(t, 128)], identity=c['ident']),
                            R=[rTk[kc], c['f']], W=[rp])
                    h = h1.next()
                    self.ln_tile(S, R, rp[:, :], rp, gb, h)
                    self.route_tile(S, R, i, h, ctx)
            S.barrier()
            S.release([b for r in R.values() for b in r.bufs] + xt.bufs + xh.bufs + [pw, psc, gb])

    def experts(self, S, inp, ctx, layer):
        nc = self.nc
        layer = layer - getattr(self, 'wbase', 0)
        c = self.c
        CAP, CT = self.CAP, self.CT
        XS, YS = ctx['XS'], ctx['YS']
        w1_d, w2_d, b1_d, b2_d = inp['w1'], inp['w2'], inp['b1'], inp['b2']
        cgs = []
        o = 0
        while o < CAP:
            n = min(512, CAP - o)
            cgs.append((o, n))
            o += n
        with ExitStack() as es:
            tpb = Ring([S.ps(es, 'tpb', [128, KC, 128], BF16) for _ in range(2)])
            hp = Ring([S.ps(es, 'hp', [128, 512]) for _ in range(4)])
            yp = Ring([S.ps(es, 'yp', [128, 512]) for _ in range(2)])
            b1T = S.sb(es, 'b1T', [128, 16, 32], F32)
            with ExitStack() as esb:
                b1r = S.sb(esb, 'b1r', [32, 2 * D], F32)
                S.dma('sp', lambda e: e.dma_start(out=b1r[:, :], in_=b1_d[layer]), b1r, R=[b1_d], W=[b1r])
                for fc in range(16):
                    p = yp.next()
                    S.op('pe', lambda e, fc=fc, p=p: e.transpose(out=p[:, 0:32], in_=b1r[:, ts(fc, 128)],
                                                                 identity=c['ident'][0:32, 0:32]), R=[b1r, c['f']], W=[p])
                    S.op('dve', lambda e, fc=fc, p=p: e.tensor_copy(out=b1T[:, fc, :], in_=p[:, 0:32]), R=[p], W=[b1T])
                S.barrier()
                S.release([b1r])
            w1b = Ring([S.sb(es, 'w1b', [128, KC, 2 * D], BF16) for _ in range(2)])
            w2b = Ring([S.sb(es, 'w2b', [128, KC, D], BF16) for _ in range(2)])
            b2b = Ring([S.sb(es, 'b2b', [128, D], F32) for _ in range(2)])
            xst = Ring([S.sb(es, 'xst', [128, CT, D], BF16) for _ in range(2)])
            xeT = Ring([S.sb(es, 'xeT', [128, KC, CAP], BF16) for _ in range(2)])
            aT = S.sb(es, 'aT', [128, KC, CAP], BF16)
            aTk = [S.view('aT%d' % k) for k in range(KC)]
            ys = Ring([S.sb(es, 'ys', [128, D], F32) for _ in range(2)])
            gc = Ring([S.sb(es, 'gc', [128, 512], F32) for _ in range(3)])
            sg = Ring([S.sb(es, 'sg', [128, 512], F32) for _ in range(3)])
            u0 = Ring([S.sb(es, 'u0', [128, 512], F32) for _ in range(3)])

            def load_w(ex):
                a = w1b.next(); b = w2b.next(); bb = b2b.next()
                for hlf in range(2):
                    S.dma('pool', lambda e, hlf=hlf: e.dma_start(
                        out=a[:, ts(hlf, 4), :],
                        in_=w1_d[layer, ex, ts(hlf, 512), :].rearrange("(kc p) f -> p kc f", p=128)),
                        a, R=[w1_d], W=[a], join=(hlf == 1))
                S.dma('pool', lambda e: e.dma_start(
                    out=b[:, :, :], in_=w2_d[layer, ex].rearrange("(kc p) f -> p kc f", p=128)), b, R=[w2_d], W=[b])
                S.dma('sp', lambda e: e.dma_start(out=bb[:, :], in_=b2_d[layer, ex:ex + 1, :].to_broadcast([128, D])),
                      bb, R=[b2_d], W=[bb])
                return a, b, bb

            def load_x(ex):
                xs_ = xst.next()
                S.dma('sp', lambda e: e.dma_start(
                    out=xs_[:, :, :], in_=XS[ex * CAP:(ex + 1) * CAP, :].rearrange("(ct p) d -> p ct d", p=128)),
                    xs_, R=[XS], W=[xs_])
                return xs_

            nxt_w = load_w(0)
            nxt_x = load_x(0)
            for ex in range(self.NE):
                wa, wb_, bb = nxt_w
                xs_ = nxt_x
                if ex + 1 < self.NE:
                    nxt_w = load_w(ex + 1)
                    nxt_x = load_x(ex + 1)
                xT_ = xeT.next()
                for ct in range(CT):
                    p = tpb.next()
                    for kc in range(KC):
                        S.op('pe', lambda e, ct=ct, kc=kc, p=p: e.transpose(
                            out=p[:, kc, :], in_=xs_[:, ct, ts(kc, 128)], identity=c['identb']),
                            R=[xs_, c['b']], W=[p])
                    eng = 'act' if ct % 2 == 0 else 'dve'
                    if eng == 'act':
                        S.op('act', lambda e, ct=ct, p=p: e.activation(out=xT_[:, :, ts(ct, 128)], in_=p[:, :, :],
                                                                        func=AF.Copy), R=[p], W=[xT_])
                    else:
                        S.op('dve', lambda e, ct=ct, p=p: e.tensor_copy(out=xT_[:, :, ts(ct, 128)], in_=p[:, :, :]),
                             R=[p], W=[xT_])
                def stage_a(j, o, n):
                    pa = hp.next(); pb = hp.next()
                    for kc in range(KC):
                        S.op('pe', lambda e, kc=kc: e.matmul(
                            pa[:, 0:n], lhsT=wa[:, kc, ts(j, 128)], rhs=xT_[:, kc, o:o + n],
                            start=(kc == 0), stop=(kc == KC - 1)), R=[wa, xT_], W=[pa])
                    for kc in range(KC):
                        S.op('pe', lambda e, kc=kc: e.matmul(
                            pb[:, 0:n], lhsT=wa[:, kc, ts(8 + j, 128)], rhs=xT_[:, kc, o:o + n],
                            start=(kc == 0), stop=(kc == KC - 1)), R=[wa, xT_], W=[pb])
                    g_ = gc.next(); s_ = sg.next(); u_ = u0.next()
                    S.op('dve', lambda e: e.tensor_scalar(
                        out=g_[:, 0:n], in0=pa[:, 0:n], scalar1=b1T[:, j, ex:ex + 1], scalar2=7.0,
                        op0=ALU.add, op1=ALU.min), R=[pa, b1T], W=[g_])
                    S.op('act', lambda e: e.activation(
                        out=u_[:, 0:n], in_=pb[:, 0:n], func=AF.Identity, bias=b1T[:, 8 + j, ex:ex + 1], scale=1.0),
                        R=[pb, b1T], W=[u_])
                    S.op('act', lambda e: e.activation(
                        out=s_[:, 0:n], in_=g_[:, 0:n], func=AF.Sigmoid, scale=1.702), R=[g_], W=[s_])
                    return (j, o, n, g_, s_, u_)

                def stage_b(st):
                    j, o, n, g_, s_, u_ = st
                    S.op('dve', lambda e: e.tensor_scalar(
                        out=u_[:, 0:n], in0=u_[:, 0:n], scalar1=7.0, scalar2=-7.0, op0=ALU.min, op1=ALU.max),
                        R=[u_], W=[u_])
                    S.op('pool', lambda e: e.tensor_tensor(
                        out=s_[:, 0:n], in0=g_[:, 0:n], in1=s_[:, 0:n], op=ALU.mult), R=[g_, s_], W=[s_])
                    S.op('dve', lambda e: e.scalar_tensor_tensor(
                        out=aT[:, j, o:o + n], in0=u_[:, 0:n], scalar=1.0, in1=s_[:, 0:n],
                        op0=ALU.add, op1=ALU.mult), R=[u_, s_], W=[aTk[j]])

                prev = None
                for j in range(KC):
                    for (o, n) in cgs:
                        cur = stage_a(j, o, n)
                        if prev is not None:
                            stage_b(prev)
                        prev = cur
                stage_b(prev)
                for ct in range(CT):
                    y_ = ys.next()
                    for dh in range(2):
                        p = yp.next()
                        for fc in range(KC):
                            S.op('pe', lambda e, fc=fc, p=p, ct=ct, dh=dh: e.matmul(
                                p[:, :], lhsT=aT[:, fc, ts(ct, 128)], rhs=wb_[:, fc, ts(dh, 512)],
                                start=(fc == 0), stop=(fc == KC - 1)), R=[aTk[fc], wb_], W=[p])
                        S.op('dve', lambda e, p=p, y_=y_, dh=dh: e.tensor_tensor(
                            out=y_[:, ts(dh, 512)], in0=p[:, :], in1=bb[:, ts(dh, 512)], op=ALU.add),
                            R=[p, bb], W=[y_])
                    r0 = ex * CAP + ct * 128
                    S.dma('sp', lambda e, y_=y_, r0=r0: e.dma_start(out=YS[r0:r0 + 128, :], in_=y_[:, :]),
                          y_, R=[y_], W=[YS], join=True)
            S.barrier()
            S.release(w1b.bufs + w2b.bufs + b2b.bufs + xst.bufs + ys.bufs)

    def combine_tile(self, S, R, i, ctx, gb, out_buf):
        H, YS = ctx['H'], ctx['YS']
        dest4, gate4 = ctx['dest4'], ctx['gate4']
        hres = R['hres'].next()
        S.dma('sp', lambda e: e.dma_start(out=hres[:, :], in_=H[ts(i, 128), :]), hres, R=[H], W=[hres])
        yk = []
        for k in range(4):
            y = R['yk'].next()
            S.dma('pool', lambda e, y=y, k=k: e.indirect_dma_start(
                out=y[:, :], out_offset=None, in_=YS[:, :],
                in_offset=bass.IndirectOffsetOnAxis(ap=dest4[:, 4 * i + k:4 * i + k + 1], axis=0),
                bounds_check=self.bc_reg, oob_is_err=False), y, R=[YS, dest4], W=[y])
            yk.append(y)
        acc = R['acc'].next()
        S.op('act', lambda e: e.activation(out=acc[:, :], in_=hres[:, :], func=AF.Copy, scale=ALPHA),
             R=[hres], W=[acc])
        for k in range(4):
            S.op('dve', lambda e, k=k: e.scalar_tensor_tensor(
                out=acc[:, :], in0=yk[k][:, :], scalar=gate4[:, 4 * i + k:4 * i + k + 1], in1=acc[:, :],
                op0=ALU.mult, op1=ALU.add), R=[yk[k], gate4, acc], W=[acc])
        self.ln_tile(S, R, acc[:, :], acc, gb, out_buf)

    def combine_bufs(self, S, es):
        R = {}
        self.ln_bufs(S, es, R)
        R['hres'] = Ring([S.sb(es, 'hres', [128, D], F32) for _ in range(2)])
        R['yk'] = Ring([S.sb(es, 'yk', [128, D], F32) for _ in range(8)])
        R['acc'] = Ring([S.sb(es, 'acc', [128, D], F32) for _ in range(2)])
        for y in R['yk'].bufs:
            S.op('dve', lambda e, y=y: e.memset(y[:, :], 0.0), W=[y])
        return R

    def combine_qkv(self, S, inp, ctx, gidx, outs):
        nc = self.nc
        c = self.c
        H2, QT, KT, VV, LF = outs['H2'], outs['QT'], outs['KT'], outs['V'], outs['LF']
        win_d, bf_d = inp['attn_w_in'], inp['attn_b_f']
        with ExitStack() as es:
            R = self.combine_bufs(S, es)
            gb = S.sb(es, 'gb2', [128, 2, D], F32)
            self.load_gb(S, gb, inp['ln_g'], inp['ln_b'], gidx)
            ho = Ring([S.sb(es, 'ho', [128, D], F32) for _ in range(2)])
            big = Ring([S.ps(es, 'big', [128, D]) for _ in range(2)])
            mm = Ring([S.ps(es, 'mm', [128, 512]) for _ in range(3)])
            fps = S.ps(es, 'fps', [16, 512])
            win = S.sb(es, 'win', [128, KC, 3088], BF16)
            for hf in range(2):
                S.dma('pool', lambda e, hf=hf: e.dma_start(
                    out=win[:, :, ts(hf, 1544)],
                    in_=win_d[0, :, ts(hf, 1544)].rearrange("(kc p) f -> p kc f", p=128)),
                    win, R=[win_d], W=[win], join=(hf == 1))
            nbf = S.sb(es, 'nbf', [16, 2], F32)
            with nc.allow_non_contiguous_dma(reason="tiny per-partition vector"):
                S.dma('sp', lambda e: e.dma_start(out=nbf[:, 0:1], in_=bf_d[0].rearrange("(h o) -> h o", o=1)),
                      nbf, R=[bf_d], W=[nbf])
            S.op('dve', lambda e: e.tensor_scalar(out=nbf[:, 1:2], in0=nbf[:, 0:1], scalar1=-1.0, scalar2=None,
                                                  op0=ALU.mult), R=[nbf], W=[nbf])
            h2T = Ring([S.sb(es, 'h2T', [128, KC, 512], BF16) for _ in range(2)])
            qks = Ring([S.sb(es, 'qks', [128, 512], BF16) for _ in range(3)])
            vsb = Ring([S.sb(es, 'vsb', [128, NH, 65], BF16) for _ in range(2)])
            for v_ in vsb.bufs:
                S.op('pool', lambda e, v_=v_: e.memset(v_[:, :, :], 1.0), W=[v_])
            fsb = Ring([S.sb(es, 'fsb', [16, 2, 512], F32) for _ in range(2)])
            for seg in range(self.NSEG + 1):
                ntile = 1 if seg == self.NSEG else 4
                W = ntile * 128
                t0 = seg * 512
                hT_ = h2T.next()
                for t in range(ntile):
                    i = seg * 4 + t
                    h = ho.next()
                    self.combine_tile(S, R, i, ctx, gb, h)
                    S.dma('sp', lambda e, h=h, i=i: e.dma_start(out=H2[ts(i, 128), :], in_=h[:, :]), h,
                          R=[h], W=[H2], join=True)
                    tp = big.next()
                    for kc in range(KC):
                        S.op('pe', lambda e, kc=kc, tp=tp, h=h: e.transpose(
                            out=tp[:, ts(kc, 128)], in_=h[:, ts(kc, 128)], identity=c['ident']),
                            R=[h, c['f']], W=[tp])
                    S.op('act', lambda e, tp=tp, t=t: e.activation(
                        out=hT_[:, :, ts(t, 128)], in_=tp[:, :].rearrange("p (kc t) -> p kc t", kc=KC), func=AF.Copy),
                        R=[tp], W=[hT_])
                if seg > 0:
                    outs['exchange'](seg - 1)
                for m in range(16):
                    p = mm.next()
                    for kc in range(KC):
                        S.op('pe', lambda e, kc=kc, p=p, m=m: e.matmul(
                            p[:, 0:W], lhsT=win[:, kc, ts(m, 128)], rhs=hT_[:, kc, 0:W],
                            start=(kc == 0), stop=(kc == KC - 1)), R=[win, hT_], W=[p])
                    q_ = qks.next()
                    S.op('act', lambda e, p=p, q_=q_, m=m: e.activation(
                        out=q_[:, 0:W], in_=p[:, 0:W], func=AF.Copy, scale=(0.125 if m < 8 else 1.0)), R=[p], W=[q_])
                    for hh in range(2):
                        hd_ = 2 * (m % 8) + hh
                        if m < 8:
                            S.dma('sp', lambda e, q_=q_, hh=hh, hd_=hd_: e.dma_start(
                                out=QT[hd_, :, t0:t0 + W], in_=q_[ts(hh, 64), 0:W]), q_, R=[q_], W=[QT], join=True)
                        else:
                            kd = KT[seg][hd_ // 8]
                            S.dma('sp', lambda e, q_=q_, hh=hh, hd_=hd_, kd=kd: e.dma_start(
                                out=kd[ts(hd_ % 8, 64), 0:W], in_=q_[ts(hh, 64), 0:W]), q_, R=[q_], W=[kd], join=True)
                for t in range(ntile):
                    v_ = vsb.next()
                    for hf in range(2):
                        p = mm.next()
                        for kc in range(KC):
                            S.op('pe', lambda e, kc=kc, p=p, t=t, hf=hf: e.matmul(
                                p[:, :], lhsT=hT_[:, kc, ts(t, 128)], rhs=win[:, kc, 2048 + hf * 512:2560 + hf * 512],
                                start=(kc == 0), stop=(kc == KC - 1)), R=[win, hT_], W=[p])
                        S.op('dve', lambda e, p=p, v_=v_, hf=hf: e.tensor_copy(
                            out=v_[:, ts(hf, 8), 0:64], in_=p[:, :].rearrange("p (h d) -> p h d", d=64)),
                             R=[p], W=[v_])
                    for q4 in range(4):
                        vd = VV[seg][q4]
                        S.dma('sp', lambda e, v_=v_, t=t, q4=q4, vd=vd: e.dma_start(
                            out=vd.t.rearrange("(h p) (k d) -> p h k d", p=128, d=65)[:, :, t, :],
                            in_=v_[:, ts(q4, 4), :]), v_, R=[v_], W=[vd], join=True)
                for kc in range(KC):
                    S.op('pe', lambda e, kc=kc: e.matmul(
                        fps[:, 0:W], lhsT=win[:, kc, 3072:3088], rhs=hT_[:, kc, 0:W],
                        start=(kc == 0), stop=(kc == KC - 1)), R=[win, hT_], W=[fps])
                f_ = fsb.next()
                S.op('act', lambda e: e.activation(out=f_[:, 0, 0:W], in_=fps[:, 0:W], func=AF.Exp,
                                                   bias=nbf[:, 1:2], scale=-1.0), R=[fps, nbf], W=[f_])
                S.op('dve', lambda e: e.tensor_scalar(out=f_[:, 0, 0:W], in0=f_[:, 0, 0:W], scalar1=1.0, scalar2=None,
                                                      op0=ALU.add), R=[f_], W=[f_])
                S.op('act', lambda e: e.activation(out=f_[:, 1, 0:W], in_=f_[:, 0, 0:W], func=AF.Ln), R=[f_], W=[f_])
                S.op('dve', lambda e: e.tensor_scalar(out=f_[:, 1, 0:W], in0=f_[:, 1, 0:W], scalar1=-1.0, scalar2=None,
                                                      op0=ALU.mult), R=[f_], W=[f_])
                S.dma('sp', lambda e, f_=f_: e.dma_start(out=LF[:, t0:t0 + W], in_=f_[:, 1, 0:W]), f_,
                      R=[f_], W=[LF], join=True)
            outs['exchange'](self.NSEG)
            outs['exchange'](-1)
            S.barrier()

    def attention(self, S, inp, es_outer, after_init=None):
        nc = self.nc
        c = self.c
        NKB = 129
        CL = 2176
        QT, AT, CALL, QA = (inp[k] for k in ('QT', 'AT', 'CALL', 'QA'))
        RK, RV, RL, LFT = (inp[k] for k in ('RCVK', 'RCVV', 'RCVL', 'LFT'))
        c2d = inp['cst2']
        with ExitStack() as es:
            c2 = S.sb(es, 'c2', [128, 466], F32)
            S.dma('sp', lambda e: e.dma_start(out=c2[:, :], in_=c2d[:, :]), c2, R=[c2d], W=[c2])
            BT = c2.t[:, 0:128]; rowsel = c2.t[:, 128:256]; Dg = c2.t[:, 256:384]; padadd = c2.t[:, 384:385]
            E65 = c2.t[0:65, 385:449]
            oh16 = c2.t[:, 449:465]; ch0col = c2.t[:, 465:466]
            mk = S.sb(es, 'maskT', [128, 16, 512], BF16)
            S.dma('sp', lambda e: e.dma_start(out=mk[:, :, :], in_=inp['maskT'].t.rearrange("b p q -> p b q")),
                  mk, R=[inp['maskT']], W=[mk])
            idxq = S.sb(es, 'idxq', [128, 1], I32)
            S.dma('sp', lambda e: e.dma_start(out=idxq[:, :], in_=inp['idxq'][:, :]), idxq, R=[inp['idxq']], W=[idxq])
            cT = S.sb(es, 'cT', [128, 136, 16], F32)
            refbc = S.sb(es, 'refbc', [128, 128], F32)
            with ExitStack() as es1:
                lfa = S.sb(es1, 'lfa', [128, CL], F32)
                ones = S.sb(es1, 'onesf', [128, CL], F32)
                call = S.sb(es1, 'call', [128, CL], F32)
                sm = S.sb(es1, 'psm', [128, 8], F32)
                cq = S.sb(es1, 'cq', [128, 512], F32)
                wq = S.sb(es1, 'wq', [128, 3, 512], F32)
                qa = S.sb(es1, 'qa', [128, 2, 512], BF16)
                tmpd = S.sb(es1, 'tmpd', [128, 128], F32)
                psA = S.ps(es1, 'psA', [128, 512])
                psB = Ring([S.ps(es1, 'psB', [128, 512]) for _ in range(2)])
                with ExitStack() as es0:
                    lfh = S.sb(es0, 'lfh', [16, 17408], F32)
                    S.op('pool', lambda e: e.memset(lfh[:, :], 0.0), W=[lfh])
                    for cl in range(4):
                        S.dma('sp', lambda e, cl=cl: e.dma_start(
                            out=lfh.t[:, 128:16512].rearrange("h (j c t) -> h c j t", c=4, t=512)[:, cl],
                            in_=RL[cl * 16:(cl + 1) * 16, 0:4096].rearrange("h (j t) -> h j t", t=512)),
                            lfh, R=[RL], W=[lfh], join=(cl > 0))
                    S.dma('sp', lambda e: e.dma_start(out=lfh[:, 0:16], in_=RL[0:16, 4096:4112]), lfh,
                          R=[RL], W=[lfh], join=True)
                    S.dma('sp', lambda e: e.dma_start(out=LFT[:, :], in_=lfh[:, :]), lfh, R=[lfh], W=[LFT])
                    S.barrier()
                    S.release([lfh])
                S.dma('sp', lambda e: e.dma_start(out=lfa[:, :], in_=LFT.t.rearrange("h (ch c) -> (h ch) c", ch=8)),
                      lfa, R=[LFT], W=[lfa])
                S.op('pool', lambda e: e.memset(ones[:, :], 1.0), W=[ones])
                S.op('dve', lambda e: e.tensor_tensor_scan(out=call[:, :], data0=ones[:, :], data1=lfa[:, :], initial=0.0,
                                                           op0=ALU.mult, op1=ALU.add), R=[ones, lfa], W=[call])
                S.op('pe', lambda e: e.matmul(psA[:, 0:2], lhsT=BT, rhs=call[:, CL - 2:CL], start=True, stop=True),
                     R=[c2, call], W=[psA])
                S.op('dve', lambda e: e.tensor_copy(out=sm[:, 0:2], in_=psA[:, 0:2]), R=[psA], W=[sm])
                S.op('dve', lambda e: e.tensor_scalar(out=call[:, :], in0=call[:, :], scalar1=sm[:, 1:2], scalar2=None,
                                                      op0=ALU.add), R=[sm, call], W=[call])
                S.dma('sp', lambda e: e.dma_start(out=CALL.t.rearrange("h (ch c) -> (h ch) c", ch=8), in_=call[:, :]),
                      call, R=[call], W=[CALL])
                cT4 = cT.t.rearrange("p (ch bl) h -> p ch bl h", ch=8)
                for bl in range(17):
                    p = psB.next()
                    S.op('pe', lambda e, p=p, bl=bl: e.transpose(out=p[:, 0:128], in_=call[:, ts(bl, 128)],
                                                                 identity=c['ident']), R=[call, c['f']], W=[p])
                    S.op('act' if bl % 2 else 'dve',
                         (lambda e, p=p, bl=bl: e.activation(out=cT4[:, :, bl, :],
                                                             in_=p[:, 0:128].rearrange("p (h ch) -> p ch h", ch=8),
                                                             func=AF.Copy)) if bl % 2 else
                         (lambda e, p=p, bl=bl: e.tensor_copy(out=cT4[:, :, bl, :],
                                                              in_=p[:, 0:128].rearrange("p (h ch) -> p ch h", ch=8))),
                         R=[p], W=[cT])
                S.op('pe', lambda e: e.matmul(psA[:, 128:256], lhsT=rowsel, rhs=cT[:, 0:128:16, :], start=True, stop=True),
                     R=[c2, cT], W=[psA])
                S.op('dve', lambda e: e.tensor_copy(out=refbc[:, :], in_=psA[:, 128:256]), R=[psA], W=[refbc])
                KA = inp['KA']
                refhj = S.sb(es1, 'refhj', [128, 8], F32)
                padm = S.sb(es1, 'padm', [128, 128], F32)
                S.op('dve', lambda e: e.memset(padm[:, :], 0.0), W=[padm])
                S.op('dve', lambda e: e.tensor_scalar(out=padm[:, 16:128], in0=padm[:, 16:128], scalar1=ch0col, scalar2=None,
                                                      op0=ALU.add), R=[padm, c2], W=[padm])
                for j in range(8):
                    S.op('dve', lambda e, j=j: e.scalar_tensor_tensor(
                        out=tmpd[:, 0:16], in0=refbc[:, j * 16:(j + 1) * 16], scalar=1.0, in1=oh16, op0=ALU.mult,
                        op1=ALU.mult, accum_out=refhj[:, j:j + 1]), R=[refbc, c2], W=[tmpd, refhj])
                kv = Ring([S.sb(es1, 'kv', [128, 2, 2176], F32) for _ in range(2)])
                kb = Ring([S.sb(es1, 'kb', [128, 2, 2176], BF16) for _ in range(2)])
                for j in range(8):
                    v_ = kv.next(); b_ = kb.next()
                    S.op('dve', lambda e, j=j, v_=v_: e.tensor_scalar(out=v_[:, 0, :], in0=call[:, :], scalar1=-1.0,
                                                                      scalar2=refhj[:, j:j + 1], op0=ALU.mult, op1=ALU.add),
                         R=[call, refhj], W=[v_])
                    S.op('dve', lambda e, v_=v_: e.tensor_tensor(out=v_[:, 0, 0:128], in0=v_[:, 0, 0:128], in1=padm[:, :],
                                                                 op=ALU.subtract), R=[v_, padm], W=[v_])
                    S.op('act', lambda e, v_=v_, b_=b_: e.activation(out=b_[:, 0, :], in_=v_[:, 0, :], func=AF.Copy),
                         R=[v_], W=[b_])
                    S.op('act', lambda e, v_=v_, b_=b_: e.activation(out=v_[:, 1, :], in_=b_[:, 0, :], func=AF.Copy),
                         R=[b_], W=[v_])
                    S.op('dve', lambda e, v_=v_, b_=b_: e.tensor_tensor(out=b_[:, 1, :], in0=v_[:, 0, :], in1=v_[:, 1, :],
                                                                        op=ALU.subtract), R=[v_], W=[b_])
                    S.dma('sp', lambda e, j=j, b_=b_: e.dma_start(
                        out=KA.t[:, :, 2 * j:2 * j + 2, :].rearrange("h ch r c -> (h ch) r c"), in_=b_[:, :, :]),
                        b_, R=[b_], W=[KA], join=True)
                S.op('dve', lambda e: e.tensor_tensor(out=tmpd[:, :], in0=refbc[:, :], in1=Dg, op=ALU.mult),
                     R=[refbc, c2], W=[tmpd])
                S.op('dve', lambda e: e.tensor_reduce(out=sm[:, 2:3], in_=tmpd[:, :], axis=mybir.AxisListType.X, op=ALU.add),
                     R=[tmpd], W=[sm])
                bq = nc.gpsimd.to_reg(16 * 34 - 1)
                S.dma('pool', lambda e: e.indirect_dma_start(
                    out=cq[:, :], out_offset=None, in_=CALL.t.rearrange("h (a c) -> (h a) c", c=512),
                    in_offset=bass.IndirectOffsetOnAxis(ap=idxq[:, 0:1], axis=0), element_offset=128,
                    bounds_check=bq, oob_is_err=False), cq, R=[CALL, idxq], W=[cq])
                S.op('dve', lambda e: e.tensor_scalar(out=wq[:, 0, :], in0=cq[:, :], scalar1=sm[:, 2:3], scalar2=None,
                                                      op0=ALU.subtract), R=[cq, sm], W=[wq])
                S.op('dve', lambda e: e.tensor_copy(out=qa[:, 0, :], in_=wq[:, 0, :]), R=[wq], W=[qa])
                S.op('dve', lambda e: e.tensor_copy(out=wq[:, 1, :], in_=qa[:, 0, :]), R=[qa], W=[wq])
                S.op('dve', lambda e: e.tensor_tensor(out=qa[:, 1, :], in0=wq[:, 0, :], in1=wq[:, 1, :], op=ALU.subtract),
                     R=[wq], W=[qa])
                S.dma('sp', lambda e: e.dma_start(out=QA[:, :, :], in_=qa[:, :, :]), qa, R=[qa], W=[QA])
                S.barrier()
                S.release([lfa, call, cq, qa] + kb.bufs)
            Kt = Ring([S.sb(es, 'Kt', [96, 136 * 128], BF16) for _ in range(2)])
            Vt = Ring([S.sb(es, 'Vt', [128, NKB, 65], BF16) for _ in range(2)])
            Qt = Ring([S.sb(es, 'Qt', [96, 512], BF16) for _ in range(8)])
            Pt = Ring([S.sb(es, 'Pt', [128, 1024], BF16) for _ in range(4)])
            osb = Ring([S.sb(es, 'osb', [64, 512], F32) for _ in range(2)])
            r65 = Ring([S.sb(es, 'r65', [65, 512], F32) for _ in range(2)])
            asb = Ring([S.sb(es, 'asb', [64, 512], BF16) for _ in range(2)])
            Sp = Ring([S.ps(es, 'Sp', [128, 1024]) for _ in range(3)])
            Op = Ring([S.ps(es, 'Op', [128, 512]) for _ in range(1)])
            bcp = S.ps(es, 'bcp', [128, 512])
            for k_ in Kt.bufs:
                S.op('pool', lambda e, k_=k_: e.memset(k_[64:96, :], 0.0), W=[k_])
                S.op('pool', lambda e, k_=k_: e.memset(k_[64:66, :], 1.0), W=[k_])
                S.op('pool', lambda e, k_=k_: e.memset(k_[0:64, 0:128], 0.0), W=[k_])
            for jq, q_ in enumerate(Qt.bufs):
                S.dma('sp', lambda e, jq=jq, q_=q_: e.dma_start(out=q_[64:96, :], in_=inp['qone'][jq]), q_,
                      R=[inp['qone']], W=[q_])
            for v_ in Vt.bufs:
                S.op('pool', lambda e, v_=v_: e.memset(v_[:, 0, :], 0.0), W=[v_])
            for r_ in r65.bufs:
                S.op('pool', lambda e, r_=r_: e.memset(r_[:, :], 0.0), W=[r_])
            if after_init is not None:
                after_init()
            units = []
            for h in range(NH):
                for j in range(self.NSEG):
                    nblk = 1 + 16 * (j + 1)
                    b = 0
                    while b < nblk:
                        nb_ = min(2, nblk - b)
                        units.append((h, j, b, nb_, nblk))
                        b += nb_
            state = {}

            def prep_hj(h, j):
                if (h, j) in state:
                    return state[(h, j)]
                if j == 0:
                    k_ = Kt.next(); v_ = Vt.next()
                    for jj in range(8):
                        rk = RK[jj][h // 8]
                        S.dma('sp', lambda e, jj=jj, rk=rk: e.dma_start(
                            out=k_.t[0:64, 128 + jj * 2048:128 + (jj + 1) * 2048].rearrange("p (c t) -> p c t", c=4),
                            in_=rk.t.rearrange("(c r) t -> r c t", c=4)[ts(h % 8, 64)]),
                            k_, R=[rk], W=[k_], join=(jj > 0))
                    rk = RK[8][h // 8]
                    S.dma('sp', lambda e: e.dma_start(out=k_[0:64, 0:16], in_=rk[ts(h % 8, 64), 0:16]),
                          k_, R=[rk], W=[k_], join=True)
                    S.dma('sp', lambda e: e.dma_start(
                        out=k_.t[66:82, :].rearrange("r (ch c) -> r ch c", ch=8),
                        in_=inp['KA'].t[h].rearrange("ch r c -> r ch c")), k_, R=[inp['KA']], W=[k_], join=True)
                    for jj in range(8):
                        rv = RV[jj][h // 4]
                        S.dma('sp', lambda e, jj=jj, rv=rv: e.dma_start(
                            out=v_.t[:, 1 + 16 * jj:17 + 16 * jj, :].rearrange("p (c k) d -> p c (k d)", c=4),
                            in_=rv.t.rearrange("(c r) kd -> r c kd", c=4)[ts(h % 4, 128)]),
                            v_, R=[rv], W=[v_], join=(jj > 0))
                    rv = RV[8][h // 4]
                    S.dma('sp', lambda e: e.dma_start(out=v_[0:16, 0, :], in_=rv[(h % 4) * 128:(h % 4) * 128 + 16, 0:65]),
                          v_, R=[rv], W=[v_], join=True)
                    state[('kv', h)] = (k_, v_)
                k_, v_ = state[('kv', h)]
                q_ = Qt.bufs[j]
                S.dma('sp', lambda e: e.dma_start(out=q_[0:64, :], in_=QT[h, :, ts(j, 512)]), q_, R=[QT], W=[q_])
                S.dma('sp', lambda e: e.dma_start(out=q_[64:66, :], in_=QA[h * 8 + j, :, :]), q_, R=[QA], W=[q_], join=True)
                state[(h, j)] = (k_, v_, q_, Op.next())
                return state[(h, j)]

            def emit_qk(un):
                h, j, b0, nb_, nblk = un
                k_, v_, q_, o_ = prep_hj(h, j)
                s_ = Sp.next()
                for i in range(nb_):
                    b = b0 + i
                    lvl = b >= nblk - 16
                    S.op('pe', lambda e, b=b, i=i, lvl=lvl: e.matmul(s_[:, ts(i, 512)], lhsT=k_[:, ts(b, 128)], rhs=q_[:, :],
                                                                      start=True, stop=not lvl), R=[k_, q_], W=[s_])
                    if lvl:
                        br = b - (nblk - 16)
                        S.op('pe', lambda e, i=i, br=br: e.matmul(s_[:, ts(i, 512)], lhsT=c['identb'], rhs=mk[:, br, :],
                                                                    start=False, stop=True), R=[c['b'], mk], W=[s_])
                return s_

            LOOK = 2
            sq = [emit_qk(units[n]) for n in range(LOOK)]
            for n, un in enumerate(units):
                h, j, b0, nb_, nblk = un
                if n + LOOK < len(units):
                    sq.append(emit_qk(units[n + LOOK]))
                k_, v_, q_, o_ = state[(h, j)]
                s_ = sq[n]
                p_ = Pt.next()
                w_ = nb_ * 512
                S.op('act', lambda e: e.activation(out=p_[:, 0:w_], in_=s_[:, 0:w_], func=AF.Exp), R=[s_], W=[p_])
                for i in range(nb_):
                    b = b0 + i
                    S.op('pe', lambda e, b=b, i=i: e.matmul(o_[0:65, :], lhsT=v_[:, b, :], rhs=p_[:, ts(i, 512)],
                                                            start=(b == 0), stop=(b == nblk - 1)), R=[v_, p_], W=[o_])
                if b0 + nb_ == nblk:
                    r_ = r65.next(); os_ = osb.next(); a_ = asb.next()
                    S.op('dve', lambda e: e.reciprocal(out=r_[64:65, :], in_=o_[64:65, :]), R=[o_], W=[r_])
                    S.op('dve', lambda e: e.tensor_copy(out=os_[:, :], in_=o_[0:64, :]), R=[o_], W=[os_])
                    S.op('pe', lambda e: e.matmul(bcp[0:64, :], lhsT=E65, rhs=r_[:, :], start=True, stop=True),
                         R=[c2, r_], W=[bcp])
                    S.op('dve', lambda e: e.tensor_tensor(out=a_[:, :], in0=os_[:, :], in1=bcp[0:64, :], op=ALU.mult),
                         R=[os_, bcp], W=[a_])
                    S.dma('sp', lambda e: e.dma_start(out=AT[h // 2, (h % 2) * 64:(h % 2) * 64 + 64, ts(j, 512)],
                                                      in_=a_[:, :]), a_, R=[a_], W=[AT], join=True)
                    del state[(h, j)]
            S.barrier()

    def oproj_route(self, S, inp, ctx, gidx, dbg_out=None):
        c = self.c
        AT, H2 = inp['AT'], inp['H2']
        wo_d = inp['attn_w_out']
        with ExitStack() as es:
            R = self.route_bufs(S, es)
            self.ln_bufs(S, es, R)
            R['big'] = Ring([S.ps(es, 'big', [128, D]) for _ in range(2)])
            mm = Ring([S.ps(es, 'mm', [128, 512]) for _ in range(3)])
            wo = S.sb(es, 'wo', [128, KC, D], BF16)
            S.dma('pool', lambda e: e.dma_start(out=wo[:, :, :], in_=wo_d[0].rearrange("(kc p) f -> p kc f", p=128)),
                  wo, R=[wo_d], W=[wo])
            gb = S.sb(es, 'gb', [128, 2, D], F32)
            self.load_gb(S, gb, inp['ln_g'], inp['ln_b'], gidx)
            aT = Ring([S.sb(es, 'aT', [128, KC, 512], BF16) for _ in range(2)])
            hres = Ring([S.sb(es, 'hres', [128, D], F32) for _ in range(2)])
            acc = Ring([S.sb(es, 'acc', [128, D], F32) for _ in range(2)])
            h3 = Ring([S.sb(es, 'h3', [128, D], F32) for _ in range(2)])
            for j in range(self.NSEG):
                a_ = aT.next()
                S.dma('sp', lambda e: e.dma_start(out=a_[:, :, :], in_=AT[:, :, ts(j, 512)].rearrange("pr p t -> p pr t")),
                      a_, R=[AT], W=[a_])
                for t in range(4):
                    i = 4 * j + t
                    hr = hres.next()
                    S.dma('sp', lambda e: e.dma_start(out=hr[:, :], in_=H2[ts(i, 128), :]), hr, R=[H2], W=[hr])
                    ac = acc.next()
                    for hf in range(2):
                        p = mm.next()
                        for pr in range(KC):
                            S.op('pe', lambda e, pr=pr: e.matmul(p[:, :], lhsT=a_[:, pr, ts(t, 128)],
                                                                 rhs=wo[:, pr, ts(hf, 512)], start=(pr == 0),
                                                                 stop=(pr == KC - 1)), R=[a_, wo], W=[p])
                        S.op('dve', lambda e: e.scalar_tensor_tensor(out=ac[:, ts(hf, 512)], in0=hr[:, ts(hf, 512)],
                                                                     scalar=ALPHA, in1=p[:, :], op0=ALU.mult, op1=ALU.add),
                             R=[hr, p], W=[ac])
                    h = h3.next()
                    self.ln_tile(S, R, ac[:, :], ac, gb, h)
                    if dbg_out is None:
                        self.route_tile(S, R, i, h, ctx)
                    else:
                        S.dma('sp', lambda e: e.dma_start(out=dbg_out[ts(i, 128), :], in_=h[:, :]), h,
                              R=[h], W=[dbg_out], join=True)
            S.barrier()
            S.release([b for r in R.values() for b in r.bufs] + aT.bufs + hres.bufs + [wo, gb])


def make_consts(CAP):
    f = np.zeros((128, 800), np.float32)
    f[:, 0:128] = np.eye(128, dtype=np.float32)
    f[:, 256:288] = (np.arange(32, dtype=np.float32) * CAP - BIG)[None, :]
    for g, w in enumerate((2, 4, 8, 16)):
        t = np.arange(128)
        f[:, 288 + g * 128:288 + (g + 1) * 128] = (1.0 / np.minimum(t + 1, w))[None, :]
    b = np.zeros((128, 384), np.float32)
    b[:, 0:128] = np.eye(128)
    b[:, 128:256] = np.triu(np.ones((128, 128)), 1)
    b[:, 256:384] = 1.0
    return f, b.astype(ml_dtypes.bfloat16)


def make_xin(x, meta, core, nseg=8):
    b, cl = core // 4, core % 4
    rows = np.zeros((nseg * 528 + 144, D), np.float32)
    for j in range(nseg):
        G = cl + 4 * j
        s = G * 512
        if s == 0:
            rows[j * 528:j * 528 + 16] = meta
        else:
            rows[j * 528:j * 528 + 16] = x[b, s - 16:s]
        rows[j * 528 + 16:(j + 1) * 528] = x[b, s:s + 512]
    rows[nseg * 528 + 16:nseg * 528 + 32] = meta
    return rows


def make_valid(NT):
    v = np.ones((128, NT), np.float32)
    v[16:, NT - 1] = 0.0
    return v


def decl_weights(P, inp, layers=(0, 1)):
    nl = len(layers)
    P.wbase = layers[0]
    inp['ln_g'] = P.dt('ln_g', [4, D], F32, 'ExternalInput')
    inp['ln_b'] = P.dt('ln_b', [4, D], F32, 'ExternalInput')
    inp['router_w'] = P.dt('router_w', [2, D, 32], F32, 'ExternalInput')
    inp['router_b'] = P.dt('router_b', [2, 32], F32, 'ExternalInput')
    inp['w1'] = P.dt('w1', [nl, 32, D, 2 * D], F32, 'ExternalInput')
    inp['b1'] = P.dt('b1', [nl, 32, 2 * D], F32, 'ExternalInput')
    inp['w2'] = P.dt('w2', [nl, 32, D, D], F32, 'ExternalInput')
    inp['b2'] = P.dt('b2', [nl, 32, D], F32, 'ExternalInput')


def make_consts2():
    f = np.zeros((128, 466), np.float32)
    k = np.arange(128)
    f[k, 449 + k // 8] = 1.0
    f[k % 8 == 0, 465] = 30000.0
    f[:, 0:128] = ((k[:, None] // 8 == k[None, :] // 8) & (k[:, None] % 8 < k[None, :] % 8)).astype(np.float32)
    f[127, 128:256] = 1.0
    for p in range(128):
        h, j = p // 8, p % 8
        f[p, 256 + j * 16 + h] = 1.0
    f[16:, 384] = 30000.0
    f[64, 385:449] = 1.0
    return f


def make_qone():
    q = np.zeros((8, 32, 512), np.float32)
    for j in range(8):
        q[j, 2 + 2 * j:4 + 2 * j, :] = 1.0
    return q.astype(ml_dtypes.bfloat16)


def make_mask(cl):
    m = np.full((16, 128, 512), NEG, np.float32)
    ki = np.arange(128)[:, None]
    qi = np.arange(128)[None, :]
    for r in range(4):
        for kb in range(4):
            for qb in range(4):
                if r < cl or (r == cl and kb < qb):
                    m[r * 4 + kb, :, ts(qb, 128)] = 0.0
                elif r == cl and kb == qb:
                    m[r * 4 + kb, :, ts(qb, 128)] = np.where(ki <= qi, 0.0, NEG)
    return m.astype(ml_dtypes.bfloat16)


def make_idxq(cl):
    p = np.arange(128)
    return ((p // 8) * 34 + cl + 4 * (p % 8)).astype(np.int32).reshape(128, 1)


def build_fused(cfg):
    P = Prog(cfg)
    nc = P.nc
    CAP = P.CAP
    NTOK = 33 * 128
    inp = {}
    inp['xin'] = P.dt('xin', [cfg['NSEG'] * 528 + 144, D], F32, 'ExternalInput')
    valid_d = P.dt('valid', [128, 33], F32, 'ExternalInput')
    inp['pool_w'] = P.dt('pool_w', [1, 4, 256, 256], F32, 'ExternalInput')
    inp['pool_scale'] = P.dt('pool_scale', [1, D], F32, 'ExternalInput')
    inp['attn_w_in'] = P.dt('attn_w_in', [1, D, 3088], F32, 'ExternalInput')
    inp['attn_b_f'] = P.dt('attn_b_f', [1, 16], F32, 'ExternalInput')
    inp['attn_w_out'] = P.dt('attn_w_out', [1, D, D], F32, 'ExternalInput')
    inp['maskT'] = P.dt('maskT', [16, 128, 512], BF16, 'ExternalInput')
    inp['idxq'] = P.dt('idxq', [128, 1], I32, 'ExternalInput')
    inp['cst2'] = P.dt('cst2', [128, 466], F32, 'ExternalInput')
    inp['qone'] = P.dt('qone', [8, 32, 512], BF16, 'ExternalInput')
    inp['KA'] = P.dt('KAs', [16, 8, 16, 2176], BF16, 'Internal')
    decl_weights(P, inp, layers=(0, 1))
    inp['H'] = P.dt('Hs', [NTOK, D], F32, 'Internal')
    inp['XS'] = P.dt('XS', [32 * CAP, D], BF16, 'Internal')
    inp['YS'] = P.dt('YS', [32 * CAP, D], F32, 'Internal')
    inp['H2'] = P.dt('H2s', [NTOK, D], F32, 'Internal')
    inp['QT'] = P.dt('QTs', [16, 64, NTOK], BF16, 'Internal')
    SNDK, SNDV, RCVK, RCVV = [], [], [], []
    for sg in range(9):
        wk = 512 if sg < 8 else 128
        wv = 260 if sg < 8 else 65
        SNDK.append([P.dt('SK%d_%d' % (sg, i), [512, wk], BF16, 'Internal') for i in range(2)])
        RCVK.append([P.dt('RK%d_%d' % (sg, i), [2048, wk], BF16, 'Internal') for i in range(2)])
        SNDV.append([P.dt('SV%d_%d' % (sg, i), [512, wv], BF16, 'Internal') for i in range(4)])
        RCVV.append([P.dt('RV%d_%d' % (sg, i), [2048, wv], BF16, 'Internal') for i in range(4)])
    SNDL = P.dt('SNDL', [16, NTOK], F32, 'Internal')
    inp['RCVK'] = RCVK
    inp['RCVV'] = RCVV
    inp['RCVL'] = P.dt('RCVL', [64, NTOK], F32, 'Internal')
    inp['LFT'] = P.dt('LFT', [16, 17408], F32, 'Internal')
    inp['AT'] = P.dt('ATs', [8, 128, 4096], BF16, 'Internal')
    inp['CALL'] = P.dt('CALL', [16, 17408], F32, 'Internal')
    inp['QA'] = P.dt('QAs', [128, 2, 512], BF16, 'Internal')
    out = P.dt('out', [32 * 128, D], F32, 'ExternalOutput')
    outs = dict(H2=inp['H2'], QT=inp['QT'], KT=SNDK, V=SNDV, LF=SNDL)
    groups = [[0, 1, 2, 3], [4, 5, 6, 7]]
    with ExitStack() as es:
        S = Sch(nc, es, n_dsem=96)
        P.consts(S, es)

        deferred = {'B': [], 'C': [], 'D': []}

        def issue(pairs):
            ds = S.dfree.pop()
            ds.nobar = True
            for (a_, d_) in pairs:
                d_.dsem = ds
                S.coll("AllGather", groups, a_, d_)
            for (a_, d_) in pairs:
                d_.w = {id(ds.sem): (ds.sem, ds.cnt, 'dma')}

        def exchange(sg):
            if sg < 0:
                S.coll("AllGather", groups, SNDL, inp['RCVL'])
                return
            issue([(SNDK[sg][0], RCVK[sg][0]), (SNDV[sg][0], RCVV[sg][0])])
            deferred['B'].append((SNDV[sg][1], RCVV[sg][1]))
            deferred['C'].append((SNDK[sg][1], RCVK[sg][1]))
            deferred['C'].append((SNDV[sg][2], RCVV[sg][2]))
            deferred['D'].append((SNDV[sg][3], RCVV[sg][3]))
        outs['exchange'] = exchange
        inp['valid_sb'] = S.sb(es, 'valid', [128, 33], F32)
        S.dma('sp', lambda e: e.dma_start(out=inp['valid_sb'][:, :], in_=valid_d[:, :]), inp['valid_sb'],
              R=[valid_d], W=[inp['valid_sb']])
        P.NT = 33
        ctx0 = P.moe_ctx(S, es, 0, inp)
        P.mixer0(S, inp, ctx0, 0)
        P.experts(S, inp, ctx0, 0)
        P.combine_qkv(S, inp, ctx0, 1, outs)
        P.NT = 32
        def rest_of_exchange():
            for g_ in ('B', 'C', 'D'):
                issue(deferred[g_])
        P.attention(S, inp, es, after_init=rest_of_exchange)
        ctx1 = P.moe_ctx(S, es, 1, inp)
        P.oproj_route(S, inp, ctx1, 2)
        P.experts(S, inp, ctx1, 1)
        with ExitStack() as es2:
            R = P.combine_bufs(S, es2)
            gb = S.sb(es2, 'gb2', [128, 2, D], F32)
            P.load_gb(S, gb, inp['ln_g'], inp['ln_b'], 3)
            ho = Ring([S.sb(es2, 'ho', [128, D], F32) for _ in range(2)])
            for i in range(32):
                h = ho.next()
                P.combine_tile(S, R, i, ctx1, gb, h)
                S.dma('sp', lambda e, h=h, i=i: e.dma_start(out=out[ts(i, 128), :], in_=h[:, :]), h,
                      R=[h], W=[out], join=True)
            S.barrier()
    return P


CFG = dict(NT=33, NSEG=8, CAP=768)


def make_maps(inputs):
    cf, cb = make_consts(CFG['CAP'])
    c2 = make_consts2()
    maps = []
    for c in range(8):
        cl = c % 4
        maps.append(dict(
            xin=make_xin(inputs['x'], inputs['meta_tokens'], c), valid=make_valid(33), cst_f=cf, cst_b=cb, cst2=c2,
            maskT=make_mask(cl), idxq=make_idxq(cl), qone=make_qone(),
            pool_w=inputs['pool_w'], pool_scale=inputs['pool_scale'], ln_g=inputs['ln_g'].reshape(4, D),
            ln_b=inputs['ln_b'].reshape(4, D), router_w=inputs['router_w'], router_b=inputs['router_b'],
            w1=inputs['w1'], b1=inputs['b1'], w2=inputs['w2'], b2=inputs['b2'],
            attn_w_in=inputs['attn_w_in'], attn_b_f=inputs['attn_b_f'], attn_w_out=inputs['attn_w_out']))
    return maps


def assemble(res):
    out = np.zeros((2, SEQ, D), np.float32)
    for c in range(8):
        b, cl = c // 4, c % 4
        o = np.asarray(res[c]['out'], dtype=np.float32)
        for j in range(8):
            G = cl + 4 * j
            out[b, G * 512:(G + 1) * 512] = o[j * 512:(j + 1) * 512]
    return out


def kernel(**inputs):
    inputs = {k: np.ascontiguousarray(np.asarray(v)) for k, v in inputs.items()}
    P = build_fused(CFG)
    res = run_bass_kernel_spmd(P.nc, make_maps(inputs), core_ids=list(range(8))).results
    return assemble(res)
```
